# Optimizing a Trainium2 kernel written in Bass

```python
import math
import jax, jax.numpy as jnp
from jax import lax
import numpy as np

D_MODEL = 1024
BATCH = 8
SEQ = 8192
DEPTH = 4

GRID_W = 64
CTX_LEN = 256
EPS = 1e-6
N_MOD = 6

GLA_HEADS = 4
GLA_DK = 64
GLA_DV = 128
GLA_KW = GLA_HEADS * GLA_DK
GLA_VW = GLA_HEADS * GLA_DV
GATE_RANK = 16
GATE_TAU = 16.0
CHUNK = 64
ROPE_BASE = 10000.0

NA_HEADS = 8
NA_DH = 64
NA_W = NA_HEADS * NA_DH
NA_KH = 8
NA_KW = 16

N_BRANCH = 2
IN_SPLITS = (GLA_KW, GLA_KW, GLA_VW, 2 * GATE_RANK, GLA_VW, NA_W, NA_W, NA_W, N_BRANCH * D_MODEL)
IN_COLS = 2 * GLA_KW + 2 * GLA_VW + 2 * GATE_RANK + 3 * NA_W + N_BRANCH * D_MODEL

PEER_HEADS = 8
N_KEYS = 128
N_EXPERTS = N_KEYS * N_KEYS
PEER_DK = 256
PEER_TOPK = 16
TOKEN_BLOCK = 128

kernel_name = "hybrid_gla_natten_peer_dit"


def rmsnorm(x, g):
    xf = x.astype(jnp.float32)
    y = xf * lax.rsqrt(jnp.mean(xf * xf, axis=-1, keepdims=True) + EPS)
    return (y * g.astype(jnp.float32)).astype(x.dtype)


def split_cols(t, sizes):
    outs, start = [], 0
    for s in sizes:
        outs.append(t[..., start:start + s])
        start += s
    return outs


def heads(t, n_heads):
    return t.reshape(t.shape[:-1] + (n_heads, t.shape[-1] // n_heads))


def axial_rope(x):
    n = x.shape[1]
    t = jnp.arange(n, dtype=jnp.int32)
    pos = jnp.stack([t // GRID_W, t % GRID_W], axis=0).astype(jnp.float32)
    half = x.shape[-1] // 2
    quarter = half // 2
    freqs = ROPE_BASE ** (-jnp.arange(quarter, dtype=jnp.float32) / quarter)
    ang = pos[:, :, None] * freqs
    cos = jnp.cos(ang).astype(x.dtype)
    sin = jnp.sin(ang).astype(x.dtype)

    def rot(xs, cs, sn):
        x1, x2 = xs[..., :quarter], xs[..., quarter:]
        cs = cs[None, :, None, :]
        sn = sn[None, :, None, :]
        return jnp.concatenate([x1 * cs - x2 * sn, x1 * sn + x2 * cs], axis=-1)

    return jnp.concatenate([rot(x[..., :half], cos[0], sin[0]),
                            rot(x[..., half:], cos[1], sin[1])], axis=-1)


def gla_scan(q, k, v, log_a, s0):
    B, L, H, dk = q.shape
    dv = v.shape[-1]
    n = L // CHUNK

    def chunks(t):
        return t.astype(jnp.float32).reshape(B, n, CHUNK, H, t.shape[-1]).transpose(1, 0, 3, 2, 4)

    mask = jnp.tril(jnp.ones((CHUNK, CHUNK), dtype=bool))[:, :, None]

    def step(s, inp):
        qi, ki, vi, ai = inp
        b = jnp.cumsum(ai, axis=2)
        rel = b[:, :, :, None, :] - b[:, :, None, :, :]
        decay = jnp.where(mask, jnp.exp(jnp.minimum(rel, 0.0)), 0.0)
        attn = jnp.einsum('bhik,bhjk,bhijk->bhij', qi, ki, decay)
        o = (jnp.einsum('bhij,bhjv->bhiv', attn, vi)
             + jnp.einsum('bhik,bhkv->bhiv', qi * jnp.exp(b), s))
        b_end = b[:, :, -1, :]
        s = (jnp.exp(b_end)[..., None] * s
             + jnp.einsum('bhjk,bhjv->bhkv', ki * jnp.exp(b_end[:, :, None, :] - b), vi))
        return s, o

    s_fin, o = lax.scan(step, s0, (chunks(q), chunks(k), chunks(v), chunks(log_a)))
    o = o.transpose(1, 0, 3, 2, 4).reshape(B, L, H, dv)
    return o.astype(v.dtype), s_fin


def flip(t):
    return jnp.flip(t, axis=1)


def na_latent(q, k, v, k_ctx, v_ctx, rpb):
    B, L, H, dh = q.shape
    rows = L // GRID_W
    kh = min(NA_KH, rows)
    scale = NA_DH ** -0.5
    qg = q.reshape(B, rows, GRID_W, H, dh)
    kg = k.reshape(B, rows, GRID_W, H, dh)
    vg = v.reshape(B, rows, GRID_W, H, dh)
    col = jnp.arange(GRID_W, dtype=jnp.int32)
    c0 = jnp.clip(col - NA_KW // 2, 0, GRID_W - NA_KW)
    col_idx = c0[:, None] + jnp.arange(NA_KW, dtype=jnp.int32)[None, :]
    col_off = col_idx - col[:, None] + (NA_KW - 1)

    def one_row(r):
        r0 = jnp.clip(r - kh // 2, 0, rows - kh)
        k_rows = lax.dynamic_slice_in_dim(kg, r0, kh, axis=1)
        v_rows = lax.dynamic_slice_in_dim(vg, r0, kh, axis=1)
        k_win = k_rows[:, :, col_idx]
        v_win = v_rows[:, :, col_idx]
        q_r = lax.dynamic_index_in_dim(qg, r, axis=1, keepdims=False)
        row_off = r0 + jnp.arange(kh, dtype=jnp.int32) - r + (NA_KH - 1)
        bias = rpb[:, row_off[None, :, None], col_off[:, None, :]]
        s_win = (jnp.einsum('bwhd,bawihd->bhwai', q_r, k_win).astype(jnp.float32) * scale
                 + bias.astype(jnp.float32)[None])
        s_ctx = jnp.einsum('bwhd,bchd->bhwc', q_r, k_ctx).astype(jnp.float32) * scale
        s_all = jnp.concatenate([s_win.reshape(B, H, GRID_W, kh * NA_KW), s_ctx], axis=-1)
        p = jax.nn.softmax(s_all, axis=-1).astype(v.dtype)
        p_win = p[..., :kh * NA_KW].reshape(B, H, GRID_W, kh, NA_KW)
        p_ctx = p[..., kh * NA_KW:]
        return (jnp.einsum('bhwai,bawihd->bwhd', p_win, v_win)
                + jnp.einsum('bhwc,bchd->bwhd', p_ctx, v_ctx))

    out = lax.map(one_row, jnp.arange(rows, dtype=jnp.int32))
    return out.transpose(1, 0, 2, 3, 4).reshape(B, L, H * dh)


def na_context(q, k, v):
    B, Lc, H, dh = q.shape
    s = jnp.einsum('bqhd,bkhd->bhqk', q, k).astype(jnp.float32) * (NA_DH ** -0.5)
    p = jax.nn.softmax(s, axis=-1).astype(v.dtype)
    return jnp.einsum('bhqk,bkhd->bqhd', p, v).reshape(B, Lc, H * dh)


def gla_inputs(parts, rope, w_af, b_af, w_ab, b_ab):
    q, k, v, z, r = parts[:5]
    q = heads(q, GLA_HEADS)
    k = heads(k, GLA_HEADS)
    if rope:
        q, k = axial_rope(q), axial_rope(k)
    q = q * (GLA_DK ** -0.5)
    v = heads(v, GLA_HEADS)
    zf, zb = z[..., :GATE_RANK], z[..., GATE_RANK:]
    la_f = heads(jax.nn.log_sigmoid((zf @ w_af + b_af).astype(jnp.float32)) / GATE_TAU, GLA_HEADS)
    la_b = heads(jax.nn.log_sigmoid((zb @ w_ab + b_ab).astype(jnp.float32)) / GATE_TAU, GLA_HEADS)
    return q, k, v, la_f, la_b, r


def gla_out(o, r, g_gla, w_pg):
    o = rmsnorm(o, g_gla)
    o = o.reshape(o.shape[:2] + (GLA_VW,)) * jax.nn.silu(r)
    return o @ w_pg


def token_mixer(h, hc, w_in, w_af, b_af, w_ab, b_ab, g_gla, rpb, w_pg, w_pn, w_o, with_ctx_out):
    p = split_cols(h @ w_in, IN_SPLITS)
    pc = split_cols(hc @ w_in, IN_SPLITS)
    B = h.shape[0]

    qc, kc, vc, lafc, labc, rc = gla_inputs(pc, False, w_af, b_af, w_ab, b_ab)
    ql, kl, vl, lafl, labl, rl = gla_inputs(p, True, w_af, b_af, w_ab, b_ab)
    s0 = jnp.zeros((B, GLA_HEADS, GLA_DK, GLA_DV), jnp.float32)
    o_cf, s_cf = gla_scan(qc, kc, vc, lafc, s0)
    o_cb, s_cb = gla_scan(flip(qc), flip(kc), flip(vc), flip(labc), s0)
    o_lf, _ = gla_scan(ql, kl, vl, lafl, s_cf)
    o_lb, _ = gla_scan(flip(ql), flip(kl), flip(vl), flip(labl), s_cb)
    ya = gla_out(o_lf + flip(o_lb), rl, g_gla, w_pg)

    nq, nk, nv = (heads(t, NA_HEADS) for t in p[5:8])
    nkc, nvc = heads(pc[6], NA_HEADS), heads(pc[7], NA_HEADS)
    yb = na_latent(nq, nk, nv, nkc, nvc, rpb) @ w_pn

    gates = p[8]
    y = (jax.nn.sigmoid(gates[..., :D_MODEL]) * ya + jax.nn.sigmoid(gates[..., D_MODEL:]) * yb) @ w_o

    if not with_ctx_out:
        return y, None
    ya_c = gla_out(o_cf + flip(o_cb), rc, g_gla, w_pg)
    yb_c = na_context(heads(pc[5], NA_HEADS), nkc, nvc) @ w_pn
    gates_c = pc[8]
    yc = (jax.nn.sigmoid(gates_c[..., :D_MODEL]) * ya_c + jax.nn.sigmoid(gates_c[..., D_MODEL:]) * yb_c) @ w_o
    return y, yc


def peer(h, w_query, sub_keys, expert_u, expert_v):
    shape = h.shape
    tok = h.reshape(-1, TOKEN_BLOCK, shape[-1])

    def block(xb):
        tb = xb.shape[0]
        qb = (xb @ w_query).reshape(tb, PEER_HEADS, 2, PEER_DK // 2)
        s = jnp.einsum('thpd,hpnd->thpn', qb, sub_keys).astype(jnp.float32)
        s_top, i_top = lax.top_k(s, PEER_TOPK)
        cand = s_top[:, :, 0, :, None] + s_top[:, :, 1, None, :]
        cand_idx = i_top[:, :, 0, :, None] * N_KEYS + i_top[:, :, 1, None, :]
        best, pos = lax.top_k(cand.reshape(tb, PEER_HEADS, PEER_TOPK * PEER_TOPK), PEER_TOPK)
        experts = jnp.take_along_axis(cand_idx.reshape(tb, PEER_HEADS, PEER_TOPK * PEER_TOPK), pos, axis=-1)
        g = jax.nn.softmax(best, axis=-1).reshape(tb, PEER_HEADS * PEER_TOPK).astype(xb.dtype)
        experts = experts.reshape(tb, PEER_HEADS * PEER_TOPK)
        u = jnp.take(expert_u, experts, axis=0)
        v = jnp.take(expert_v, experts, axis=0)
        act = jax.nn.gelu(jnp.einsum('td,ted->te', xb, u))
        return jnp.einsum('te,ted->td', act * g, v)

    return lax.map(block, tok).reshape(shape)


def setup_inputs(seed: int = 0) -> dict:
    key = jax.random.key(seed)
    ks = jax.random.split(key, 32)
    D = D_MODEL
    f32 = jnp.float32

    def nrm(k, shape, s):
        return jax.random.normal(k, shape, f32) * s

    return {
        "x": nrm(ks[0], (BATCH, SEQ, D), 1.0),
        "c": nrm(ks[1], (BATCH, D), 1.0),
        "ctx": nrm(ks[2], (BATCH, CTX_LEN, D), 1.0),
        "c_ctx": nrm(ks[3], (D,), 1.0),
        "w_mod": nrm(ks[4], (DEPTH, D, N_MOD * D), 0.5 * D ** -0.5),
        "b_mod": nrm(ks[5], (DEPTH, N_MOD * D), 0.02),
        "g_norm1": 1.0 + nrm(ks[6], (DEPTH, D), 0.02),
        "w_in": nrm(ks[7], (DEPTH, D, IN_COLS), D ** -0.5),
        "w_alpha_f": nrm(ks[8], (DEPTH, GATE_RANK, GLA_KW), GATE_RANK ** -0.5),
        "b_alpha_f": nrm(ks[9], (DEPTH, GLA_KW), 0.1),
        "w_alpha_b": nrm(ks[10], (DEPTH, GATE_RANK, GLA_KW), GATE_RANK ** -0.5),
        "b_alpha_b": nrm(ks[11], (DEPTH, GLA_KW), 0.1),
        "g_gla": 1.0 + nrm(ks[12], (DEPTH, GLA_DV), 0.02),
        "rpb": nrm(ks[13], (DEPTH, NA_HEADS, 2 * NA_KH - 1, 2 * NA_KW - 1), 0.1),
        "w_proj_gla": nrm(ks[14], (DEPTH, GLA_VW, D), GLA_VW ** -0.5),
        "w_proj_na": nrm(ks[15], (DEPTH, NA_W, D), NA_W ** -0.5),
        "w_out": nrm(ks[16], (DEPTH, D, D), D ** -0.5),
        "g_norm2": 1.0 + nrm(ks[17], (DEPTH, D), 0.02),
        "w_query": nrm(ks[18], (DEPTH, D, PEER_HEADS * PEER_DK), D ** -0.5),
        "sub_keys": nrm(ks[19], (DEPTH, PEER_HEADS, 2, N_KEYS, PEER_DK // 2), (PEER_DK // 2) ** -0.5),
        "expert_u": nrm(ks[20], (DEPTH, N_EXPERTS, D), D ** -0.5),
        "expert_v": nrm(ks[21], (DEPTH, N_EXPERTS, D), PEER_HEADS ** -0.5),
        "g_final": 1.0 + nrm(ks[22], (D,), 0.02),
    }


def reference(x, c, ctx, c_ctx, w_mod, b_mod, g_norm1, w_in, w_alpha_f, b_alpha_f, w_alpha_b, b_alpha_b,
              g_gla, rpb, w_proj_gla, w_proj_na, w_out, g_norm2, w_query, sub_keys, expert_u, expert_v,
              g_final):
    c_act = jax.nn.silu(c)
    cc_act = jax.nn.silu(c_ctx)
    xc = ctx
    for i in range(DEPTH):
        last = i == DEPTH - 1
        mod = c_act @ w_mod[i] + b_mod[i]
        mod_c = cc_act @ w_mod[i] + b_mod[i]
        sh1, sc1, ga1, sh2, sc2, ga2 = jnp.split(mod[:, None, :], N_MOD, axis=-1)
        csh1, csc1, cga1, csh2, csc2, cga2 = jnp.split(mod_c, N_MOD, axis=-1)

        h = rmsnorm(x, g_norm1[i]) * (1 + sc1) + sh1
        hc = rmsnorm(xc, g_norm1[i]) * (1 + csc1) + csh1
        y, yc = token_mixer(h, hc, w_in[i], w_alpha_f[i], b_alpha_f[i], w_alpha_b[i], b_alpha_b[i],
                            g_gla[i], rpb[i], w_proj_gla[i], w_proj_na[i], w_out[i], not last)
        x = x + ga1 * y
        h2 = rmsnorm(x, g_norm2[i]) * (1 + sc2) + sh2
        x = x + ga2 * peer(h2, w_query[i], sub_keys[i], expert_u[i], expert_v[i])

        if not last:
            xc = xc + cga1 * yc
            hc2 = rmsnorm(xc, g_norm2[i]) * (1 + csc2) + csh2
            xc = xc + cga2 * peer(hc2, w_query[i], sub_keys[i], expert_u[i], expert_v[i])
    return rmsnorm(x, g_final)
```

```python
import contextlib
import numpy as np
import concourse.bass as bass
import concourse.mybir as mybir
from concourse.bass_utils import run_bass_kernel_spmd

F32 = mybir.dt.float32
BF16 = mybir.dt.bfloat16
I32 = mybir.dt.int32
U32 = mybir.dt.uint32
AF = mybir.ActivationFunctionType
ALU = mybir.AluOpType
AX = mybir.AxisListType

N_DMA_SLOTS = 8
DM = 1024
EPS = 1e-6
NEG = -30000.0


class T:
    __slots__ = ("h", "w", "r", "name")

    def __init__(self, h=None, name=""):
        self.h = h
        self.w = None
        self.r = []
        self.name = name

    def __getitem__(self, k):
        return self.h[k]


class Prog:
    ENG = ("pe", "act", "dve", "pool", "sp")

    def __init__(self, nc):
        self.nc = nc
        self.ops = {e: [] for e in self.ENG}
        self.seq = {e: 0 for e in self.ENG}
        self.known = {e: {} for e in self.ENG}
        self.known_ver = {e: None for e in self.ENG}
        self.dcount = {}
        self.dnext = {e: 0 for e in self.ENG}
        self.last_dma = {}
        self.n_instr = 0

    def _snap(self, eng):
        s = self.known_ver[eng]
        if s is None:
            s = dict(self.known[eng])
            self.known_ver[eng] = s
        return s

    def _learn(self, eng, d):
        kn = self.known[eng]
        ch = False
        for k, v in d.items():
            if kn.get(k, 0) < v:
                kn[k] = v
                ch = True
        if ch:
            self.known_ver[eng] = None

    @staticmethod
    def _dep_events(reads, writes):
        ev = []
        for r in reads:
            if r.w is not None:
                ev.append(r.w)
        for w in writes:
            if w.w is not None:
                ev.append(w.w)
            ev.extend(w.r)
        return ev

    def _waits_for(self, eng, events):
        kn = self.known[eng]
        waits = []
        for ev in sorted(events, key=lambda e: -e[1]):
            sk, val, src, snap = ev
            if src == eng and eng == "pe":
                continue
            if kn.get(sk, 0) >= val:
                continue
            waits.append((sk, val))
            kn[sk] = val
            self.known_ver[eng] = None
            if snap is not None:
                self._learn(eng, snap)
        out = []
        seen = {}
        for sk, val in waits:
            if seen.get(sk, 0) >= val:
                continue
            seen[sk] = val
            out.append((sk, val))
        return out

    def op(self, eng, fn, reads=(), writes=()):
        events = self._dep_events(reads, writes)
        waits = self._waits_for(eng, events)
        self.seq[eng] += 1
        me = (("e", eng), self.seq[eng], eng, self._snap(eng))
        self.ops[eng].append((waits, fn, (("e", eng), 1)))
        for r in reads:
            r.r.append(me)
        for w in writes:
            w.w = me
            w.r = []
        self.n_instr += 1 + max(0, len(waits) - 1)
        return me

    def dma(self, q, fn, reads=(), writes=()):
        events = self._dep_events(reads, writes)
        slot = self.dnext[q] % N_DMA_SLOTS
        self.dnext[q] += 1
        sk = ("d", q, slot)
        cnt = self.dcount.get(sk, 0)
        if cnt > 0:
            events = list(events) + [self.last_dma[sk]]
        waits = self._waits_for(q, events)
        self.dcount[sk] = cnt + 16
        me = (sk, cnt + 16, "dma", self._snap(q))
        self.last_dma[sk] = me
        self.ops[q].append((waits, fn, (sk, 16)))
        for r in reads:
            r.r.append(me)
        for w in writes:
            w.w = me
            w.r = []
        self.n_instr += 1 + max(0, len(waits) - 1)
        return me

    def barrier(self):
        events = []
        for e in self.ENG:
            if self.seq[e] > 0:
                events.append((("e", e), self.seq[e], e, None))
        for sk, ev in self.last_dma.items():
            events.append(ev)
        for e in self.ENG:
            kn = self.known[e]
            waits = []
            for sk, val, src, snap in events:
                if kn.get(sk, 0) >= val:
                    continue
                kn[sk] = val
                waits.append((sk, val))
            self.known_ver[e] = None
            if waits:
                self.ops[e].append((waits, None, None))
                self.n_instr += len(waits)

    def build(self):
        nc = self.nc
        keys = set()
        for e in self.ENG:
            for waits, fn, inc in self.ops[e]:
                for sk, _ in waits:
                    keys.add(sk)
                if inc is not None:
                    keys.add(inc[0])
        keys = sorted(keys, key=str)
        with contextlib.ExitStack() as st:
            semh = {}
            for i, k in enumerate(keys):
                semh[k] = st.enter_context(nc.semaphore("s%d" % i))
            block = st.enter_context(nc.Block())

            def runner(e):
                def run(engh):
                    for waits, fn, inc in self.ops[e]:
                        if fn is None:
                            for sk, val in waits:
                                engh.wait_ge(semh[sk], val)
                            continue
                        for sk, val in waits[1:]:
                            engh.wait_ge(semh[sk], val)
                        ins = fn(engh)
                        if waits:
                            ins._wait_ge(semh[waits[0][0]], waits[0][1])
                        ins.then_inc(semh[inc[0]], inc[1])
                return run

            block.tensor(runner("pe"))
            block.scalar(runner("act"))
            block.vector(runner("dve"))
            block.gpsimd(runner("pool"))
            block.sync(runner("sp"))
        return nc


class Cfg:
    def __init__(self, n_lat=64, layers=(0, 1, 2, 3), depth=4, debug=False, phases="MABCDE", final=True):
        self.n_lat = n_lat
        self.nt = n_lat + 2
        self.layers = tuple(layers)
        self.depth = depth
        self.debug = debug
        self.phases = phases
        self.final = final


def na_patterns(n_lat):
    rows = n_lat * 2
    pats, ids, starts, keymap = [], [], [], {}
    kp = np.arange(128)
    for m in range(n_lat):
        st = int(np.clip(m - 2, 0, n_lat - 5))
        j = np.arange(128)
        r = 2 * m + j // 64
        c = j % 64
        r0 = np.clip(r - 4, 0, rows - 8)
        c0 = np.clip(c - 8, 0, 64 - 16)
        kb = np.arange(5)
        kr = (st * 2 + kb[:, None] * 2 + (kp[None, :] // 64))[:, :, None]
        kc = (kp % 64)[None, :, None] + np.zeros((5, 1, 1), np.int64)
        valid = (kr >= r0[None, None, :]) & (kr < r0[None, None, :] + 8) & (kc >= c0[None, None, :]) & (kc < c0[None, None, :] + 16)
        roff = np.where(valid, kr - r[None, None, :] + 7, 0)
        coff = np.where(valid, kc - c[None, None, :] + 15, 0)
        key = (valid.tobytes(), roff.tobytes(), coff.tobytes())
        if key not in keymap:
            keymap[key] = len(pats)
            pats.append((valid, roff, coff))
        ids.append(keymap[key])
        starts.append(st)
    return ids, starts, pats


class Builder:
    def __init__(self, cfg):
        self.cfg = cfg
        nc = bass.Bass("TRN2", target_bir_lowering=False)
        self.nc = nc
        self.P = Prog(nc)
        self.out_names = []
        self.pat_ids, self.pat_starts, self.pats = na_patterns(cfg.n_lat)
        self.npat = len(self.pats)
        cnt = np.bincount(self.pat_ids)
        self.pat_main = int(np.argmax(cnt))
        self._declare()

    def dram_in(self, name, shape, dt=F32):
        return self.nc.dram_tensor(name, list(shape), dt, kind="ExternalInput").ap()

    def dram_scr(self, name, shape, dt=F32):
        kind = "ExternalOutput" if self.cfg.debug else "Internal"
        if self.cfg.debug:
            self.out_names.append(name)
        return self.nc.dram_tensor(name, list(shape), dt, kind=kind).ap()

    def _declare(self):
        c = self.cfg
        NT, NL = c.nt, c.depth
        TOK = NT * 128
        di = self.dram_in
        self.xin = di("xin", [TOK, DM])
        self.cvec = di("cvec", [128, 8, 2])
        self.w_mod = di("w_mod", [NL, DM, 6 * DM])
        self.b_mod = di("b_mod", [NL, 6 * DM])
        self.g1 = di("g_norm1", [NL, DM])
        self.w_in = di("w_in", [NL, DM, 5152])
        self.waf = di("w_alpha_f", [NL, 16, 256])
        self.baf = di("b_alpha_f", [NL, 256])
        self.wab = di("w_alpha_b", [NL, 16, 256])
        self.bab = di("b_alpha_b", [NL, 256])
        self.ggla = di("g_gla", [NL, 128])
        self.biasT = di("biasT", [NL, self.npat, 128, 8, 5, 128])
        self.w_pg = di("w_proj_gla", [NL, 512, DM])
        self.w_pn = di("w_proj_na", [NL, 512, DM])
        self.w_o = di("w_out", [NL, DM, DM])
        self.g2 = di("g_norm2", [NL, DM])
        self.w_q = di("w_query", [NL, DM, 2048])
        self.skT = di("skT", [NL, 16, 128, 128])
        self.eu = di("expert_u", [NL, 16384, DM])
        self.ev = di("expert_v", [NL, 16384, DM])
        self.gfin = di("g_final", [DM])
        self.rope = di("rope", [c.n_lat, 64, 2, 128])
        self.rotm = di("rotm", [64, 64])
        ds = self.dram_scr
        self.X = ds("X", [TOK, DM])
        self.AUX = ds("AUX", [2, 4, DM])
        self.QKT = ds("QKT", [NT, 64, 8, 128], BF16)
        self.ZT = ds("ZT", [32, TOK])
        self.V = ds("V", [TOK, 512], BF16)
        self.RG = ds("RG", [TOK, 512], BF16)
        self.NQT = ds("NQT", [NT, 128, 4, 128], BF16)
        self.NKT = ds("NKT", [NT, 128, 4, 128], BF16)
        self.NVX = ds("NVX", [TOK, 520], BF16)
        self.G = ds("G", [TOK, 2048], BF16)
        self.OF = ds("OF", [TOK, 512])
        self.OB = ds("OB", [TOK, 512])
        self.NAO = ds("NAO", [TOK, 512], BF16)
        self.UB = ds("UB", [16384, DM], BF16)
        self.VB = ds("VB", [16384, DM], BF16)
        self.Y = self.nc.dram_tensor("Y", [c.n_lat * 128, DM], F32, kind="ExternalOutput").ap()

    def sb(self, st, name, shape, dt=F32):
        self._uid = getattr(self, "_uid", 0) + 1
        name = "%s_u%d" % (name, self._uid)
        h = st.enter_context(self.nc.sbuf_tensor(name, list(shape), dt))
        return T(h, name)

    def ps(self, st, name, shape, dt=F32):
        self._uid = getattr(self, "_uid", 0) + 1
        name = "%s_u%d" % (name, self._uid)
        h = st.enter_context(self.nc.psum_tensor(name, list(shape), dt))
        return T(h, name)

    def consts(self, st):
        P = self.P
        sb = self.sb
        self.identf = sb(st, "identf", [128, 128], F32)
        self.identb = sb(st, "identb", [128, 128], BF16)
        self.ones_row = sb(st, "ones_row", [1, 128], F32)
        self.triF = sb(st, "triF", [128, 128], F32)
        self.triB = sb(st, "triB", [128, 128], F32)
        self.mF = sb(st, "mF", [64, 64], F32)
        self.mB = sb(st, "mB", [64, 64], F32)
        self.rotb = sb(st, "rotb", [64, 64], BF16)
        self.cact = sb(st, "cact", [128, 8, 2], F32)
        idf, idb = self.identf, self.identb
        P.op("pool", lambda e: e.memset(idf[:], 1.0), writes=[idf])
        P.op("pool", lambda e: e.affine_select(out=idf[:], in_=idf[:], pattern=[[-1, 128]], compare_op=ALU.is_equal,
                                               fill=0.0, base=0, channel_multiplier=1), reads=[idf], writes=[idf])
        P.op("pool", lambda e: e.tensor_copy(out=idb[:], in_=idf[:]), reads=[idf], writes=[idb])
        P.op("pool", lambda e: e.memset(self.ones_row[:], 1.0), writes=[self.ones_row])
        v = -1.0 / 16.0
        tF, tB = self.triF, self.triB
        P.op("pool", lambda e: e.memset(tF[:], v), writes=[tF])
        P.op("pool", lambda e: e.affine_select(out=tF[:], in_=tF[:], pattern=[[1, 128]], compare_op=ALU.is_ge,
                                               fill=0.0, base=0, channel_multiplier=-1), reads=[tF], writes=[tF])
        P.op("pool", lambda e: e.memset(tF[0:64, 64:128], 0.0), reads=[tF], writes=[tF])
        P.op("pool", lambda e: e.memset(tB[:], v), writes=[tB])
        P.op("pool", lambda e: e.affine_select(out=tB[:], in_=tB[:], pattern=[[-1, 128]], compare_op=ALU.is_ge,
                                               fill=0.0, base=0, channel_multiplier=1), reads=[tB], writes=[tB])
        P.op("pool", lambda e: e.memset(tB[64:128, 0:64], 0.0), reads=[tB], writes=[tB])
        mF, mB = self.mF, self.mB
        P.op("pool", lambda e: e.memset(mF[:], 1.0), writes=[mF])
        P.op("pool", lambda e: e.affine_select(out=mF[:], in_=mF[:], pattern=[[1, 64]], compare_op=ALU.is_ge,
                                               fill=0.0, base=0, channel_multiplier=-1), reads=[mF], writes=[mF])
        P.op("pool", lambda e: e.memset(mB[:], 1.0), writes=[mB])
        P.op("pool", lambda e: e.affine_select(out=mB[:], in_=mB[:], pattern=[[-1, 64]], compare_op=ALU.is_ge,
                                               fill=0.0, base=0, channel_multiplier=1), reads=[mB], writes=[mB])
        with contextlib.ExitStack() as s2:
            rf = self.sb(s2, "rotf", [64, 64], F32)
            cv = self.sb(s2, "cvt", [128, 8, 2], F32)
            sg = self.sb(s2, "csg", [128, 8, 2], F32)
            P.dma("sp", lambda e: e.dma_start(out=rf[:], in_=self.rotm[:, :]), writes=[rf])
            P.op("dve", lambda e: e.tensor_copy(out=self.rotb[:], in_=rf[:]), reads=[rf], writes=[self.rotb])
            P.dma("sp", lambda e: e.dma_start(out=cv[:], in_=self.cvec[:, :, :]), writes=[cv])
            P.op("act", lambda e: e.activation(out=sg[:], in_=cv[:], func=AF.Sigmoid), reads=[cv], writes=[sg])
            P.op("dve", lambda e: e.tensor_tensor(out=self.cact[:], in0=cv[:], in1=sg[:], op=ALU.mult),
                 reads=[cv, sg], writes=[self.cact])
            P.barrier()

    def phase_mod(self, l, modst):
        P, nc = self.P, self.nc
        self.A1T = self.sb(modst, "A1T", [128, 8, 2])
        self.B1T = self.sb(modst, "B1T", [128, 8, 2])
        with contextlib.ExitStack() as st:
            wm = [self.sb(st, "wm%d" % i, [128, 8, 1024]) for i in range(2)]
            bmT = self.sb(st, "bmT", [128, 48])
            g1T = self.sb(st, "g1T", [128, 8])
            g2T = self.sb(st, "g2T", [128, 8])
            modT = self.sb(st, "modT", [128, 48, 2])
            tmp = self.sb(st, "modtmp", [128, 8, 2])
            A2T = self.sb(st, "A2T", [128, 8, 2])
            mps = self.ps(st, "mod_ps", [128, 48, 2])
            ncd = nc.allow_non_contiguous_dma(reason="tiny per-feature vectors")
            st.enter_context(ncd)
            P.dma("sp", lambda e: e.dma_start(out=bmT[:], in_=self.b_mod[l].rearrange("(c p) -> p c", p=128)), writes=[bmT])
            P.dma("sp", lambda e: e.dma_start(out=g1T[:], in_=self.g1[l].rearrange("(c p) -> p c", p=128)), writes=[g1T])
            P.dma("sp", lambda e: e.dma_start(out=g2T[:], in_=self.g2[l].rearrange("(c p) -> p c", p=128)), writes=[g2T])
            for g in range(6):
                w = wm[g % 2]
                P.dma("sp", lambda e, w=w, g=g: e.dma_start(
                    out=w[:], in_=self.w_mod[l][:, g * 1024:(g + 1) * 1024].rearrange("(k p) n -> p k n", p=128)), writes=[w])
                for b in range(8):
                    blk = g * 8 + b
                    for k in range(8):
                        P.op("pe", lambda e, w=w, b=b, k=k, blk=blk: e.matmul(
                            mps[:, blk, :], lhsT=w[:, k, b * 128:(b + 1) * 128], rhs=self.cact[:, k, :],
                            start=(k == 0), stop=(k == 7)), reads=[w, self.cact], writes=[mps])
            P.op("dve", lambda e: e.tensor_tensor(out=modT[:], in0=mps[:], in1=bmT[:].unsqueeze(2).to_broadcast([128, 48, 2]),
                                                  op=ALU.add), reads=[mps, bmT], writes=[modT])
            P.op("dve", lambda e: e.tensor_scalar(out=tmp[:], in0=modT[:, 8:16, :], scalar1=1.0, scalar2=None, op0=ALU.add),
                 reads=[modT], writes=[tmp])
            P.op("dve", lambda e: e.tensor_tensor(out=self.A1T[:], in0=tmp[:], in1=g1T[:].unsqueeze(2).to_broadcast([128, 8, 2]),
                                                  op=ALU.mult), reads=[tmp, g1T], writes=[self.A1T])
            P.op("dve", lambda e: e.tensor_copy(out=self.B1T[:], in_=modT[:, 0:8, :]), reads=[modT], writes=[self.B1T])
            P.op("dve", lambda e: e.tensor_scalar(out=tmp[:], in0=modT[:, 32:40, :], scalar1=1.0, scalar2=None, op0=ALU.add),
                 reads=[modT], writes=[tmp])
            P.op("dve", lambda e: e.tensor_tensor(out=A2T[:], in0=tmp[:], in1=g2T[:].unsqueeze(2).to_broadcast([128, 8, 2]),
                                                  op=ALU.mult), reads=[tmp, g2T], writes=[A2T])
            srcs = [(A2T, None), (modT, 24), (modT, 16), (modT, 40)]
            for i, (t, off) in enumerate(srcs):
                for r in range(2):
                    if off is None:
                        P.dma("sp", lambda e, t=t, i=i, r=r: e.dma_start(
                            out=self.AUX[r, i, :].rearrange("(k p) -> p k", p=128), in_=t[:, :, r]), reads=[t])
                    else:
                        P.dma("sp", lambda e, t=t, i=i, r=r, off=off: e.dma_start(
                            out=self.AUX[r, i, :].rearrange("(k p) -> p k", p=128), in_=t[:, off:off + 8, r]), reads=[t])
            P.barrier()

    def phase_a(self, l):
        P, nc, c = self.P, self.nc, self.cfg
        NT = c.nt
        with contextlib.ExitStack() as st:
            wb = self.sb(st, "a_wb", [128, 8, 5152], BF16)
            stg = [self.sb(st, "a_stg%d" % i, [128, 2576]) for i in range(2)]
            for k in range(8):
                for hf in range(2):
                    s = stg[hf]
                    cs = slice(hf * 2576, (hf + 1) * 2576)
                    P.dma("sp", lambda e, s=s, k=k, cs=cs: e.dma_start(out=s[:], in_=self.w_in[l][k * 128:(k + 1) * 128, cs]), writes=[s])
                    if hf == 0:
                        P.op("act", lambda e, s=s, k=k, cs=cs: e.copy(out=wb[:, k, cs], in_=s[:]), reads=[s], writes=[wb])
                    else:
                        P.op("pool", lambda e, s=s, k=k, cs=cs: e.tensor_copy(out=wb[:, k, cs], in_=s[:]), reads=[s], writes=[wb])
            xt = [self.sb(st, "a_x%d" % i, [128, DM]) for i in range(2)]
            junk = self.sb(st, "a_junk", [128, DM], BF16)
            ss = [self.sb(st, "a_ss%d" % i, [128, 4]) for i in range(2)]
            xn = [self.sb(st, "a_xn%d" % i, [128, DM]) for i in range(2)]
            hT = [self.sb(st, "a_hT%d" % i, [128, 8, 128], BF16) for i in range(2)]
            vt = [self.sb(st, "a_v%d" % i, [128, 512], BF16) for i in range(2)]
            rt = [self.sb(st, "a_r%d" % i, [128, 512], BF16) for i in range(2)]
            rsg = [self.sb(st, "a_rsg%d" % i, [128, 512]) for i in range(2)]
            nvx = [self.sb(st, "a_nvx%d" % i, [128, 8, 65], BF16) for i in range(2)]
            gt = [self.sb(st, "a_g%d" % i, [128, 2048], BF16) for i in range(2)]
            qkb = [self.sb(st, "a_qkb%d" % i, [64, 8, 128], BF16) for i in range(2)]
            qkr = [self.sb(st, "a_qkr%d" % i, [64, 8, 128], BF16) for i in range(2)]
            t1 = [self.sb(st, "a_t1%d" % i, [64, 8, 128]) for i in range(2)]
            t2 = [self.sb(st, "a_t2%d" % i, [64, 8, 128]) for i in range(2)]
            zt = [self.sb(st, "a_z%d" % i, [32, 128]) for i in range(2)]
            nqt = [self.sb(st, "a_nq%d" % i, [128, 4, 128], BF16) for i in range(2)]
            nkt = [self.sb(st, "a_nk%d" % i, [128, 4, 128], BF16) for i in range(2)]
            rp = [self.sb(st, "a_rp%d" % i, [64, 2, 128]) for i in range(2)]
            tps = self.ps(st, "a_tps", [128, 8, 128])
            tok = [self.ps(st, "a_tok%d" % i, [128, 512]) for i in range(2)]
            fm = [self.ps(st, "a_fm%d" % i, [128, 4, 128]) for i in range(2)]
            rot = self.ps(st, "a_rot", [64, 8, 128])
            for i in range(2):
                P.op("pool", lambda e, i=i: e.memset(nvx[i][:], 1.0), writes=[nvx[i]])
            C_Q, C_K, C_V, C_Z, C_R, C_NQ, C_NK, C_NV, C_G = 0, 256, 512, 1024, 1056, 1568, 2080, 2592, 3104
            ntok = 0
            nfm = 0
            for t in range(NT):
                b = t % 2
                r = 1 if t < 2 else 0
                src = self.xin if l == 0 else self.X
                x_, ss_, xn_, hT_ = xt[b], ss[b], xn[b], hT[b]
                P.dma("sp", lambda e, x_=x_, t=t, src=src: e.dma_start(out=x_[:], in_=src[t * 128:(t + 1) * 128, :]), writes=[x_])
                P.op("act", lambda e, x_=x_, ss_=ss_: e.activation(out=junk[:], in_=x_[:], func=AF.Square, accum_out=ss_[:, 0:1]),
                     reads=[x_], writes=[junk, ss_])
                P.op("dve", lambda e, ss_=ss_: e.tensor_scalar(out=ss_[:, 1:2], in0=ss_[:, 0:1], scalar1=1.0 / DM, scalar2=EPS,
                                                               op0=ALU.mult, op1=ALU.add), reads=[ss_], writes=[ss_])
                P.op("act", lambda e, ss_=ss_: e.activation(out=ss_[:, 2:3], in_=ss_[:, 1:2], func=AF.Sqrt), reads=[ss_], writes=[ss_])
                P.op("dve", lambda e, ss_=ss_: e.reciprocal(out=ss_[:, 3:4], in_=ss_[:, 2:3]), reads=[ss_], writes=[ss_])
                P.op("dve", lambda e, x_=x_, xn_=xn_, ss_=ss_: e.tensor_scalar(out=xn_[:], in0=x_[:], scalar1=ss_[:, 3:4], scalar2=None,
                                                                               op0=ALU.mult), reads=[x_, ss_], writes=[xn_])
                for k in range(8):
                    P.op("pe", lambda e, xn_=xn_, k=k: e.transpose(out=tps[:, k, :], in_=xn_[:, k * 128:(k + 1) * 128],
                                                                   identity=self.identf[:]), reads=[xn_, self.identf], writes=[tps])
                for k in range(8):
                    P.op("act", lambda e, hT_=hT_, k=k, r=r: e.activation(
                        out=hT_[:, k, :], in_=tps[:, k, :], func=AF.Identity, scale=self.A1T[:, k, r:r + 1],
                        bias=self.B1T[:, k, r:r + 1]), reads=[tps, self.A1T, self.B1T], writes=[hT_])
                tm = [("v", C_V), ("r", C_R), ("nv", C_NV), ("g0", C_G), ("g1", C_G + 512), ("g2", C_G + 1024), ("g3", C_G + 1536)]
                for nm, c0 in tm:
                    pt = tok[ntok % 2]
                    ntok += 1
                    for k in range(8):
                        P.op("pe", lambda e, pt=pt, hT_=hT_, k=k, c0=c0: e.matmul(
                            pt[:], lhsT=hT_[:, k, :], rhs=wb[:, k, c0:c0 + 512], start=(k == 0), stop=(k == 7)),
                            reads=[hT_, wb], writes=[pt])
                    if nm == "v":
                        P.op("dve", lambda e, pt=pt, b=b: e.tensor_copy(out=vt[b][:], in_=pt[:]), reads=[pt], writes=[vt[b]])
                    elif nm == "r":
                        P.op("act", lambda e, pt=pt, b=b: e.activation(out=rsg[b][:], in_=pt[:], func=AF.Sigmoid),
                             reads=[pt], writes=[rsg[b]])
                        P.op("dve", lambda e, pt=pt, b=b: e.tensor_tensor(out=rt[b][:], in0=pt[:], in1=rsg[b][:], op=ALU.mult),
                             reads=[pt, rsg[b]], writes=[rt[b]])
                    elif nm == "nv":
                        P.op("dve", lambda e, pt=pt, b=b: e.tensor_copy(
                            out=nvx[b][:, :, 0:64], in_=pt[:].rearrange("p (h d) -> p h d", h=8)), reads=[pt], writes=[nvx[b]])
                    else:
                        gi = int(nm[1])
                        P.op("act", lambda e, pt=pt, b=b, gi=gi: e.activation(
                            out=gt[b][:, gi * 512:(gi + 1) * 512], in_=pt[:], func=AF.Sigmoid), reads=[pt], writes=[gt[b]])
                groups = [("q", [C_Q + i * 64 for i in range(4)]), ("k", [C_K + i * 64 for i in range(4)]), ("z", [C_Z]),
                          ("nq", [C_NQ + i * 128 for i in range(4)]), ("nk", [C_NK + i * 128 for i in range(4)])]
                for nm, cols in groups:
                    pf = fm[nfm % 2]
                    nfm += 1
                    for bi, c0 in enumerate(cols):
                        M = 32 if nm == "z" else (64 if nm in ("q", "k") else 128)
                        for k in range(8):
                            P.op("pe", lambda e, pf=pf, hT_=hT_, k=k, c0=c0, bi=bi, M=M: e.matmul(
                                pf[0:M, bi, :], lhsT=wb[:, k, c0:c0 + M], rhs=hT_[:, k, :], start=(k == 0), stop=(k == 7)),
                                reads=[hT_, wb], writes=[pf])
                    if nm in ("q", "k"):
                        o4 = 0 if nm == "q" else 4
                        P.op("act", lambda e, pf=pf, b=b, o4=o4: e.copy(out=qkb[b][:, o4:o4 + 4, :], in_=pf[0:64, :, :]), reads=[pf], writes=[qkb[b]])
                    elif nm == "z":
                        P.op("dve", lambda e, pf=pf, b=b: e.tensor_copy(out=zt[b][:], in_=pf[0:32, 0, :]), reads=[pf], writes=[zt[b]])
                    elif nm == "nq":
                        P.op("act", lambda e, pf=pf, b=b: e.copy(out=nqt[b][:], in_=pf[:]), reads=[pf], writes=[nqt[b]])
                    else:
                        P.op("dve", lambda e, pf=pf, b=b: e.tensor_copy(out=nkt[b][:], in_=pf[:]), reads=[pf], writes=[nkt[b]])
                if t >= 2:
                    P.dma("sp", lambda e, b=b, t=t: e.dma_start(out=rp[b][:], in_=self.rope[t - 2]), writes=[rp[b]])
                    for bi in range(8):
                        P.op("pe", lambda e, b=b, bi=bi: e.matmul(rot[:, bi, :], lhsT=self.rotb[:], rhs=qkb[b][:, bi, :],
                                                                 start=True, stop=True), reads=[qkb[b], self.rotb], writes=[rot])
                    P.op("dve", lambda e, b=b: e.tensor_tensor(out=t1[b][:], in0=qkb[b][:],
                                                               in1=rp[b][:, 0:1, :].to_broadcast([64, 8, 128]), op=ALU.mult),
                         reads=[qkb[b], rp[b]], writes=[t1[b]])
                    P.op("dve", lambda e, b=b: e.tensor_tensor(out=t2[b][:], in0=rot[:],
                                                               in1=rp[b][:, 1:2, :].to_broadcast([64, 8, 128]), op=ALU.mult),
                         reads=[rot, rp[b]], writes=[t2[b]])
                    P.op("pool", lambda e, b=b: e.tensor_tensor(out=qkr[b][:], in0=t1[b][:], in1=t2[b][:], op=ALU.add),
                         reads=[t1[b], t2[b]], writes=[qkr[b]])
                    qsrc = qkr[b]
                else:
                    qsrc = qkb[b]
                rows = slice(t * 128, (t + 1) * 128)
                P.dma("sp", lambda e, qsrc=qsrc, t=t: e.dma_start(out=self.QKT[t], in_=qsrc[:]), reads=[qsrc])
                P.dma("sp", lambda e, b=b, rows=rows: e.dma_start(out=self.ZT[:, rows], in_=zt[b][:]), reads=[zt[b]])
                P.dma("sp", lambda e, b=b, rows=rows: e.dma_start(out=self.V[rows, :], in_=vt[b][:]), reads=[vt[b]])
                P.dma("sp", lambda e, b=b, rows=rows: e.dma_start(out=self.RG[rows, :], in_=rt[b][:]), reads=[rt[b]])
                P.dma("sp", lambda e, b=b, t=t: e.dma_start(out=self.NQT[t], in_=nqt[b][:]), reads=[nqt[b]])
                P.dma("sp", lambda e, b=b, t=t: e.dma_start(out=self.NKT[t], in_=nkt[b][:]), reads=[nkt[b]])
                P.dma("sp", lambda e, b=b, rows=rows: e.dma_start(out=self.NVX[rows, :], in_=nvx[b][:].rearrange("p h d -> p (h d)")),
                      reads=[nvx[b]])
                P.dma("sp", lambda e, b=b, rows=rows: e.dma_start(out=self.G[rows, :], in_=gt[b][:]), reads=[gt[b]])
            P.barrier()

    def phase_b(self, l):
        P, nc, c = self.P, self.nc, self.cfg
        NT = c.nt
        with contextlib.ExitStack() as st:
            wa = {}
            ba = {}
            for d, (wsrc, bsrc) in (("f", (self.waf, self.baf)), ("b", (self.wab, self.bab))):
                wa[d] = self.sb(st, "b_wa" + d, [16, 256])
                ba[d] = self.sb(st, "b_ba" + d, [1, 256])
                P.dma("sp", lambda e, d=d, wsrc=wsrc: e.dma_start(out=wa[d][:], in_=wsrc[l]), writes=[wa[d]])
                P.dma("sp", lambda e, d=d, bsrc=bsrc: e.dma_start(out=ba[d][:], in_=bsrc[l:l + 1, :]), writes=[ba[d]])
            S32 = {d: self.sb(st, "b_S32" + d, [64, 4, 128]) for d in "fb"}
            Sbf = {d: self.sb(st, "b_Sbf" + d, [64, 4, 128], BF16) for d in "fb"}
            for d in "fb":
                P.op("pool", lambda e, d=d: e.memset(S32[d][:], 0.0), writes=[S32[d]])
                P.op("pool", lambda e, d=d: e.memset(Sbf[d][:], 0.0), writes=[Sbf[d]])
            mk = lambda nm, shp, dt=F32: {d: [self.sb(st, "b_%s%s%d" % (nm, d, i), shp, dt) for i in range(2)] for d in "fb"}
            zt = mk("z", [16, 128])
            qk = mk("qk", [64, 8, 128], BF16)
            vv = mk("v", [64, 2, 512], BF16)
            e1 = mk("e1", [128, 256])
            Lt = mk("L", [128, 256])
            eb = mk("eb", [64, 4, 128])
            enb = mk("enb", [64, 4, 128])
            qe = mk("qe", [64, 4, 128], BF16)
            ke = mk("ke", [64, 4, 128], BF16)
            ktok = mk("ktok", [64, 4, 64], BF16)
            Am = mk("Am", [64, 4, 64], BF16)
            Ot = mk("Ot", [64, 512])
            lb_ps = {d: self.ps(st, "b_lb" + d, [128, 512]) for d in "fb"}
            kT_ps = self.ps(st, "b_kT", [64, 4, 64], BF16)
            A_ps = {d: self.ps(st, "b_A" + d, [64, 4, 64]) for d in "fb"}
            O_ps = {d: self.ps(st, "b_O" + d, [64, 512]) for d in "fb"}
            dS_ps = self.ps(st, "b_dS", [64, 4, 128])
            tri = {"f": self.triF, "b": self.triB}
            msk = {"f": self.mF, "b": self.mB}
            OD = {"f": self.OF, "b": self.OB}
            order_f = list(range(NT))
            order_b = [1, 0] + list(range(NT - 1, 1, -1))
            cnt = {"f": 0, "b": 0}

            def emit(d, t):
                i = cnt[d] % 2
                cnt[d] += 1
                z_, qk_, v_, e1_, L_, eb_, enb_, qe_, ke_ = zt[d][i], qk[d][i], vv[d][i], e1[d][i], Lt[d][i], eb[d][i], enb[d][i], qe[d][i], ke[d][i]
                rows = slice(t * 128, (t + 1) * 128)
                zr = slice(0, 16) if d == "f" else slice(16, 32)
                P.dma("sp", lambda e: e.dma_start(out=z_[:], in_=self.ZT[zr, rows]), writes=[z_])
                P.dma("sp", lambda e: e.dma_start(out=qk_[:], in_=self.QKT[t]), writes=[qk_])
                P.dma("sp", lambda e: e.dma_start(out=v_[:], in_=self.V[rows, :].rearrange("(c p) n -> p c n", p=64)), writes=[v_])
                lp = lb_ps[d]
                la = lp[:, 0:256]
                bp = lp[0:64, :].rearrange("p (h n) -> p h n", h=4)
                P.op("pe", lambda e: e.matmul(la, lhsT=z_[:], rhs=wa[d][:], start=True, stop=False), reads=[z_, wa[d]], writes=[lp])
                P.op("pe", lambda e: e.matmul(la, lhsT=self.ones_row[:], rhs=ba[d][:], start=False, stop=True),
                     reads=[self.ones_row, ba[d]], writes=[lp])
                P.op("act", lambda e: e.activation(out=e1_[:], in_=la, func=AF.Exp, scale=-1.0), reads=[lp], writes=[e1_])
                P.op("act", lambda e: e.activation(out=L_[:], in_=e1_[:], func=AF.Ln, bias=1.0), reads=[e1_], writes=[L_])
                for h in range(4):
                    P.op("pe", lambda e, h=h: e.matmul(bp[:, h, :], lhsT=L_[:, h * 64:(h + 1) * 64], rhs=tri[d][:], start=True, stop=True),
                         reads=[L_, tri[d]], writes=[lp])
                P.op("act", lambda e: e.activation(out=eb_[:], in_=bp, func=AF.Exp), reads=[lp], writes=[eb_])
                P.op("act", lambda e: e.activation(out=enb_[:], in_=bp, func=AF.Exp, scale=-1.0), reads=[lp], writes=[enb_])
                P.op("dve", lambda e: e.scalar_tensor_tensor(out=qe_[:], in0=qk_[:, 0:4, :], scalar=0.125, in1=eb_[:],
                                                             op0=ALU.mult, op1=ALU.mult), reads=[qk_, eb_], writes=[qe_])
                P.op("dve", lambda e: e.tensor_tensor(out=ke_[:], in0=qk_[:, 4:8, :], in1=enb_[:], op=ALU.mult),
                     reads=[qk_, enb_], writes=[ke_])
                def chunk(c_):
                    cs = slice(c_ * 64, (c_ + 1) * 64)
                    kt_, Am_, Ot_ = ktok[d][c_], Am[d][c_], Ot[d][c_]
                    for h in range(4):
                        P.op("pe", lambda e, h=h: e.transpose(out=kT_ps[:, h, :], in_=ke_[:, h, cs], identity=self.identb[0:64, 0:64]),
                             reads=[ke_, self.identb], writes=[kT_ps])
                    P.op("act", lambda e: e.copy(out=kt_[:], in_=kT_ps[:]), reads=[kT_ps], writes=[kt_])
                    for h in range(4):
                        P.op("pe", lambda e, h=h: e.matmul(A_ps[d][:, h, :], lhsT=ke_[:, h, cs], rhs=qe_[:, h, cs], start=True, stop=True),
                             reads=[ke_, qe_], writes=[A_ps[d]])
                    P.op("dve", lambda e: e.tensor_tensor(out=Am_[:], in0=A_ps[d][:],
                                                          in1=msk[d][:].unsqueeze(1).to_broadcast([64, 4, 64]), op=ALU.mult),
                         reads=[A_ps[d], msk[d]], writes=[Am_])
                    for h in range(4):
                        P.op("pe", lambda e, h=h: e.matmul(O_ps[d][:, h * 128:(h + 1) * 128], lhsT=Am_[:, h, :],
                                                          rhs=v_[:, c_, h * 128:(h + 1) * 128], start=True, stop=False),
                             reads=[Am_, v_], writes=[O_ps[d]])
                        P.op("pe", lambda e, h=h: e.matmul(O_ps[d][:, h * 128:(h + 1) * 128], lhsT=qe_[:, h, cs], rhs=Sbf[d][:, h, :],
                                                          start=False, stop=True), reads=[qe_, Sbf[d]], writes=[O_ps[d]])
                    for h in range(4):
                        P.op("pe", lambda e, h=h: e.matmul(dS_ps[:, h, :], lhsT=kt_[:, h, :], rhs=v_[:, c_, h * 128:(h + 1) * 128],
                                                          start=True, stop=True), reads=[kt_, v_], writes=[dS_ps])
                    P.op("act", lambda e: e.copy(out=Ot_[:], in_=O_ps[d][:]), reads=[O_ps[d]], writes=[Ot_])
                    r0 = t * 128 + c_ * 64
                    P.dma("sp", lambda e: e.dma_start(out=OD[d][r0:r0 + 64, :], in_=Ot_[:]), reads=[Ot_])
                    P.op("dve", lambda e: e.tensor_tensor(out=S32[d][:], in0=S32[d][:], in1=dS_ps[:], op=ALU.add),
                         reads=[S32[d], dS_ps], writes=[S32[d]])
                    tcol = c_ * 64 + 63 if d == "f" else c_ * 64
                    P.op("dve", lambda e, tcol=tcol: e.tensor_tensor(
                        out=S32[d][:], in0=S32[d][:], in1=eb_[:, :, tcol:tcol + 1].to_broadcast([64, 4, 128]), op=ALU.mult),
                        reads=[S32[d], eb_], writes=[S32[d]])
                    P.op("act", lambda e: e.copy(out=Sbf[d][:], in_=S32[d][:]), reads=[S32[d]], writes=[Sbf[d]])

                for c_ in ((0, 1) if d == "f" else (1, 0)):
                    chunk(c_)

            for s_ in range(NT):
                emit("f", order_f[s_])
                emit("b", order_b[s_])
            P.barrier()

    def phase_c(self, l, with_ctx):
        P, nc, c = self.P, self.nc, self.cfg
        with contextlib.ExitStack() as st:
            st.enter_context(nc.allow_non_contiguous_dma(reason="tile-blocked K layout"))
            kTc = self.sb(st, "c_kTc", [128, 2, 4, 128], BF16)
            vxc = self.sb(st, "c_vxc", [128, 2, 520], BF16)
            biasM = self.sb(st, "c_biasM", [128, 8, 5, 128])
            biasE = self.sb(st, "c_biasE", [128, 8, 5, 128])
            P.dma("sp", lambda e: e.dma_start(out=kTc[:], in_=self.NKT[0:2].rearrange("t p b k -> p t b k")), writes=[kTc])
            P.dma("sp", lambda e: e.dma_start(out=vxc[:], in_=self.NVX[0:256, :].rearrange("(t p) d -> p t d", p=128)), writes=[vxc])
            P.dma("sp", lambda e: e.dma_start(out=biasM[:], in_=self.biasT[l, self.pat_main]), writes=[biasM])
            qT = [self.sb(st, "c_qT%d" % i, [128, 4, 128], BF16) for i in range(2)]
            kT = [self.sb(st, "c_kT%d" % i, [128, 5, 4, 128], BF16) for i in range(2)]
            vx = [self.sb(st, "c_vx%d" % i, [128, 5, 520], BF16) for i in range(2)]
            Tt = [self.sb(st, "c_T%d" % i, [128, 5, 128]) for i in range(2)]
            PT = [self.sb(st, "c_PT%d" % i, [128, 7, 128], BF16) for i in range(2)]
            rec = [self.sb(st, "c_rec%d" % i, [128, 8]) for i in range(2)]
            ot = [self.sb(st, "c_o%d" % i, [128, 8, 64], BF16) for i in range(2)]
            S_ps = [self.ps(st, "c_S%d" % i, [128, 8, 128]) for i in range(2)]
            O_ps = [[self.ps(st, "c_O%d%d" % (i, j), [128, 4, 65]) for j in range(2)] for i in range(2)]
            tiles = ([0, 1] if with_ctx else []) + list(range(2, c.nt))
            cur_edge = None
            nh = 0
            for n_, t in enumerate(tiles):
                b = n_ % 2
                win = t >= 2
                P.dma("sp", lambda e, b=b, t=t: e.dma_start(out=qT[b][:], in_=self.NQT[t]), writes=[qT[b]])
                if win:
                    m = t - 2
                    s0 = 2 + self.pat_starts[m]
                    P.dma("sp", lambda e, b=b, s0=s0: e.dma_start(out=kT[b][:], in_=self.NKT[s0:s0 + 5].rearrange("t p b k -> p t b k")),
                          writes=[kT[b]])
                    P.dma("sp", lambda e, b=b, s0=s0: e.dma_start(
                        out=vx[b][:], in_=self.NVX[s0 * 128:(s0 + 5) * 128, :].rearrange("(t p) d -> p t d", p=128)), writes=[vx[b]])
                    pid = self.pat_ids[m]
                    if pid == self.pat_main:
                        bias = biasM
                    else:
                        if cur_edge != pid:
                            P.dma("sp", lambda e, pid=pid: e.dma_start(out=biasE[:], in_=self.biasT[l, pid]), writes=[biasE])
                            cur_edge = pid
                        bias = biasE
                for h in range(8):
                    blk, po = h // 2, (h % 2) * 64
                    sp_ = S_ps[nh % 2]
                    T_, PT_ = Tt[nh % 2], PT[nh % 2]
                    nh += 1
                    if win:
                        for kb in range(5):
                            P.op("pe", lambda e, kb=kb, b=b, blk=blk, po=po, sp_=sp_: e.matmul(
                                sp_[:, kb, :], lhsT=kT[b][po:po + 64, kb, blk, :], rhs=qT[b][po:po + 64, blk, :], start=True, stop=True),
                                reads=[kT[b], qT[b]], writes=[sp_])
                    for cb in range(2):
                        P.op("pe", lambda e, cb=cb, b=b, blk=blk, po=po, sp_=sp_: e.matmul(
                            sp_[:, 5 + cb, :], lhsT=kTc[po:po + 64, cb, blk, :], rhs=qT[b][po:po + 64, blk, :], start=True, stop=True),
                            reads=[kTc, qT[b]], writes=[sp_])
                    if win:
                        P.op("dve", lambda e, sp_=sp_, T_=T_, h=h, bias=bias: e.scalar_tensor_tensor(
                            out=T_[:], in0=sp_[:, 0:5, :], scalar=0.125, in1=bias[:, h, :, :], op0=ALU.mult, op1=ALU.add),
                            reads=[sp_, bias], writes=[T_])
                        P.op("act", lambda e, T_=T_, PT_=PT_: e.activation(out=PT_[:, 0:5, :], in_=T_[:], func=AF.Exp),
                             reads=[T_], writes=[PT_])
                    P.op("act", lambda e, sp_=sp_, PT_=PT_: e.activation(out=PT_[:, 5:7, :], in_=sp_[:, 5:7, :], func=AF.Exp, scale=0.125),
                         reads=[sp_], writes=[PT_])
                    op_ = O_ps[b][h // 4]
                    hl = h % 4
                    if win:
                        for kb in range(5):
                            P.op("pe", lambda e, kb=kb, b=b, h=h, op_=op_, hl=hl, PT_=PT_: e.matmul(
                                op_[:, hl, :], lhsT=PT_[:, kb, :], rhs=vx[b][:, kb, h * 65:(h + 1) * 65], start=(kb == 0), stop=False),
                                reads=[PT_, vx[b]], writes=[op_])
                    for cb in range(2):
                        P.op("pe", lambda e, cb=cb, h=h, op_=op_, hl=hl, PT_=PT_, win=win: e.matmul(
                            op_[:, hl, :], lhsT=PT_[:, 5 + cb, :], rhs=vxc[:, cb, h * 65:(h + 1) * 65],
                            start=(cb == 0 and not win), stop=(cb == 1)), reads=[PT_, vxc], writes=[op_])
                for j in range(2):
                    op_ = O_ps[b][j]
                    P.op("dve", lambda e, b=b, j=j, op_=op_: e.reciprocal(out=rec[b][:, j * 4:(j + 1) * 4], in_=op_[:, :, 64]),
                         reads=[op_], writes=[rec[b]])
                    P.op("dve", lambda e, b=b, j=j, op_=op_: e.tensor_tensor(
                        out=ot[b][:, j * 4:(j + 1) * 4, :], in0=op_[:, :, 0:64],
                        in1=rec[b][:, j * 4:(j + 1) * 4].unsqueeze(2).to_broadcast([128, 4, 64]), op=ALU.mult),
                        reads=[op_, rec[b]], writes=[ot[b]])
                P.dma("sp", lambda e, b=b, t=t: e.dma_start(out=self.NAO[t * 128:(t + 1) * 128, :],
                                                            in_=ot[b][:].rearrange("p h d -> p (h d)")), reads=[ot[b]])
            P.barrier()

    def phase_d(self, l, with_ctx):
        P, nc, c = self.P, self.nc, self.cfg
        with contextlib.ExitStack() as st:
            wpg = self.sb(st, "d_wpg", [128, 4, DM], BF16)
            wpn = self.sb(st, "d_wpn", [128, 4, DM], BF16)
            wo = self.sb(st, "d_wo", [128, 8, DM], BF16)
            stg = [self.sb(st, "d_stg%d" % i, [128, DM]) for i in range(2)]
            n = 0
            for (dst, src, nk) in ((wpg, self.w_pg, 4), (wpn, self.w_pn, 4), (wo, self.w_o, 8)):
                for k in range(nk):
                    s = stg[n % 2]
                    P.dma("sp", lambda e, s=s, k=k, src=src: e.dma_start(out=s[:], in_=src[l][k * 128:(k + 1) * 128, :]), writes=[s])
                    if n % 2 == 0:
                        P.op("act", lambda e, s=s, k=k, dst=dst: e.copy(out=dst[:, k, :], in_=s[:]), reads=[s], writes=[dst])
                    else:
                        P.op("pool", lambda e, s=s, k=k, dst=dst: e.tensor_copy(out=dst[:, k, :], in_=s[:]), reads=[s], writes=[dst])
                    n += 1
            gg = self.sb(st, "d_gg", [128, 128])
            ga1 = [self.sb(st, "d_ga1_%d" % r, [128, DM]) for r in range(2)]
            P.dma("sp", lambda e: e.dma_start(out=gg[:], in_=self.ggla[l:l + 1, :].partition_broadcast(128)), writes=[gg])
            for r in range(2):
                P.dma("sp", lambda e, r=r: e.dma_start(out=ga1[r][:], in_=self.AUX[r, 2:3, :].partition_broadcast(128)), writes=[ga1[r]])
            of = [self.sb(st, "d_of%d" % i, [128, 4, 128]) for i in range(2)]
            ob = [self.sb(st, "d_ob%d" % i, [128, 4, 128]) for i in range(2)]
            rg = [self.sb(st, "d_rg%d" % i, [128, 512], BF16) for i in range(2)]
            nao = [self.sb(st, "d_nao%d" % i, [128, 512], BF16) for i in range(2)]
            gt = [self.sb(st, "d_g%d" % i, [128, 2048], BF16) for i in range(2)]
            xt = [self.sb(st, "d_x%d" % i, [128, DM]) for i in range(2)]
            junk = self.sb(st, "d_junk", [128, 128], BF16)
            ss = [self.sb(st, "d_ss%d" % i, [128, 4, 4]) for i in range(2)]
            onb = self.sb(st, "d_onb", [128, 512], BF16)
            onT = self.sb(st, "d_onT", [128, 4, 128], BF16)
            naT = self.sb(st, "d_naT", [128, 4, 128], BF16)
            m1 = self.sb(st, "d_m1", [128, DM])
            m2 = self.sb(st, "d_m2", [128, DM])
            mb = self.sb(st, "d_mb", [128, DM], BF16)
            mT = self.sb(st, "d_mT", [128, 8, 128], BF16)
            tpA = self.ps(st, "d_tpA", [128, 8, 128], BF16)
            tpB = self.ps(st, "d_tpB", [128, 8, 128], BF16)
            ya = self.ps(st, "d_ya", [128, DM])
            yb = self.ps(st, "d_yb", [128, DM])
            yy = self.ps(st, "d_yy", [128, DM])
            tiles = ([0, 1] if with_ctx else []) + list(range(2, c.nt))
            for n_, t in enumerate(tiles):
                b = n_ % 2
                r = 1 if t < 2 else 0
                rows = slice(t * 128, (t + 1) * 128)
                of_, ob_, rg_, nao_, g_, x_, ss_ = of[b], ob[b], rg[b], nao[b], gt[b], xt[b], ss[b]
                P.dma("sp", lambda e, of_=of_, rows=rows: e.dma_start(out=of_[:].rearrange("p h d -> p (h d)"), in_=self.OF[rows, :]), writes=[of_])
                P.dma("sp", lambda e, ob_=ob_, rows=rows: e.dma_start(out=ob_[:].rearrange("p h d -> p (h d)"), in_=self.OB[rows, :]), writes=[ob_])
                P.dma("sp", lambda e, rg_=rg_, rows=rows: e.dma_start(out=rg_[:], in_=self.RG[rows, :]), writes=[rg_])
                P.dma("sp", lambda e, nao_=nao_, rows=rows: e.dma_start(out=nao_[:], in_=self.NAO[rows, :]), writes=[nao_])
                P.dma("sp", lambda e, g_=g_, rows=rows: e.dma_start(out=g_[:], in_=self.G[rows, :]), writes=[g_])
                P.dma("sp", lambda e, x_=x_, rows=rows, l=l: e.dma_start(out=x_[:], in_=(self.xin if l == 0 else self.X)[rows, :]), writes=[x_])
                P.op("dve", lambda e, of_=of_, ob_=ob_: e.tensor_tensor(out=of_[:], in0=of_[:], in1=ob_[:], op=ALU.add),
                     reads=[of_, ob_], writes=[of_])
                for h in range(4):
                    P.op("act", lambda e, of_=of_, ss_=ss_, h=h: e.activation(out=junk[:], in_=of_[:, h, :], func=AF.Square,
                                                                             accum_out=ss_[:, 0, h:h + 1]), reads=[of_], writes=[junk, ss_])
                P.op("dve", lambda e, ss_=ss_: e.tensor_scalar(out=ss_[:, 1, :], in0=ss_[:, 0, :], scalar1=1.0 / 128, scalar2=EPS,
                                                               op0=ALU.mult, op1=ALU.add), reads=[ss_], writes=[ss_])
                P.op("act", lambda e, ss_=ss_: e.activation(out=ss_[:, 2, :], in_=ss_[:, 1, :], func=AF.Sqrt), reads=[ss_], writes=[ss_])
                P.op("dve", lambda e, ss_=ss_: e.reciprocal(out=ss_[:, 3, :], in_=ss_[:, 2, :]), reads=[ss_], writes=[ss_])
                P.op("dve", lambda e, of_=of_, ss_=ss_: e.tensor_tensor(
                    out=of_[:], in0=of_[:], in1=ss_[:, 3, :].unsqueeze(2).to_broadcast([128, 4, 128]), op=ALU.mult),
                    reads=[of_, ss_], writes=[of_])
                P.op("pool", lambda e, of_=of_: e.tensor_tensor(
                    out=of_[:], in0=of_[:], in1=gg[:].unsqueeze(1).to_broadcast([128, 4, 128]), op=ALU.mult),
                    reads=[of_, gg], writes=[of_])
                P.op("dve", lambda e, of_=of_, rg_=rg_: e.tensor_tensor(out=onb[:], in0=of_[:].rearrange("p h d -> p (h d)"),
                                                                      in1=rg_[:], op=ALU.mult), reads=[of_, rg_], writes=[onb])
                for k in range(4):
                    P.op("pe", lambda e, k=k: e.transpose(out=tpA[:, k, :], in_=onb[:, k * 128:(k + 1) * 128], identity=self.identb[:]),
                         reads=[onb, self.identb], writes=[tpA])
                for k in range(4):
                    P.op("pe", lambda e, k=k, nao_=nao_: e.transpose(out=tpA[:, 4 + k, :], in_=nao_[:, k * 128:(k + 1) * 128],
                                                                    identity=self.identb[:]), reads=[nao_, self.identb], writes=[tpA])
                P.op("act", lambda e: e.copy(out=onT[:], in_=tpA[:, 0:4, :]), reads=[tpA], writes=[onT])
                P.op("act", lambda e: e.copy(out=naT[:], in_=tpA[:, 4:8, :]), reads=[tpA], writes=[naT])
                for (yp, xT, w) in ((ya, onT, wpg), (yb, naT, wpn)):
                    for cc in range(2):
                        for k in range(4):
                            P.op("pe", lambda e, yp=yp, xT=xT, w=w, cc=cc, k=k: e.matmul(
                                yp[:, cc * 512:(cc + 1) * 512], lhsT=xT[:, k, :], rhs=w[:, k, cc * 512:(cc + 1) * 512],
                                start=(k == 0), stop=(k == 3)), reads=[xT, w], writes=[yp])
                P.op("dve", lambda e, g_=g_: e.tensor_tensor(out=m1[:], in0=ya[:], in1=g_[:, 0:DM], op=ALU.mult),
                     reads=[ya, g_], writes=[m1])
                P.op("dve", lambda e, g_=g_: e.tensor_tensor(out=m2[:], in0=yb[:], in1=g_[:, DM:2 * DM], op=ALU.mult),
                     reads=[yb, g_], writes=[m2])
                P.op("pool", lambda e: e.tensor_tensor(out=mb[:], in0=m1[:], in1=m2[:], op=ALU.add), reads=[m1, m2], writes=[mb])
                for k in range(8):
                    P.op("pe", lambda e, k=k: e.transpose(out=tpB[:, k, :], in_=mb[:, k * 128:(k + 1) * 128], identity=self.identb[:]),
                         reads=[mb, self.identb], writes=[tpB])
                P.op("act", lambda e: e.copy(out=mT[:], in_=tpB[:]), reads=[tpB], writes=[mT])
                for cc in range(2):
                    for k in range(8):
                        P.op("pe", lambda e, cc=cc, k=k: e.matmul(yy[:, cc * 512:(cc + 1) * 512], lhsT=mT[:, k, :],
                                                                 rhs=wo[:, k, cc * 512:(cc + 1) * 512], start=(k == 0), stop=(k == 7)),
                             reads=[mT, wo], writes=[yy])
                P.op("dve", lambda e, r=r: e.tensor_tensor(out=m1[:], in0=yy[:], in1=ga1[r][:], op=ALU.mult),
                     reads=[yy, ga1[r]], writes=[m1])
                P.op("pool", lambda e, x_=x_: e.tensor_tensor(out=x_[:], in0=x_[:], in1=m1[:], op=ALU.add), reads=[x_, m1], writes=[x_])
                P.dma("sp", lambda e, x_=x_, rows=rows: e.dma_start(out=self.X[rows, :], in_=x_[:]), reads=[x_])
            P.barrier()

    def phase_e(self, l, with_ctx, final):
        P, nc, c = self.P, self.nc, self.cfg
        NB = 4
        with contextlib.ExitStack() as st:
            f32t = [self.sb(st, "e_cf%d" % i, [128, 8, DM]) for i in range(2)]
            b16t = [self.sb(st, "e_cb%d" % i, [128, 8, DM], BF16) for i in range(2)]
            n = 0
            for (src, dst) in ((self.eu, self.UB), (self.ev, self.VB)):
                for i in range(16):
                    a, bb = f32t[n % 2], b16t[n % 2]
                    rs = slice(i * 1024, (i + 1) * 1024)
                    P.dma("sp", lambda e, a=a, src=src, rs=rs: e.dma_start(out=a[:], in_=src[l][rs, :].rearrange("(p r) d -> p r d", r=8)),
                          writes=[a])
                    if n % 2 == 0:
                        P.op("act", lambda e, a=a, bb=bb: e.copy(out=bb[:], in_=a[:]), reads=[a], writes=[bb])
                    else:
                        P.op("pool", lambda e, a=a, bb=bb: e.tensor_copy(out=bb[:], in_=a[:]), reads=[a], writes=[bb])
                    P.dma("sp", lambda e, bb=bb, dst=dst, rs=rs: e.dma_start(out=dst[rs, :].rearrange("(p r) d -> p r d", r=8), in_=bb[:]),
                          reads=[bb])
                    n += 1
            P.barrier()
        with contextlib.ExitStack() as st:
            st.enter_context(nc.allow_non_contiguous_dma(reason="sub-key layout"))
            wq = self.sb(st, "e_wq", [128, 8, 2048], BF16)
            skb = self.sb(st, "e_skb", [128, 16, 128], BF16)
            stg = [self.sb(st, "e_stg%d" % i, [128, 2048]) for i in range(2)]
            for k in range(8):
                s = stg[k % 2]
                P.dma("sp", lambda e, s=s, k=k: e.dma_start(out=s[:], in_=self.w_q[l][k * 128:(k + 1) * 128, :]), writes=[s])
                if k % 2 == 0:
                    P.op("act", lambda e, s=s, k=k: e.copy(out=wq[:, k, :], in_=s[:]), reads=[s], writes=[wq])
                else:
                    P.op("pool", lambda e, s=s, k=k: e.tensor_copy(out=wq[:, k, :], in_=s[:]), reads=[s], writes=[wq])
            s = stg[0]
            P.dma("sp", lambda e, s=s: e.dma_start(out=s[:].rearrange("p (g n) -> p g n", g=16), in_=self.skT[l].rearrange("g d n -> d g n")),
                  writes=[s])
            P.op("act", lambda e, s=s: e.copy(out=skb[:], in_=s[:].rearrange("p (g n) -> p g n", g=16)), reads=[s], writes=[skb])
            A2 = self.sb(st, "e_A2", [128, DM])
            B2 = self.sb(st, "e_B2", [128, DM])
            GA2 = self.sb(st, "e_GA2", [128, DM])
            gfin = self.sb(st, "e_gfin", [128, DM])
            if final:
                P.dma("sp", lambda e: e.dma_start(out=gfin[:], in_=self.gfin.rearrange("(o d) -> o d", o=1).partition_broadcast(128)),
                      writes=[gfin])
            xt = [self.sb(st, "e_x%d" % i, [128, DM]) for i in range(2)]
            ss = [self.sb(st, "e_ss%d" % i, [128, 8]) for i in range(2)]
            h2 = self.sb(st, "e_h2", [128, DM])
            junkb = self.sb(st, "e_junkb", [128, DM], BF16)
            h2T = self.sb(st, "e_h2T", [128, 8, 128], BF16)
            qpT = self.sb(st, "e_qpT", [128, 16, 128], BF16)
            sc = self.sb(st, "e_sc", [128, 16, 128])
            s8 = self.sb(st, "e_s8", [128, 16, 16])
            i8 = self.sb(st, "e_i8", [128, 16, 16], U32)
            i8f = self.sb(st, "e_i8f", [128, 16, 16])
            i0s = self.sb(st, "e_i0s", [128, 8, 16])
            cand = self.sb(st, "e_cand", [128, 8, 16, 16])
            candw = self.sb(st, "e_candw", [128, 8, 16, 16])
            cidx = self.sb(st, "e_cidx", [128, 8, 16, 16])
            j256 = self.sb(st, "e_j256", [128, 256])
            best = self.sb(st, "e_best", [128, 8, 16])
            eidf = self.sb(st, "e_eidf", [128, 128])
            eidx = self.sb(st, "e_eidx", [128, 128], I32)
            ex = self.sb(st, "e_ex", [128, 8, 16])
            sm = self.sb(st, "e_sm", [128, 8, 2])
            gg = self.sb(st, "e_g", [128, 128])
            act = self.sb(st, "e_act", [128, 128])
            ta = self.sb(st, "e_ta", [128, 128])
            tb = self.sb(st, "e_tb", [128, 128])
            wgt = self.sb(st, "e_w", [128, 128])
            Dm = self.sb(st, "e_Dm", [128, 128, 128], BF16)
            ring = [self.sb(st, "e_ring%d" % i, [128, DM], BF16) for i in range(NB)]
            pa = self.ps(st, "e_pa", [128, 8, 128])
            qp_ps = [self.ps(st, "e_qp%d" % i, [128, 4, 128]) for i in range(2)]
            sc_ps = self.ps(st, "e_scps", [128, 16, 128])
            tiles = ([0, 1] if with_ctx else []) + list(range(2, c.nt))
            cur_r = None
            nring = 0
            for n_, t in enumerate(tiles):
                b = n_ % 2
                r = 1 if t < 2 else 0
                rows = slice(t * 128, (t + 1) * 128)
                if cur_r != r:
                    for i_, dst in ((0, A2), (1, B2), (3, GA2)):
                        P.dma("sp", lambda e, i_=i_, dst=dst, r=r: e.dma_start(out=dst[:], in_=self.AUX[r, i_:i_ + 1, :].partition_broadcast(128)),
                              writes=[dst])
                    cur_r = r
                x_, ss_ = xt[b], ss[b]
                P.dma("sp", lambda e, x_=x_, rows=rows: e.dma_start(out=x_[:], in_=self.X[rows, :]), writes=[x_])
                P.op("act", lambda e, x_=x_, ss_=ss_: e.activation(out=junkb[:], in_=x_[:], func=AF.Square, accum_out=ss_[:, 0:1]),
                     reads=[x_], writes=[junkb, ss_])
                P.op("dve", lambda e, ss_=ss_: e.tensor_scalar(out=ss_[:, 1:2], in0=ss_[:, 0:1], scalar1=1.0 / DM, scalar2=EPS,
                                                               op0=ALU.mult, op1=ALU.add), reads=[ss_], writes=[ss_])
                P.op("act", lambda e, ss_=ss_: e.activation(out=ss_[:, 2:3], in_=ss_[:, 1:2], func=AF.Sqrt), reads=[ss_], writes=[ss_])
                P.op("dve", lambda e, ss_=ss_: e.reciprocal(out=ss_[:, 3:4], in_=ss_[:, 2:3]), reads=[ss_], writes=[ss_])
                P.op("dve", lambda e, x_=x_, ss_=ss_: e.scalar_tensor_tensor(out=h2[:], in0=x_[:], scalar=ss_[:, 3:4], in1=A2[:],
                                                                             op0=ALU.mult, op1=ALU.mult), reads=[x_, ss_, A2], writes=[h2])
                P.op("pool", lambda e: e.tensor_tensor(out=h2[:], in0=h2[:], in1=B2[:], op=ALU.add), reads=[h2, B2], writes=[h2])
                for k in range(8):
                    P.op("pe", lambda e, k=k: e.transpose(out=pa[:, k, :], in_=h2[:, k * 128:(k + 1) * 128], identity=self.identf[:]),
                         reads=[h2, self.identf], writes=[pa])
                P.op("act", lambda e: e.copy(out=h2T[:, 0:4, :], in_=pa[:, 0:4, :]), reads=[pa], writes=[h2T])
                P.op("act", lambda e: e.copy(out=h2T[:, 4:8, :], in_=pa[:, 4:8, :]), reads=[pa], writes=[h2T])
                for q4 in range(4):
                    pq = qp_ps[q4 % 2]
                    for bi in range(4):
                        blk = q4 * 4 + bi
                        for k in range(8):
                            P.op("pe", lambda e, pq=pq, bi=bi, blk=blk, k=k: e.matmul(
                                pq[:, bi, :], lhsT=wq[:, k, blk * 128:(blk + 1) * 128], rhs=h2T[:, k, :], start=(k == 0), stop=(k == 7)),
                                reads=[wq, h2T], writes=[pq])
                    if q4 % 2 == 0:
                        P.op("act", lambda e, pq=pq, q4=q4: e.copy(out=qpT[:, q4 * 4:(q4 + 1) * 4, :], in_=pq[:]), reads=[pq], writes=[qpT])
                    else:
                        P.op("dve", lambda e, pq=pq, q4=q4: e.tensor_copy(out=qpT[:, q4 * 4:(q4 + 1) * 4, :], in_=pq[:]), reads=[pq], writes=[qpT])
                for g in range(16):
                    P.op("pe", lambda e, g=g: e.matmul(sc_ps[:, g, :], lhsT=qpT[:, g, :], rhs=skb[:, g, :], start=True, stop=True),
                         reads=[qpT, skb], writes=[sc_ps])
                for q4 in range(4):
                    P.op("act", lambda e, q4=q4: e.copy(out=sc[:, q4 * 4:(q4 + 1) * 4, :], in_=sc_ps[:, q4 * 4:(q4 + 1) * 4, :]),
                         reads=[sc_ps], writes=[sc])
                for g in range(16):
                    P.op("dve", lambda e, g=g: e.max(out=s8[:, g, 0:8], in_=sc[:, g, :]), reads=[sc], writes=[s8])
                    P.op("dve", lambda e, g=g: e.max_index(out=i8[:, g, 0:8], in_max=s8[:, g, 0:8], in_values=sc[:, g, :]),
                         reads=[sc, s8], writes=[i8])
                    P.op("dve", lambda e, g=g: e.match_replace(out=sc[:, g, :], in_to_replace=s8[:, g, 0:8], in_values=sc[:, g, :],
                                                               imm_value=-1e30), reads=[sc, s8], writes=[sc])
                    P.op("dve", lambda e, g=g: e.max(out=s8[:, g, 8:16], in_=sc[:, g, :]), reads=[sc], writes=[s8])
                    P.op("dve", lambda e, g=g: e.max_index(out=i8[:, g, 8:16], in_max=s8[:, g, 8:16], in_values=sc[:, g, :]),
                         reads=[sc, s8], writes=[i8])
                P.op("dve", lambda e: e.tensor_copy(out=i8f[:], in_=i8[:]), reads=[i8], writes=[i8f])
                s8v = s8[:].rearrange("p (h two) k -> p h two k", two=2)
                i8v = i8f[:].rearrange("p (h two) k -> p h two k", two=2)
                P.op("dve", lambda e: e.tensor_tensor(out=cand[:], in0=s8v[:, :, 0, :].unsqueeze(3).to_broadcast([128, 8, 16, 16]),
                                                      in1=s8v[:, :, 1, :].unsqueeze(2).to_broadcast([128, 8, 16, 16]), op=ALU.add),
                     reads=[s8], writes=[cand])
                P.op("dve", lambda e: e.tensor_scalar(out=i0s[:], in0=i8v[:, :, 0, :], scalar1=128.0, scalar2=None, op0=ALU.mult),
                     reads=[i8f], writes=[i0s])
                P.op("dve", lambda e: e.tensor_tensor(out=cidx[:], in0=i0s[:].unsqueeze(3).to_broadcast([128, 8, 16, 16]),
                                                      in1=i8v[:, :, 1, :].unsqueeze(2).to_broadcast([128, 8, 16, 16]), op=ALU.add),
                     reads=[i0s, i8f], writes=[cidx])
                cand3 = cand[:].rearrange("p h a b -> p h (a b)")
                candw3 = candw[:].rearrange("p h a b -> p h (a b)")
                cidx3 = cidx[:].rearrange("p h a b -> p h (a b)")
                for h in range(8):
                    P.op("dve", lambda e, h=h: e.max(out=best[:, h, 0:8], in_=cand3[:, h, :]), reads=[cand], writes=[best])
                    P.op("dve", lambda e, h=h: e.match_replace(out=candw3[:, h, :], in_to_replace=best[:, h, 0:8], in_values=cand3[:, h, :],
                                                               imm_value=-1e30), reads=[cand, best], writes=[candw])
                    P.op("dve", lambda e, h=h: e.max(out=best[:, h, 8:16], in_=candw3[:, h, :]), reads=[candw], writes=[best])
                for h in range(8):
                    for k in range(16):
                        P.op("dve", lambda e, h=h, k=k: e.scalar_tensor_tensor(
                            out=j256[:], in0=cand3[:, h, :], scalar=best[:, h, k:k + 1], in1=cidx3[:, h, :], op0=ALU.is_equal, op1=ALU.mult,
                            accum_out=eidf[:, h * 16 + k:h * 16 + k + 1]), reads=[cand, best, cidx], writes=[j256, eidf])
                P.op("dve", lambda e: e.tensor_scalar(out=eidf[:], in0=eidf[:], scalar1=16383.0, scalar2=0.0, op0=ALU.min, op1=ALU.max),
                     reads=[eidf], writes=[eidf])
                P.op("dve", lambda e: e.tensor_copy(out=eidx[:], in_=eidf[:]), reads=[eidf], writes=[eidx])
                P.op("dve", lambda e: e.tensor_tensor(out=ex[:], in0=best[:], in1=best[:, :, 0:1].to_broadcast([128, 8, 16]), op=ALU.subtract),
                     reads=[best], writes=[ex])
                P.op("act", lambda e: e.activation(out=ex[:], in_=ex[:], func=AF.Exp), reads=[ex], writes=[ex])
                P.op("dve", lambda e: e.tensor_reduce(out=sm[:, :, 0], in_=ex[:], axis=AX.X, op=ALU.add), reads=[ex], writes=[sm])
                P.op("dve", lambda e: e.reciprocal(out=sm[:, :, 1], in_=sm[:, :, 0]), reads=[sm], writes=[sm])
                P.op("dve", lambda e: e.tensor_tensor(out=gg[:].rearrange("p (h k) -> p h k", h=8), in0=ex[:],
                                                      in1=sm[:, :, 1:2].to_broadcast([128, 8, 16]), op=ALU.mult), reads=[ex, sm], writes=[gg])
                for j in range(128):
                    rb = ring[nring % NB]
                    nring += 1
                    P.dma("pool", lambda e, rb=rb, j=j: e.indirect_dma_start(
                        out=rb[:], out_offset=None, in_=self.UB[:, :], in_offset=bass.IndirectOffsetOnAxis(ap=eidx[:, j:j + 1], axis=0)),
                        reads=[eidx], writes=[rb])
                    P.op("dve", lambda e, rb=rb, j=j: e.scalar_tensor_tensor(
                        out=junkb[:], in0=rb[:], scalar=1.0, in1=h2[:], op0=ALU.mult, op1=ALU.mult, accum_out=act[:, j:j + 1]),
                        reads=[rb, h2], writes=[junkb, act])
                P.op("dve", lambda e: e.tensor_tensor(out=ta[:], in0=act[:], in1=act[:], op=ALU.mult), reads=[act], writes=[ta])
                P.op("dve", lambda e: e.tensor_scalar(out=ta[:], in0=ta[:], scalar1=0.044715, scalar2=1.0, op0=ALU.mult, op1=ALU.add),
                     reads=[ta], writes=[ta])
                P.op("dve", lambda e: e.tensor_tensor(out=ta[:], in0=ta[:], in1=act[:], op=ALU.mult), reads=[ta, act], writes=[ta])
                P.op("act", lambda e: e.activation(out=tb[:], in_=ta[:], func=AF.Sigmoid, scale=1.5957691216057308), reads=[ta], writes=[tb])
                P.op("dve", lambda e: e.tensor_tensor(out=tb[:], in0=tb[:], in1=act[:], op=ALU.mult), reads=[tb, act], writes=[tb])
                P.op("dve", lambda e: e.tensor_tensor(out=wgt[:], in0=tb[:], in1=gg[:], op=ALU.mult), reads=[tb, gg], writes=[wgt])
                P.op("pool", lambda e: e.tensor_tensor(out=Dm[:], in0=self.identb[:].unsqueeze(1).to_broadcast([128, 128, 128]),
                                                       in1=wgt[:].unsqueeze(2).to_broadcast([128, 128, 128]), op=ALU.mult),
                     reads=[self.identb, wgt], writes=[Dm])
                acc = pa[:].rearrange("p k n -> p (k n)")
                for j in range(128):
                    rb = ring[nring % NB]
                    nring += 1
                    P.dma("pool", lambda e, rb=rb, j=j: e.indirect_dma_start(
                        out=rb[:], out_offset=None, in_=self.VB[:, :], in_offset=bass.IndirectOffsetOnAxis(ap=eidx[:, j:j + 1], axis=0)),
                        reads=[eidx], writes=[rb])
                    for hf in range(2):
                        P.op("pe", lambda e, rb=rb, j=j, hf=hf: e.matmul(acc[:, hf * 512:(hf + 1) * 512], lhsT=Dm[:, j, :],
                                                                        rhs=rb[:, hf * 512:(hf + 1) * 512], start=(j == 0), stop=(j == 127)),
                             reads=[Dm, rb], writes=[pa])
                P.op("dve", lambda e: e.tensor_tensor(out=h2[:], in0=acc, in1=GA2[:], op=ALU.mult), reads=[pa, GA2], writes=[h2])
                P.op("dve", lambda e, x_=x_: e.tensor_tensor(out=x_[:], in0=x_[:], in1=h2[:], op=ALU.add), reads=[x_, h2], writes=[x_])
                if final and t >= 2:
                    P.op("act", lambda e, x_=x_, ss_=ss_: e.activation(out=junkb[:], in_=x_[:], func=AF.Square, accum_out=ss_[:, 4:5]),
                         reads=[x_], writes=[junkb, ss_])
                    P.op("dve", lambda e, ss_=ss_: e.tensor_scalar(out=ss_[:, 5:6], in0=ss_[:, 4:5], scalar1=1.0 / DM, scalar2=EPS,
                                                                   op0=ALU.mult, op1=ALU.add), reads=[ss_], writes=[ss_])
                    P.op("act", lambda e, ss_=ss_: e.activation(out=ss_[:, 6:7], in_=ss_[:, 5:6], func=AF.Sqrt), reads=[ss_], writes=[ss_])
                    P.op("dve", lambda e, ss_=ss_: e.reciprocal(out=ss_[:, 7:8], in_=ss_[:, 6:7]), reads=[ss_], writes=[ss_])
                    P.op("dve", lambda e, x_=x_, ss_=ss_: e.scalar_tensor_tensor(out=x_[:], in0=x_[:], scalar=ss_[:, 7:8], in1=gfin[:],
                                                                                 op0=ALU.mult, op1=ALU.mult), reads=[x_, ss_, gfin], writes=[x_])
                    P.dma("sp", lambda e, x_=x_, t=t: e.dma_start(out=self.Y[(t - 2) * 128:(t - 1) * 128, :], in_=x_[:]), reads=[x_])
                else:
                    P.dma("sp", lambda e, x_=x_, rows=rows: e.dma_start(out=self.X[rows, :], in_=x_[:]), reads=[x_])
            P.barrier()

    def build(self):
        c = self.cfg
        with contextlib.ExitStack() as st:
            self.consts(st)
            for l in c.layers:
                last = (l == c.depth - 1)
                with contextlib.ExitStack() as modst:
                    if "M" in c.phases:
                        self.phase_mod(l, modst)
                    if "A" in c.phases:
                        self.phase_a(l)
                if "B" in c.phases:
                    self.phase_b(l)
                if "C" in c.phases:
                    self.phase_c(l, not last)
                if "D" in c.phases:
                    self.phase_d(l, not last)
                if "E" in c.phases:
                    self.phase_e(l, not last, last and c.final)
            self.P.barrier()
            with self.nc.allow_non_contiguous_dma(reason='small strided layout DMAs'):
                self.P.build()
        return self.nc


def host_consts(n_lat):
    p = np.arange(64)
    f = p % 64
    i = f % 16
    freq = (10000.0 ** (-(i.astype(np.float32)) / 16.0)).astype(np.float32)
    rope = np.zeros((n_lat, 64, 2, 128), np.float32)
    j = np.arange(128)
    for m in range(n_lat):
        row = (2 * m + j // 64).astype(np.float32)
        col = (j % 64).astype(np.float32)
        pos = np.where((f < 32)[:, None], row[None, :], col[None, :]).astype(np.float32)
        ang = pos * freq[:, None]
        rope[m, :, 0, :] = np.cos(ang)
        rope[m, :, 1, :] = np.sin(ang)
    rotm = np.zeros((64, 64), np.float32)
    for m_ in range(64):
        fm = m_ % 64
        if (fm % 32) < 16:
            rotm[m_ + 16, m_] = -1.0
        else:
            rotm[m_ - 16, m_] = 1.0
    return rope, rotm


def host_bias(rpb, pats):
    L = rpb.shape[0]
    out = np.empty((L, len(pats), 128, 8, 5, 128), np.float32)
    for pi, (valid, roff, coff) in enumerate(pats):
        g = rpb[:, :, roff, coff]
        g = np.where(valid[None, None], g, np.float32(NEG))
        out[:, pi] = g.transpose(0, 3, 1, 2, 4)
    return out


_CACHE = {}


def make_inputs(inputs, cfg, cores):
    n_lat = cfg.n_lat
    rope, rotm = host_consts(n_lat)
    _, _, pats = na_patterns(n_lat)
    shared = dict(
        w_mod=inputs["w_mod"], b_mod=inputs["b_mod"], g_norm1=inputs["g_norm1"], w_in=inputs["w_in"],
        w_alpha_f=inputs["w_alpha_f"], b_alpha_f=inputs["b_alpha_f"], w_alpha_b=inputs["w_alpha_b"], b_alpha_b=inputs["b_alpha_b"],
        g_gla=inputs["g_gla"], biasT=host_bias(np.asarray(inputs["rpb"]), pats), w_proj_gla=inputs["w_proj_gla"],
        w_proj_na=inputs["w_proj_na"], w_out=inputs["w_out"], g_norm2=inputs["g_norm2"], w_query=inputs["w_query"],
        skT=np.ascontiguousarray(np.asarray(inputs["sub_keys"]).reshape(-1, 16, 128, 128).transpose(0, 1, 3, 2)),
        expert_u=inputs["expert_u"], expert_v=inputs["expert_v"], g_final=inputs["g_final"], rope=rope, rotm=rotm)
    shared = {k: np.ascontiguousarray(np.asarray(v, dtype=np.float32)) for k, v in shared.items()}
    maps = []
    for b in cores:
        xin = np.concatenate([np.asarray(inputs["ctx"][b]), np.asarray(inputs["x"][b][:n_lat * 128])], axis=0).astype(np.float32)
        cv = np.stack([np.asarray(inputs["c"][b]), np.asarray(inputs["c_ctx"])], axis=-1).astype(np.float32)
        cvec = np.ascontiguousarray(cv.reshape(8, 128, 2).transpose(1, 0, 2))
        m = dict(shared)
        m["xin"] = np.ascontiguousarray(xin)
        m["cvec"] = cvec
        maps.append(m)
    return maps


def kernel(**inputs):
    cfg = Cfg()
    if "nc" not in _CACHE:
        _CACHE["nc"] = Builder(cfg).build()
    nc = _CACHE["nc"]
    maps = make_inputs(inputs, cfg, list(range(8)))
    res = run_bass_kernel_spmd(nc, maps, core_ids=list(range(8)))
    out = np.stack([np.asarray(r["Y"]) for r in res.results], axis=0).astype(np.float32)
    return out
```

```python
import contextlib
import numpy as np
import concourse.bass as bass
import concourse.mybir as mybir
from concourse.bass_utils import run_bass_kernel_spmd

F32 = mybir.dt.float32
BF16 = mybir.dt.bfloat16
I32 = mybir.dt.int32
U32 = mybir.dt.uint32
AF = mybir.ActivationFunctionType
ALU = mybir.AluOpType
AX = mybir.AxisListType

N_DMA_SLOTS = 20
DM = 1024
EPS = 1e-6
NEG = -30000.0


class T:
    __slots__ = ("h", "w", "r", "name")

    def __init__(self, h=None, name=""):
        self.h = h
        self.w = None
        self.r = []
        self.name = name

    def __getitem__(self, k):
        return self.h[k]


class Prog:
    ENG = ("pe", "act", "dve", "pool", "sp")

    def __init__(self, nc):
        self.nc = nc
        self.ops = {e: [] for e in self.ENG}
        self.seq = {e: 0 for e in self.ENG}
        self.known = {e: {} for e in self.ENG}
        self.known_ver = {e: None for e in self.ENG}
        self.dcount = {}
        self.dnext = {e: 0 for e in self.ENG}
        self.last_dma = {}
        self.n_instr = 0

    def _snap(self, eng):
        s = self.known_ver[eng]
        if s is None:
            s = dict(self.known[eng])
            self.known_ver[eng] = s
        return s

    def _learn(self, eng, d):
        kn = self.known[eng]
        ch = False
        for k, v in d.items():
            if kn.get(k, 0) < v:
                kn[k] = v
                ch = True
        if ch:
            self.known_ver[eng] = None

    @staticmethod
    def _dep_events(reads, writes):
        ev = []
        for r in reads:
            if r.w is not None:
                ev.append(r.w)
        for w in writes:
            if w.w is not None:
                ev.append(w.w)
            ev.extend(w.r)
        return ev

    def _waits_for(self, eng, events):
        kn = self.known[eng]
        waits = []
        for ev in sorted(events, key=lambda e: -e[1]):
            sk, val, src, snap = ev
            if src == eng and eng == "pe":
                continue
            if kn.get(sk, 0) >= val:
                continue
            waits.append((sk, val))
            kn[sk] = val
            self.known_ver[eng] = None
            if snap is not None:
                self._learn(eng, snap)
        out = []
        seen = {}
        for sk, val in waits:
            if seen.get(sk, 0) >= val:
                continue
            seen[sk] = val
            out.append((sk, val))
        return out

    def op(self, eng, fn, reads=(), writes=()):
        events = self._dep_events(reads, writes)
        waits = self._waits_for(eng, events)
        self.seq[eng] += 1
        me = (("e", eng), self.seq[eng], eng, self._snap(eng))
        self.ops[eng].append((waits, fn, (("e", eng), 1)))
        for r in reads:
            r.r.append(me)
        for w in writes:
            w.w = me
            w.r = []
        self.n_instr += 1 + max(0, len(waits) - 1)
        return me

    def dma(self, q, fn, reads=(), writes=()):
        events = self._dep_events(reads, writes)
        slot = self.dnext[q] % N_DMA_SLOTS
        self.dnext[q] += 1
        sk = ("d", q, slot)
        cnt = self.dcount.get(sk, 0)
        if cnt > 0:
            events = list(events) + [self.last_dma[sk]]
        waits = self._waits_for(q, events)
        self.dcount[sk] = cnt + 16
        me = (sk, cnt + 16, "dma", self._snap(q))
        self.last_dma[sk] = me
        self.ops[q].append((waits, fn, (sk, 16)))
        for r in reads:
            r.r.append(me)
        for w in writes:
            w.w = me
            w.r = []
        self.n_instr += 1 + max(0, len(waits) - 1)
        return me

    def barrier(self):
        events = []
        for e in self.ENG:
            if self.seq[e] > 0:
                events.append((("e", e), self.seq[e], e, None))
        for sk, ev in self.last_dma.items():
            events.append(ev)
        for e in self.ENG:
            kn = self.known[e]
            waits = []
            for sk, val, src, snap in events:
                if kn.get(sk, 0) >= val:
                    continue
                kn[sk] = val
                waits.append((sk, val))
            self.known_ver[e] = None
            if waits:
                self.ops[e].append((waits, None, None))
                self.n_instr += len(waits)

    def build(self):
        nc = self.nc
        keys = set()
        for e in self.ENG:
            for waits, fn, inc in self.ops[e]:
                for sk, _ in waits:
                    keys.add(sk)
                if inc is not None:
                    keys.add(inc[0])
        keys = sorted(keys, key=str)
        with contextlib.ExitStack() as st:
            semh = {}
            for i, k in enumerate(keys):
                semh[k] = st.enter_context(nc.semaphore("s%d" % i))
            block = st.enter_context(nc.Block())

            def runner(e):
                def run(engh):
                    for waits, fn, inc in self.ops[e]:
                        if fn is None:
                            for sk, val in waits:
                                engh.wait_ge(semh[sk], val)
                            continue
                        for sk, val in waits[1:]:
                            engh.wait_ge(semh[sk], val)
                        ins = fn(engh)
                        if waits:
                            ins._wait_ge(semh[waits[0][0]], waits[0][1])
                        ins.then_inc(semh[inc[0]], inc[1])
                return run

            block.tensor(runner("pe"))
            block.scalar(runner("act"))
            block.vector(runner("dve"))
            block.gpsimd(runner("pool"))
            block.sync(runner("sp"))
        return nc


class Cfg:
    def __init__(self, n_lat=64, layers=(0, 1, 2, 3), depth=4, debug=False, phases="MABCDE", final=True):
        self.n_lat = n_lat
        self.nt = n_lat + 2
        self.layers = tuple(layers)
        self.depth = depth
        self.debug = debug
        self.phases = phases
        self.final = final


def na_patterns(n_lat):
    rows = n_lat * 2
    pats, ids, starts, keymap = [], [], [], {}
    kp = np.arange(128)
    for m in range(n_lat):
        st = int(np.clip(m - 2, 0, n_lat - 5))
        j = np.arange(128)
        r = 2 * m + j // 64
        c = j % 64
        r0 = np.clip(r - 4, 0, rows - 8)
        c0 = np.clip(c - 8, 0, 64 - 16)
        kb = np.arange(5)
        kr = (st * 2 + kb[:, None] * 2 + (kp[None, :] // 64))[:, :, None]
        kc = (kp % 64)[None, :, None] + np.zeros((5, 1, 1), np.int64)
        valid = (kr >= r0[None, None, :]) & (kr < r0[None, None, :] + 8) & (kc >= c0[None, None, :]) & (kc < c0[None, None, :] + 16)
        roff = np.where(valid, kr - r[None, None, :] + 7, 0)
        coff = np.where(valid, kc - c[None, None, :] + 15, 0)
        key = (valid.tobytes(), roff.tobytes(), coff.tobytes())
        if key not in keymap:
            keymap[key] = len(pats)
            pats.append((valid, roff, coff))
        ids.append(keymap[key])
        starts.append(st)
    return ids, starts, pats


class Builder:
    def __init__(self, cfg):
        self.cfg = cfg
        nc = bass.Bass("TRN2", target_bir_lowering=False)
        self.nc = nc
        self.P = Prog(nc)
        self.out_names = []
        self.pat_ids, self.pat_starts, self.pats = na_patterns(cfg.n_lat)
        self.npat = len(self.pats)
        cnt = np.bincount(self.pat_ids)
        self.pat_main = int(np.argmax(cnt))
        self._declare()

    def dram_in(self, name, shape, dt=F32):
        return self.nc.dram_tensor(name, list(shape), dt, kind="ExternalInput").ap()

    def dram_scr(self, name, shape, dt=F32):
        kind = "ExternalOutput" if self.cfg.debug else "Internal"
        if self.cfg.debug:
            self.out_names.append(name)
        return self.nc.dram_tensor(name, list(shape), dt, kind=kind).ap()

    def _declare(self):
        c = self.cfg
        NT, NL = c.nt, c.depth
        TOK = NT * 128
        di = self.dram_in
        self.xin = di("xin", [TOK, DM])
        self.cvec = di("cvec", [128, 8, 2])
        self.w_mod = di("w_mod", [NL, DM, 6 * DM])
        self.b_mod = di("b_mod", [NL, 6 * DM])
        self.g1 = di("g_norm1", [NL, DM])
        self.w_in = di("w_in", [NL, DM, 5152])
        self.waf = di("w_alpha_f", [NL, 16, 256])
        self.baf = di("b_alpha_f", [NL, 256])
        self.wab = di("w_alpha_b", [NL, 16, 256])
        self.bab = di("b_alpha_b", [NL, 256])
        self.ggla = di("g_gla", [NL, 128])
        self.biasT = di("biasT", [NL, self.npat, 128, 8, 5, 128])
        self.w_pg = di("w_proj_gla", [NL, 512, DM])
        self.w_pn = di("w_proj_na", [NL, 512, DM])
        self.w_o = di("w_out", [NL, DM, DM])
        self.g2 = di("g_norm2", [NL, DM])
        self.w_q = di("w_query", [NL, DM, 2048])
        self.skT = di("skT", [NL, 16, 128, 128])
        self.eu = di("expert_u", [NL, 16384, DM])
        self.ev = di("expert_v", [NL, 16384, DM])
        self.gfin = di("g_final", [DM])
        self.rope = di("rope", [c.n_lat, 64, 2, 128])
        self.rotm = di("rotm", [64, 64])
        ds = self.dram_scr
        self.X = ds("X", [TOK, DM])
        self.AUX = ds("AUX", [2, 4, DM])
        self.QKT = ds("QKT", [NT, 64, 8, 128], BF16)
        self.ZT = ds("ZT", [32, TOK])
        self.V = ds("V", [TOK, 512], BF16)
        self.RG = ds("RG", [TOK, 512], BF16)
        self.NQT = ds("NQT", [NT, 128, 4, 128], BF16)
        self.NKT = ds("NKT", [NT, 128, 4, 128], BF16)
        self.NVX = ds("NVX", [TOK, 520], BF16)
        self.G = ds("G", [TOK, 2048], BF16)
        self.OF = ds("OF", [TOK, 512])
        self.OB = ds("OB", [TOK, 512])
        self.NAO = ds("NAO", [TOK, 512], BF16)
        self.UVB = ds("UVB", [16384, 2 * DM], BF16)
        self.Y = self.nc.dram_tensor("Y", [c.n_lat * 128, DM], F32, kind="ExternalOutput").ap()

    def sb(self, st, name, shape, dt=F32):
        self._uid = getattr(self, "_uid", 0) + 1
        name = "%s_u%d" % (name, self._uid)
        h = st.enter_context(self.nc.sbuf_tensor(name, list(shape), dt))
        return T(h, name)

    def ps(self, st, name, shape, dt=F32):
        self._uid = getattr(self, "_uid", 0) + 1
        name = "%s_u%d" % (name, self._uid)
        h = st.enter_context(self.nc.psum_tensor(name, list(shape), dt))
        return T(h, name)

    def consts(self, st):
        P = self.P
        sb = self.sb
        self.identf = sb(st, "identf", [128, 128], F32)
        self.identb = sb(st, "identb", [128, 128], BF16)
        self.ones_row = sb(st, "ones_row", [1, 128], F32)
        self.triF = sb(st, "triF", [128, 128], F32)
        self.triB = sb(st, "triB", [128, 128], F32)
        self.mF = sb(st, "mF", [64, 64], F32)
        self.mB = sb(st, "mB", [64, 64], F32)
        self.rotb = sb(st, "rotb", [64, 64], BF16)
        self.cact = sb(st, "cact", [128, 8, 2], F32)
        idf, idb = self.identf, self.identb
        P.op("pool", lambda e: e.memset(idf[:], 1.0), writes=[idf])
        P.op("pool", lambda e: e.affine_select(out=idf[:], in_=idf[:], pattern=[[-1, 128]], compare_op=ALU.is_equal,
                                               fill=0.0, base=0, channel_multiplier=1), reads=[idf], writes=[idf])
        P.op("pool", lambda e: e.tensor_copy(out=idb[:], in_=idf[:]), reads=[idf], writes=[idb])
        P.op("pool", lambda e: e.memset(self.ones_row[:], 1.0), writes=[self.ones_row])
        v = -1.0 / 16.0
        tF, tB = self.triF, self.triB
        P.op("pool", lambda e: e.memset(tF[:], v), writes=[tF])
        P.op("pool", lambda e: e.affine_select(out=tF[:], in_=tF[:], pattern=[[1, 128]], compare_op=ALU.is_ge,
                                               fill=0.0, base=0, channel_multiplier=-1), reads=[tF], writes=[tF])
        P.op("pool", lambda e: e.memset(tF[0:64, 64:128], 0.0), reads=[tF], writes=[tF])
        P.op("pool", lambda e: e.memset(tB[:], v), writes=[tB])
        P.op("pool", lambda e: e.affine_select(out=tB[:], in_=tB[:], pattern=[[-1, 128]], compare_op=ALU.is_ge,
                                               fill=0.0, base=0, channel_multiplier=1), reads=[tB], writes=[tB])
        P.op("pool", lambda e: e.memset(tB[64:128, 0:64], 0.0), reads=[tB], writes=[tB])
        mF, mB = self.mF, self.mB
        P.op("pool", lambda e: e.memset(mF[:], 1.0), writes=[mF])
        P.op("pool", lambda e: e.affine_select(out=mF[:], in_=mF[:], pattern=[[1, 64]], compare_op=ALU.is_ge,
                                               fill=0.0, base=0, channel_multiplier=-1), reads=[mF], writes=[mF])
        P.op("pool", lambda e: e.memset(mB[:], 1.0), writes=[mB])
        P.op("pool", lambda e: e.affine_select(out=mB[:], in_=mB[:], pattern=[[-1, 64]], compare_op=ALU.is_ge,
                                               fill=0.0, base=0, channel_multiplier=1), reads=[mB], writes=[mB])
        with contextlib.ExitStack() as s2:
            rf = self.sb(s2, "rotf", [64, 64], F32)
            cv = self.sb(s2, "cvt", [128, 8, 2], F32)
            sg = self.sb(s2, "csg", [128, 8, 2], F32)
            P.dma("sp", lambda e: e.dma_start(out=rf[:], in_=self.rotm[:, :]), writes=[rf])
            P.op("dve", lambda e: e.tensor_copy(out=self.rotb[:], in_=rf[:]), reads=[rf], writes=[self.rotb])
            P.dma("sp", lambda e: e.dma_start(out=cv[:], in_=self.cvec[:, :, :]), writes=[cv])
            P.op("act", lambda e: e.activation(out=sg[:], in_=cv[:], func=AF.Sigmoid), reads=[cv], writes=[sg])
            P.op("dve", lambda e: e.tensor_tensor(out=self.cact[:], in0=cv[:], in1=sg[:], op=ALU.mult),
                 reads=[cv, sg], writes=[self.cact])
            P.barrier()

    def phase_mod(self, l, modst):
        P, nc = self.P, self.nc
        self.A1T = self.sb(modst, "A1T", [128, 8, 2])
        self.B1T = self.sb(modst, "B1T", [128, 8, 2])
        with contextlib.ExitStack() as st:
            wm = [self.sb(st, "wm%d" % i, [128, 8, 1024]) for i in range(2)]
            bmT = self.sb(st, "bmT", [128, 48])
            g1T = self.sb(st, "g1T", [128, 8])
            g2T = self.sb(st, "g2T", [128, 8])
            modT = self.sb(st, "modT", [128, 48, 2])
            tmp = self.sb(st, "modtmp", [128, 8, 2])
            A2T = self.sb(st, "A2T", [128, 8, 2])
            mps = self.ps(st, "mod_ps", [128, 48, 2])
            ncd = nc.allow_non_contiguous_dma(reason="tiny per-feature vectors")
            st.enter_context(ncd)
            P.dma("sp", lambda e: e.dma_start(out=bmT[:], in_=self.b_mod[l].rearrange("(c p) -> p c", p=128)), writes=[bmT])
            P.dma("sp", lambda e: e.dma_start(out=g1T[:], in_=self.g1[l].rearrange("(c p) -> p c", p=128)), writes=[g1T])
            P.dma("sp", lambda e: e.dma_start(out=g2T[:], in_=self.g2[l].rearrange("(c p) -> p c", p=128)), writes=[g2T])
            for g in range(6):
                w = wm[g % 2]
                P.dma("sp", lambda e, w=w, g=g: e.dma_start(
                    out=w[:], in_=self.w_mod[l][:, g * 1024:(g + 1) * 1024].rearrange("(k p) n -> p k n", p=128)), writes=[w])
                for b in range(8):
                    blk = g * 8 + b
                    for k in range(8):
                        P.op("pe", lambda e, w=w, b=b, k=k, blk=blk: e.matmul(
                            mps[:, blk, :], lhsT=w[:, k, b * 128:(b + 1) * 128], rhs=self.cact[:, k, :],
                            start=(k == 0), stop=(k == 7)), reads=[w, self.cact], writes=[mps])
            P.op("dve", lambda e: e.tensor_tensor(out=modT[:], in0=mps[:], in1=bmT[:].unsqueeze(2).to_broadcast([128, 48, 2]),
                                                  op=ALU.add), reads=[mps, bmT], writes=[modT])
            P.op("dve", lambda e: e.tensor_scalar(out=tmp[:], in0=modT[:, 8:16, :], scalar1=1.0, scalar2=None, op0=ALU.add),
                 reads=[modT], writes=[tmp])
            P.op("dve", lambda e: e.tensor_tensor(out=self.A1T[:], in0=tmp[:], in1=g1T[:].unsqueeze(2).to_broadcast([128, 8, 2]),
                                                  op=ALU.mult), reads=[tmp, g1T], writes=[self.A1T])
            P.op("dve", lambda e: e.tensor_copy(out=self.B1T[:], in_=modT[:, 0:8, :]), reads=[modT], writes=[self.B1T])
            P.op("dve", lambda e: e.tensor_scalar(out=tmp[:], in0=modT[:, 32:40, :], scalar1=1.0, scalar2=None, op0=ALU.add),
                 reads=[modT], writes=[tmp])
            P.op("dve", lambda e: e.tensor_tensor(out=A2T[:], in0=tmp[:], in1=g2T[:].unsqueeze(2).to_broadcast([128, 8, 2]),
                                                  op=ALU.mult), reads=[tmp, g2T], writes=[A2T])
            srcs = [(A2T, None), (modT, 24), (modT, 16), (modT, 40)]
            for i, (t, off) in enumerate(srcs):
                for r in range(2):
                    if off is None:
                        P.dma("sp", lambda e, t=t, i=i, r=r: e.dma_start(
                            out=self.AUX[r, i, :].rearrange("(k p) -> p k", p=128), in_=t[:, :, r]), reads=[t])
                    else:
                        P.dma("sp", lambda e, t=t, i=i, r=r, off=off: e.dma_start(
                            out=self.AUX[r, i, :].rearrange("(k p) -> p k", p=128), in_=t[:, off:off + 8, r]), reads=[t])
            P.barrier()

    def phase_a(self, l):
        P, nc, c = self.P, self.nc, self.cfg
        NT = c.nt
        with contextlib.ExitStack() as st:
            wb = self.sb(st, "a_wb", [128, 8, 5152], BF16)
            stg = [self.sb(st, "a_stg%d" % i, [128, 2576]) for i in range(2)]
            for k in range(8):
                for hf in range(2):
                    s = stg[hf]
                    cs = slice(hf * 2576, (hf + 1) * 2576)
                    P.dma("sp", lambda e, s=s, k=k, cs=cs: e.dma_start(out=s[:], in_=self.w_in[l][k * 128:(k + 1) * 128, cs]), writes=[s])
                    if hf == 0:
                        P.op("act", lambda e, s=s, k=k, cs=cs: e.copy(out=wb[:, k, cs], in_=s[:]), reads=[s], writes=[wb])
                    else:
                        P.op("pool", lambda e, s=s, k=k, cs=cs: e.tensor_copy(out=wb[:, k, cs], in_=s[:]), reads=[s], writes=[wb])
            xt = [self.sb(st, "a_x%d" % i, [128, DM]) for i in range(2)]
            junk = self.sb(st, "a_junk", [128, DM], BF16)
            ss = [self.sb(st, "a_ss%d" % i, [128, 4]) for i in range(2)]
            xn = [self.sb(st, "a_xn%d" % i, [128, DM]) for i in range(2)]
            hT = [self.sb(st, "a_hT%d" % i, [128, 8, 128], BF16) for i in range(2)]
            vt = [self.sb(st, "a_v%d" % i, [128, 512], BF16) for i in range(2)]
            rt = [self.sb(st, "a_r%d" % i, [128, 512], BF16) for i in range(2)]
            rsg = [self.sb(st, "a_rsg%d" % i, [128, 512]) for i in range(2)]
            nvx = [self.sb(st, "a_nvx%d" % i, [128, 8, 65], BF16) for i in range(2)]
            gt = [self.sb(st, "a_g%d" % i, [128, 2048], BF16) for i in range(2)]
            qkb = [self.sb(st, "a_qkb%d" % i, [64, 8, 128], BF16) for i in range(2)]
            qkr = [self.sb(st, "a_qkr%d" % i, [64, 8, 128], BF16) for i in range(2)]
            t1 = [self.sb(st, "a_t1%d" % i, [64, 8, 128]) for i in range(2)]
            t2 = [self.sb(st, "a_t2%d" % i, [64, 8, 128]) for i in range(2)]
            zt = [self.sb(st, "a_z%d" % i, [32, 128]) for i in range(2)]
            nqt = [self.sb(st, "a_nq%d" % i, [128, 4, 128], BF16) for i in range(2)]
            nkt = [self.sb(st, "a_nk%d" % i, [128, 4, 128], BF16) for i in range(2)]
            rp = [self.sb(st, "a_rp%d" % i, [64, 2, 128]) for i in range(2)]
            tps = self.ps(st, "a_tps", [128, 8, 128])
            tok = [self.ps(st, "a_tok%d" % i, [128, 512]) for i in range(2)]
            fm = [self.ps(st, "a_fm%d" % i, [128, 4, 128]) for i in range(2)]
            rot = self.ps(st, "a_rot", [64, 8, 128])
            for i in range(2):
                P.op("pool", lambda e, i=i: e.memset(nvx[i][:], 1.0), writes=[nvx[i]])
            C_Q, C_K, C_V, C_Z, C_R, C_NQ, C_NK, C_NV, C_G = 0, 256, 512, 1024, 1056, 1568, 2080, 2592, 3104
            ntok = 0
            nfm = 0
            for t in range(NT):
                b = t % 2
                r = 1 if t < 2 else 0
                src = self.xin if l == 0 else self.X
                x_, ss_, xn_, hT_ = xt[b], ss[b], xn[b], hT[b]
                P.dma("sp", lambda e, x_=x_, t=t, src=src: e.dma_start(out=x_[:], in_=src[t * 128:(t + 1) * 128, :]), writes=[x_])
                P.op("act", lambda e, x_=x_, ss_=ss_: e.activation(out=junk[:], in_=x_[:], func=AF.Square, accum_out=ss_[:, 0:1]),
                     reads=[x_], writes=[junk, ss_])
                P.op("dve", lambda e, ss_=ss_: e.tensor_scalar(out=ss_[:, 1:2], in0=ss_[:, 0:1], scalar1=1.0 / DM, scalar2=EPS,
                                                               op0=ALU.mult, op1=ALU.add), reads=[ss_], writes=[ss_])
                P.op("act", lambda e, ss_=ss_: e.activation(out=ss_[:, 2:3], in_=ss_[:, 1:2], func=AF.Sqrt), reads=[ss_], writes=[ss_])
                P.op("dve", lambda e, ss_=ss_: e.reciprocal(out=ss_[:, 3:4], in_=ss_[:, 2:3]), reads=[ss_], writes=[ss_])
                P.op("dve", lambda e, x_=x_, xn_=xn_, ss_=ss_: e.tensor_scalar(out=xn_[:], in0=x_[:], scalar1=ss_[:, 3:4], scalar2=None,
                                                                               op0=ALU.mult), reads=[x_, ss_], writes=[xn_])
                for k in range(8):
                    P.op("pe", lambda e, xn_=xn_, k=k: e.transpose(out=tps[:, k, :], in_=xn_[:, k * 128:(k + 1) * 128],
                                                                   identity=self.identf[:]), reads=[xn_, self.identf], writes=[tps])
                for k in range(8):
                    P.op("act", lambda e, hT_=hT_, k=k, r=r: e.activation(
                        out=hT_[:, k, :], in_=tps[:, k, :], func=AF.Identity, scale=self.A1T[:, k, r:r + 1],
                        bias=self.B1T[:, k, r:r + 1]), reads=[tps, self.A1T, self.B1T], writes=[hT_])
                tm = [("v", C_V), ("r", C_R), ("nv", C_NV), ("g0", C_G), ("g1", C_G + 512), ("g2", C_G + 1024), ("g3", C_G + 1536)]
                for nm, c0 in tm:
                    pt = tok[ntok % 2]
                    ntok += 1
                    for k in range(8):
                        P.op("pe", lambda e, pt=pt, hT_=hT_, k=k, c0=c0: e.matmul(
                            pt[:], lhsT=hT_[:, k, :], rhs=wb[:, k, c0:c0 + 512], start=(k == 0), stop=(k == 7)),
                            reads=[hT_, wb], writes=[pt])
                    if nm == "v":
                        P.op("dve", lambda e, pt=pt, b=b: e.tensor_copy(out=vt[b][:], in_=pt[:]), reads=[pt], writes=[vt[b]])
                    elif nm == "r":
                        P.op("act", lambda e, pt=pt, b=b: e.activation(out=rsg[b][:], in_=pt[:], func=AF.Sigmoid),
                             reads=[pt], writes=[rsg[b]])
                        P.op("dve", lambda e, pt=pt, b=b: e.tensor_tensor(out=rt[b][:], in0=pt[:], in1=rsg[b][:], op=ALU.mult),
                             reads=[pt, rsg[b]], writes=[rt[b]])
                    elif nm == "nv":
                        P.op("dve", lambda e, pt=pt, b=b: e.tensor_copy(
                            out=nvx[b][:, :, 0:64], in_=pt[:].rearrange("p (h d) -> p h d", h=8)), reads=[pt], writes=[nvx[b]])
                    else:
                        gi = int(nm[1])
                        P.op("act", lambda e, pt=pt, b=b, gi=gi: e.activation(
                            out=gt[b][:, gi * 512:(gi + 1) * 512], in_=pt[:], func=AF.Sigmoid), reads=[pt], writes=[gt[b]])
                groups = [("q", [C_Q + i * 64 for i in range(4)]), ("k", [C_K + i * 64 for i in range(4)]), ("z", [C_Z]),
                          ("nq", [C_NQ + i * 128 for i in range(4)]), ("nk", [C_NK + i * 128 for i in range(4)])]
                for nm, cols in groups:
                    pf = fm[nfm % 2]
                    nfm += 1
                    for bi, c0 in enumerate(cols):
                        M = 32 if nm == "z" else (64 if nm in ("q", "k") else 128)
                        for k in range(8):
                            P.op("pe", lambda e, pf=pf, hT_=hT_, k=k, c0=c0, bi=bi, M=M: e.matmul(
                                pf[0:M, bi, :], lhsT=wb[:, k, c0:c0 + M], rhs=hT_[:, k, :], start=(k == 0), stop=(k == 7)),
                                reads=[hT_, wb], writes=[pf])
                    if nm in ("q", "k"):
                        o4 = 0 if nm == "q" else 4
                        P.op("act", lambda e, pf=pf, b=b, o4=o4: e.copy(out=qkb[b][:, o4:o4 + 4, :], in_=pf[0:64, :, :]), reads=[pf], writes=[qkb[b]])
                    elif nm == "z":
                        P.op("dve", lambda e, pf=pf, b=b: e.tensor_copy(out=zt[b][:], in_=pf[0:32, 0, :]), reads=[pf], writes=[zt[b]])
                    elif nm == "nq":
                        P.op("act", lambda e, pf=pf, b=b: e.copy(out=nqt[b][:], in_=pf[:]), reads=[pf], writes=[nqt[b]])
                    else:
                        P.op("dve", lambda e, pf=pf, b=b: e.tensor_copy(out=nkt[b][:], in_=pf[:]), reads=[pf], writes=[nkt[b]])
                if t >= 2:
                    P.dma("sp", lambda e, b=b, t=t: e.dma_start(out=rp[b][:], in_=self.rope[t - 2]), writes=[rp[b]])
                    for bi in range(8):
                        P.op("pe", lambda e, b=b, bi=bi: e.matmul(rot[:, bi, :], lhsT=self.rotb[:], rhs=qkb[b][:, bi, :],
                                                                 start=True, stop=True), reads=[qkb[b], self.rotb], writes=[rot])
                    P.op("dve", lambda e, b=b: e.tensor_tensor(out=t1[b][:], in0=qkb[b][:],
                                                               in1=rp[b][:, 0:1, :].to_broadcast([64, 8, 128]), op=ALU.mult),
                         reads=[qkb[b], rp[b]], writes=[t1[b]])
                    P.op("dve", lambda e, b=b: e.tensor_tensor(out=t2[b][:], in0=rot[:],
                                                               in1=rp[b][:, 1:2, :].to_broadcast([64, 8, 128]), op=ALU.mult),
                         reads=[rot, rp[b]], writes=[t2[b]])
                    P.op("pool", lambda e, b=b: e.tensor_tensor(out=qkr[b][:], in0=t1[b][:], in1=t2[b][:], op=ALU.add),
                         reads=[t1[b], t2[b]], writes=[qkr[b]])
                    qsrc = qkr[b]
                else:
                    qsrc = qkb[b]
                rows = slice(t * 128, (t + 1) * 128)
                P.dma("sp", lambda e, qsrc=qsrc, t=t: e.dma_start(out=self.QKT[t], in_=qsrc[:]), reads=[qsrc])
                P.dma("sp", lambda e, b=b, rows=rows: e.dma_start(out=self.ZT[:, rows], in_=zt[b][:]), reads=[zt[b]])
                P.dma("sp", lambda e, b=b, rows=rows: e.dma_start(out=self.V[rows, :], in_=vt[b][:]), reads=[vt[b]])
                P.dma("sp", lambda e, b=b, rows=rows: e.dma_start(out=self.RG[rows, :], in_=rt[b][:]), reads=[rt[b]])
                P.dma("sp", lambda e, b=b, t=t: e.dma_start(out=self.NQT[t], in_=nqt[b][:]), reads=[nqt[b]])
                P.dma("sp", lambda e, b=b, t=t: e.dma_start(out=self.NKT[t], in_=nkt[b][:]), reads=[nkt[b]])
                P.dma("sp", lambda e, b=b, rows=rows: e.dma_start(out=self.NVX[rows, :], in_=nvx[b][:].rearrange("p h d -> p (h d)")),
                      reads=[nvx[b]])
                P.dma("sp", lambda e, b=b, rows=rows: e.dma_start(out=self.G[rows, :], in_=gt[b][:]), reads=[gt[b]])
            P.barrier()

    def phase_b(self, l):
        P, nc, c = self.P, self.nc, self.cfg
        NT = c.nt
        with contextlib.ExitStack() as st:
            wa = {}
            ba = {}
            for d, (wsrc, bsrc) in (("f", (self.waf, self.baf)), ("b", (self.wab, self.bab))):
                wa[d] = self.sb(st, "b_wa" + d, [16, 256])
                ba[d] = self.sb(st, "b_ba" + d, [1, 256])
                P.dma("sp", lambda e, d=d, wsrc=wsrc: e.dma_start(out=wa[d][:], in_=wsrc[l]), writes=[wa[d]])
                P.dma("sp", lambda e, d=d, bsrc=bsrc: e.dma_start(out=ba[d][:], in_=bsrc[l:l + 1, :]), writes=[ba[d]])
            S32 = {d: self.sb(st, "b_S32" + d, [64, 4, 128]) for d in "fb"}
            Sbf = {d: self.sb(st, "b_Sbf" + d, [64, 4, 128], BF16) for d in "fb"}
            for d in "fb":
                P.op("pool", lambda e, d=d: e.memset(S32[d][:], 0.0), writes=[S32[d]])
                P.op("pool", lambda e, d=d: e.memset(Sbf[d][:], 0.0), writes=[Sbf[d]])
            mk = lambda nm, shp, dt=F32: {d: [self.sb(st, "b_%s%s%d" % (nm, d, i), shp, dt) for i in range(2)] for d in "fb"}
            zt = mk("z", [16, 128])
            qk = mk("qk", [64, 8, 128], BF16)
            vv = mk("v", [64, 2, 512], BF16)
            e1 = mk("e1", [128, 256])
            Lt = mk("L", [128, 256])
            eb = mk("eb", [64, 4, 128])
            enb = mk("enb", [64, 4, 128])
            qe = mk("qe", [64, 4, 128], BF16)
            ke = mk("ke", [64, 4, 128], BF16)
            ktok = mk("ktok", [64, 4, 64], BF16)
            Am = mk("Am", [64, 4, 64], BF16)
            Ot = mk("Ot", [64, 512])
            lb_ps = {d: self.ps(st, "b_lb" + d, [128, 512]) for d in "fb"}
            kT_ps = self.ps(st, "b_kT", [64, 4, 64], BF16)
            A_ps = {d: self.ps(st, "b_A" + d, [64, 4, 64]) for d in "fb"}
            O_ps = {d: self.ps(st, "b_O" + d, [64, 512]) for d in "fb"}
            dS_ps = self.ps(st, "b_dS", [64, 4, 128])
            tri = {"f": self.triF, "b": self.triB}
            msk = {"f": self.mF, "b": self.mB}
            OD = {"f": self.OF, "b": self.OB}
            order_f = list(range(NT))
            order_b = [1, 0] + list(range(NT - 1, 1, -1))
            cnt = {"f": 0, "b": 0}

            def emit(d, t):
                i = cnt[d] % 2
                cnt[d] += 1
                z_, qk_, v_, e1_, L_, eb_, enb_, qe_, ke_ = zt[d][i], qk[d][i], vv[d][i], e1[d][i], Lt[d][i], eb[d][i], enb[d][i], qe[d][i], ke[d][i]
                rows = slice(t * 128, (t + 1) * 128)
                zr = slice(0, 16) if d == "f" else slice(16, 32)
                P.dma("sp", lambda e: e.dma_start(out=z_[:], in_=self.ZT[zr, rows]), writes=[z_])
                P.dma("sp", lambda e: e.dma_start(out=qk_[:], in_=self.QKT[t]), writes=[qk_])
                P.dma("sp", lambda e: e.dma_start(out=v_[:], in_=self.V[rows, :].rearrange("(c p) n -> p c n", p=64)), writes=[v_])
                lp = lb_ps[d]
                la = lp[:, 0:256]
                bp = lp[0:64, :].rearrange("p (h n) -> p h n", h=4)
                P.op("pe", lambda e: e.matmul(la, lhsT=z_[:], rhs=wa[d][:], start=True, stop=False), reads=[z_, wa[d]], writes=[lp])
                P.op("pe", lambda e: e.matmul(la, lhsT=self.ones_row[:], rhs=ba[d][:], start=False, stop=True),
                     reads=[self.ones_row, ba[d]], writes=[lp])
                P.op("act", lambda e: e.activation(out=e1_[:], in_=la, func=AF.Exp, scale=-1.0), reads=[lp], writes=[e1_])
                P.op("act", lambda e: e.activation(out=L_[:], in_=e1_[:], func=AF.Ln, bias=1.0), reads=[e1_], writes=[L_])
                for h in range(4):
                    P.op("pe", lambda e, h=h: e.matmul(bp[:, h, :], lhsT=L_[:, h * 64:(h + 1) * 64], rhs=tri[d][:], start=True, stop=True),
                         reads=[L_, tri[d]], writes=[lp])
                P.op("act", lambda e: e.activation(out=eb_[:], in_=bp, func=AF.Exp), reads=[lp], writes=[eb_])
                P.op("act", lambda e: e.activation(out=enb_[:], in_=bp, func=AF.Exp, scale=-1.0), reads=[lp], writes=[enb_])
                P.op("dve", lambda e: e.scalar_tensor_tensor(out=qe_[:], in0=qk_[:, 0:4, :], scalar=0.125, in1=eb_[:],
                                                             op0=ALU.mult, op1=ALU.mult), reads=[qk_, eb_], writes=[qe_])
                P.op("dve", lambda e: e.tensor_tensor(out=ke_[:], in0=qk_[:, 4:8, :], in1=enb_[:], op=ALU.mult),
                     reads=[qk_, enb_], writes=[ke_])
                def chunk(c_):
                    cs = slice(c_ * 64, (c_ + 1) * 64)
                    kt_, Am_, Ot_ = ktok[d][c_], Am[d][c_], Ot[d][c_]
                    for h in range(4):
                        P.op("pe", lambda e, h=h: e.transpose(out=kT_ps[:, h, :], in_=ke_[:, h, cs], identity=self.identb[0:64, 0:64]),
                             reads=[ke_, self.identb], writes=[kT_ps])
                    P.op("act", lambda e: e.copy(out=kt_[:], in_=kT_ps[:]), reads=[kT_ps], writes=[kt_])
                    for h in range(4):
                        P.op("pe", lambda e, h=h: e.matmul(A_ps[d][:, h, :], lhsT=ke_[:, h, cs], rhs=qe_[:, h, cs], start=True, stop=True),
                             reads=[ke_, qe_], writes=[A_ps[d]])
                    P.op("dve", lambda e: e.tensor_tensor(out=Am_[:], in0=A_ps[d][:],
                                                          in1=msk[d][:].unsqueeze(1).to_broadcast([64, 4, 64]), op=ALU.mult),
                         reads=[A_ps[d], msk[d]], writes=[Am_])
                    for h in range(4):
                        P.op("pe", lambda e, h=h: e.matmul(O_ps[d][:, h * 128:(h + 1) * 128], lhsT=Am_[:, h, :],
                                                          rhs=v_[:, c_, h * 128:(h + 1) * 128], start=True, stop=False),
                             reads=[Am_, v_], writes=[O_ps[d]])
                        P.op("pe", lambda e, h=h: e.matmul(O_ps[d][:, h * 128:(h + 1) * 128], lhsT=qe_[:, h, cs], rhs=Sbf[d][:, h, :],
                                                          start=False, stop=True), reads=[qe_, Sbf[d]], writes=[O_ps[d]])
                    for h in range(4):
                        P.op("pe", lambda e, h=h: e.matmul(dS_ps[:, h, :], lhsT=kt_[:, h, :], rhs=v_[:, c_, h * 128:(h + 1) * 128],
                                                          start=True, stop=True), reads=[kt_, v_], writes=[dS_ps])
                    P.op("act", lambda e: e.copy(out=Ot_[:], in_=O_ps[d][:]), reads=[O_ps[d]], writes=[Ot_])
                    r0 = t * 128 + c_ * 64
                    P.dma("sp", lambda e: e.dma_start(out=OD[d][r0:r0 + 64, :], in_=Ot_[:]), reads=[Ot_])
                    P.op("dve", lambda e: e.tensor_tensor(out=S32[d][:], in0=S32[d][:], in1=dS_ps[:], op=ALU.add),
                         reads=[S32[d], dS_ps], writes=[S32[d]])
                    tcol = c_ * 64 + 63 if d == "f" else c_ * 64
                    P.op("dve", lambda e, tcol=tcol: e.tensor_tensor(
                        out=S32[d][:], in0=S32[d][:], in1=eb_[:, :, tcol:tcol + 1].to_broadcast([64, 4, 128]), op=ALU.mult),
                        reads=[S32[d], eb_], writes=[S32[d]])
                    P.op("act", lambda e: e.copy(out=Sbf[d][:], in_=S32[d][:]), reads=[S32[d]], writes=[Sbf[d]])

                for c_ in ((0, 1) if d == "f" else (1, 0)):
                    chunk(c_)

            for s_ in range(NT):
                emit("f", order_f[s_])
                emit("b", order_b[s_])
            P.barrier()

    def phase_c(self, l, with_ctx):
        P, nc, c = self.P, self.nc, self.cfg
        with contextlib.ExitStack() as st:
            st.enter_context(nc.allow_non_contiguous_dma(reason="tile-blocked K layout"))
            kTc = self.sb(st, "c_kTc", [128, 2, 4, 128], BF16)
            vxc = self.sb(st, "c_vxc", [128, 2, 520], BF16)
            biasM = self.sb(st, "c_biasM", [128, 8, 5, 128])
            biasE = self.sb(st, "c_biasE", [128, 8, 5, 128])
            P.dma("sp", lambda e: e.dma_start(out=kTc[:], in_=self.NKT[0:2].rearrange("t p b k -> p t b k")), writes=[kTc])
            P.dma("sp", lambda e: e.dma_start(out=vxc[:], in_=self.NVX[0:256, :].rearrange("(t p) d -> p t d", p=128)), writes=[vxc])
            P.dma("sp", lambda e: e.dma_start(out=biasM[:], in_=self.biasT[l, self.pat_main]), writes=[biasM])
            qT = [self.sb(st, "c_qT%d" % i, [128, 4, 128], BF16) for i in range(2)]
            kT = [self.sb(st, "c_kT%d" % i, [128, 5, 4, 128], BF16) for i in range(2)]
            vx = [self.sb(st, "c_vx%d" % i, [128, 5, 520], BF16) for i in range(2)]
            Tt = [self.sb(st, "c_T%d" % i, [128, 5, 128]) for i in range(2)]
            PT = [self.sb(st, "c_PT%d" % i, [128, 7, 128], BF16) for i in range(2)]
            rec = [self.sb(st, "c_rec%d" % i, [128, 8]) for i in range(2)]
            ot = [self.sb(st, "c_o%d" % i, [128, 8, 64], BF16) for i in range(2)]
            S_ps = [self.ps(st, "c_S%d" % i, [128, 8, 128]) for i in range(2)]
            O_ps = [[self.ps(st, "c_O%d%d" % (i, j), [128, 4, 65]) for j in range(2)] for i in range(2)]
            tiles = ([0, 1] if with_ctx else []) + list(range(2, c.nt))
            cur_edge = None
            nh = 0
            for n_, t in enumerate(tiles):
                b = n_ % 2
                win = t >= 2
                P.dma("sp", lambda e, b=b, t=t: e.dma_start(out=qT[b][:], in_=self.NQT[t]), writes=[qT[b]])
                if win:
                    m = t - 2
                    s0 = 2 + self.pat_starts[m]
                    P.dma("sp", lambda e, b=b, s0=s0: e.dma_start(out=kT[b][:], in_=self.NKT[s0:s0 + 5].rearrange("t p b k -> p t b k")),
                          writes=[kT[b]])
                    P.dma("sp", lambda e, b=b, s0=s0: e.dma_start(
                        out=vx[b][:], in_=self.NVX[s0 * 128:(s0 + 5) * 128, :].rearrange("(t p) d -> p t d", p=128)), writes=[vx[b]])
                    pid = self.pat_ids[m]
                    if pid == self.pat_main:
                        bias = biasM
                    else:
                        if cur_edge != pid:
                            P.dma("sp", lambda e, pid=pid: e.dma_start(out=biasE[:], in_=self.biasT[l, pid]), writes=[biasE])
                            cur_edge = pid
                        bias = biasE
                for h in range(8):
                    blk, po = h // 2, (h % 2) * 64
                    sp_ = S_ps[nh % 2]
                    T_, PT_ = Tt[nh % 2], PT[nh % 2]
                    nh += 1
                    if win:
                        for kb in range(5):
                            P.op("pe", lambda e, kb=kb, b=b, blk=blk, po=po, sp_=sp_: e.matmul(
                                sp_[:, kb, :], lhsT=kT[b][po:po + 64, kb, blk, :], rhs=qT[b][po:po + 64, blk, :], start=True, stop=True),
                                reads=[kT[b], qT[b]], writes=[sp_])
                    for cb in range(2):
                        P.op("pe", lambda e, cb=cb, b=b, blk=blk, po=po, sp_=sp_: e.matmul(
                            sp_[:, 5 + cb, :], lhsT=kTc[po:po + 64, cb, blk, :], rhs=qT[b][po:po + 64, blk, :], start=True, stop=True),
                            reads=[kTc, qT[b]], writes=[sp_])
                    if win:
                        P.op("dve", lambda e, sp_=sp_, T_=T_, h=h, bias=bias: e.scalar_tensor_tensor(
                            out=T_[:], in0=sp_[:, 0:5, :], scalar=0.125, in1=bias[:, h, :, :], op0=ALU.mult, op1=ALU.add),
                            reads=[sp_, bias], writes=[T_])
                        P.op("act", lambda e, T_=T_, PT_=PT_: e.activation(out=PT_[:, 0:5, :], in_=T_[:], func=AF.Exp),
                             reads=[T_], writes=[PT_])
                    P.op("act", lambda e, sp_=sp_, PT_=PT_: e.activation(out=PT_[:, 5:7, :], in_=sp_[:, 5:7, :], func=AF.Exp, scale=0.125),
                         reads=[sp_], writes=[PT_])
                    op_ = O_ps[b][h // 4]
                    hl = h % 4
                    if win:
                        for kb in range(5):
                            P.op("pe", lambda e, kb=kb, b=b, h=h, op_=op_, hl=hl, PT_=PT_: e.matmul(
                                op_[:, hl, :], lhsT=PT_[:, kb, :], rhs=vx[b][:, kb, h * 65:(h + 1) * 65], start=(kb == 0), stop=False),
                                reads=[PT_, vx[b]], writes=[op_])
                    for cb in range(2):
                        P.op("pe", lambda e, cb=cb, h=h, op_=op_, hl=hl, PT_=PT_, win=win: e.matmul(
                            op_[:, hl, :], lhsT=PT_[:, 5 + cb, :], rhs=vxc[:, cb, h * 65:(h + 1) * 65],
                            start=(cb == 0 and not win), stop=(cb == 1)), reads=[PT_, vxc], writes=[op_])
                for j in range(2):
                    op_ = O_ps[b][j]
                    P.op("dve", lambda e, b=b, j=j, op_=op_: e.reciprocal(out=rec[b][:, j * 4:(j + 1) * 4], in_=op_[:, :, 64]),
                         reads=[op_], writes=[rec[b]])
                    P.op("dve", lambda e, b=b, j=j, op_=op_: e.tensor_tensor(
                        out=ot[b][:, j * 4:(j + 1) * 4, :], in0=op_[:, :, 0:64],
                        in1=rec[b][:, j * 4:(j + 1) * 4].unsqueeze(2).to_broadcast([128, 4, 64]), op=ALU.mult),
                        reads=[op_, rec[b]], writes=[ot[b]])
                P.dma("sp", lambda e, b=b, t=t: e.dma_start(out=self.NAO[t * 128:(t + 1) * 128, :],
                                                            in_=ot[b][:].rearrange("p h d -> p (h d)")), reads=[ot[b]])
            P.barrier()

    def phase_d(self, l, with_ctx):
        P, nc, c = self.P, self.nc, self.cfg
        with contextlib.ExitStack() as st:
            wpg = self.sb(st, "d_wpg", [128, 4, DM], BF16)
            wpn = self.sb(st, "d_wpn", [128, 4, DM], BF16)
            wo = self.sb(st, "d_wo", [128, 8, DM], BF16)
            stg = [self.sb(st, "d_stg%d" % i, [128, DM]) for i in range(2)]
            n = 0
            for (dst, src, nk) in ((wpg, self.w_pg, 4), (wpn, self.w_pn, 4), (wo, self.w_o, 8)):
                for k in range(nk):
                    s = stg[n % 2]
                    P.dma("sp", lambda e, s=s, k=k, src=src: e.dma_start(out=s[:], in_=src[l][k * 128:(k + 1) * 128, :]), writes=[s])
                    if n % 2 == 0:
                        P.op("act", lambda e, s=s, k=k, dst=dst: e.copy(out=dst[:, k, :], in_=s[:]), reads=[s], writes=[dst])
                    else:
                        P.op("pool", lambda e, s=s, k=k, dst=dst: e.tensor_copy(out=dst[:, k, :], in_=s[:]), reads=[s], writes=[dst])
                    n += 1
            gg = self.sb(st, "d_gg", [128, 128])
            ga1 = [self.sb(st, "d_ga1_%d" % r, [128, DM]) for r in range(2)]
            P.dma("sp", lambda e: e.dma_start(out=gg[:], in_=self.ggla[l:l + 1, :].partition_broadcast(128)), writes=[gg])
            for r in range(2):
                P.dma("sp", lambda e, r=r: e.dma_start(out=ga1[r][:], in_=self.AUX[r, 2:3, :].partition_broadcast(128)), writes=[ga1[r]])
            of = [self.sb(st, "d_of%d" % i, [128, 4, 128]) for i in range(2)]
            ob = [self.sb(st, "d_ob%d" % i, [128, 4, 128]) for i in range(2)]
            rg = [self.sb(st, "d_rg%d" % i, [128, 512], BF16) for i in range(2)]
            nao = [self.sb(st, "d_nao%d" % i, [128, 512], BF16) for i in range(2)]
            gt = [self.sb(st, "d_g%d" % i, [128, 2048], BF16) for i in range(2)]
            xt = [self.sb(st, "d_x%d" % i, [128, DM]) for i in range(2)]
            junk = self.sb(st, "d_junk", [128, 128], BF16)
            ss = [self.sb(st, "d_ss%d" % i, [128, 4, 4]) for i in range(2)]
            onb = self.sb(st, "d_onb", [128, 512], BF16)
            onT = self.sb(st, "d_onT", [128, 4, 128], BF16)
            naT = self.sb(st, "d_naT", [128, 4, 128], BF16)
            m1 = self.sb(st, "d_m1", [128, DM])
            m2 = self.sb(st, "d_m2", [128, DM])
            mb = self.sb(st, "d_mb", [128, DM], BF16)
            mT = self.sb(st, "d_mT", [128, 8, 128], BF16)
            tpA = self.ps(st, "d_tpA", [128, 8, 128], BF16)
            tpB = self.ps(st, "d_tpB", [128, 8, 128], BF16)
            ya = self.ps(st, "d_ya", [128, DM])
            yb = self.ps(st, "d_yb", [128, DM])
            yy = self.ps(st, "d_yy", [128, DM])
            tiles = ([0, 1] if with_ctx else []) + list(range(2, c.nt))
            for n_, t in enumerate(tiles):
                b = n_ % 2
                r = 1 if t < 2 else 0
                rows = slice(t * 128, (t + 1) * 128)
                of_, ob_, rg_, nao_, g_, x_, ss_ = of[b], ob[b], rg[b], nao[b], gt[b], xt[b], ss[b]
                P.dma("sp", lambda e, of_=of_, rows=rows: e.dma_start(out=of_[:].rearrange("p h d -> p (h d)"), in_=self.OF[rows, :]), writes=[of_])
                P.dma("sp", lambda e, ob_=ob_, rows=rows: e.dma_start(out=ob_[:].rearrange("p h d -> p (h d)"), in_=self.OB[rows, :]), writes=[ob_])
                P.dma("sp", lambda e, rg_=rg_, rows=rows: e.dma_start(out=rg_[:], in_=self.RG[rows, :]), writes=[rg_])
                P.dma("sp", lambda e, nao_=nao_, rows=rows: e.dma_start(out=nao_[:], in_=self.NAO[rows, :]), writes=[nao_])
                P.dma("sp", lambda e, g_=g_, rows=rows: e.dma_start(out=g_[:], in_=self.G[rows, :]), writes=[g_])
                P.dma("sp", lambda e, x_=x_, rows=rows, l=l: e.dma_start(out=x_[:], in_=(self.xin if l == 0 else self.X)[rows, :]), writes=[x_])
                P.op("dve", lambda e, of_=of_, ob_=ob_: e.tensor_tensor(out=of_[:], in0=of_[:], in1=ob_[:], op=ALU.add),
                     reads=[of_, ob_], writes=[of_])
                for h in range(4):
                    P.op("act", lambda e, of_=of_, ss_=ss_, h=h: e.activation(out=junk[:], in_=of_[:, h, :], func=AF.Square,
                                                                             accum_out=ss_[:, 0, h:h + 1]), reads=[of_], writes=[junk, ss_])
                P.op("dve", lambda e, ss_=ss_: e.tensor_scalar(out=ss_[:, 1, :], in0=ss_[:, 0, :], scalar1=1.0 / 128, scalar2=EPS,
                                                               op0=ALU.mult, op1=ALU.add), reads=[ss_], writes=[ss_])
                P.op("act", lambda e, ss_=ss_: e.activation(out=ss_[:, 2, :], in_=ss_[:, 1, :], func=AF.Sqrt), reads=[ss_], writes=[ss_])
                P.op("dve", lambda e, ss_=ss_: e.reciprocal(out=ss_[:, 3, :], in_=ss_[:, 2, :]), reads=[ss_], writes=[ss_])
                P.op("dve", lambda e, of_=of_, ss_=ss_: e.tensor_tensor(
                    out=of_[:], in0=of_[:], in1=ss_[:, 3, :].unsqueeze(2).to_broadcast([128, 4, 128]), op=ALU.mult),
                    reads=[of_, ss_], writes=[of_])
                P.op("pool", lambda e, of_=of_: e.tensor_tensor(
                    out=of_[:], in0=of_[:], in1=gg[:].unsqueeze(1).to_broadcast([128, 4, 128]), op=ALU.mult),
                    reads=[of_, gg], writes=[of_])
                P.op("dve", lambda e, of_=of_, rg_=rg_: e.tensor_tensor(out=onb[:], in0=of_[:].rearrange("p h d -> p (h d)"),
                                                                      in1=rg_[:], op=ALU.mult), reads=[of_, rg_], writes=[onb])
                for k in range(4):
                    P.op("pe", lambda e, k=k: e.transpose(out=tpA[:, k, :], in_=onb[:, k * 128:(k + 1) * 128], identity=self.identb[:]),
                         reads=[onb, self.identb], writes=[tpA])
                for k in range(4):
                    P.op("pe", lambda e, k=k, nao_=nao_: e.transpose(out=tpA[:, 4 + k, :], in_=nao_[:, k * 128:(k + 1) * 128],
                                                                    identity=self.identb[:]), reads=[nao_, self.identb], writes=[tpA])
                P.op("act", lambda e: e.copy(out=onT[:], in_=tpA[:, 0:4, :]), reads=[tpA], writes=[onT])
                P.op("act", lambda e: e.copy(out=naT[:], in_=tpA[:, 4:8, :]), reads=[tpA], writes=[naT])
                for (yp, xT, w) in ((ya, onT, wpg), (yb, naT, wpn)):
                    for cc in range(2):
                        for k in range(4):
                            P.op("pe", lambda e, yp=yp, xT=xT, w=w, cc=cc, k=k: e.matmul(
                                yp[:, cc * 512:(cc + 1) * 512], lhsT=xT[:, k, :], rhs=w[:, k, cc * 512:(cc + 1) * 512],
                                start=(k == 0), stop=(k == 3)), reads=[xT, w], writes=[yp])
                P.op("dve", lambda e, g_=g_: e.tensor_tensor(out=m1[:], in0=ya[:], in1=g_[:, 0:DM], op=ALU.mult),
                     reads=[ya, g_], writes=[m1])
                P.op("dve", lambda e, g_=g_: e.tensor_tensor(out=m2[:], in0=yb[:], in1=g_[:, DM:2 * DM], op=ALU.mult),
                     reads=[yb, g_], writes=[m2])
                P.op("pool", lambda e: e.tensor_tensor(out=mb[:], in0=m1[:], in1=m2[:], op=ALU.add), reads=[m1, m2], writes=[mb])
                for k in range(8):
                    P.op("pe", lambda e, k=k: e.transpose(out=tpB[:, k, :], in_=mb[:, k * 128:(k + 1) * 128], identity=self.identb[:]),
                         reads=[mb, self.identb], writes=[tpB])
                P.op("act", lambda e: e.copy(out=mT[:], in_=tpB[:]), reads=[tpB], writes=[mT])
                for cc in range(2):
                    for k in range(8):
                        P.op("pe", lambda e, cc=cc, k=k: e.matmul(yy[:, cc * 512:(cc + 1) * 512], lhsT=mT[:, k, :],
                                                                 rhs=wo[:, k, cc * 512:(cc + 1) * 512], start=(k == 0), stop=(k == 7)),
                             reads=[mT, wo], writes=[yy])
                P.op("dve", lambda e, r=r: e.tensor_tensor(out=m1[:], in0=yy[:], in1=ga1[r][:], op=ALU.mult),
                     reads=[yy, ga1[r]], writes=[m1])
                P.op("pool", lambda e, x_=x_: e.tensor_tensor(out=x_[:], in0=x_[:], in1=m1[:], op=ALU.add), reads=[x_, m1], writes=[x_])
                P.dma("sp", lambda e, x_=x_, rows=rows: e.dma_start(out=self.X[rows, :], in_=x_[:]), reads=[x_])
            P.barrier()

    def phase_e(self, l, with_ctx, final):
        P, nc, c = self.P, self.nc, self.cfg
        NBUF = 18
        BS = 8
        NBATCH = 128 // BS
        with contextlib.ExitStack() as st:
            f32t = [self.sb(st, "e_cf%d" % i, [128, 8, DM]) for i in range(2)]
            b16t = [self.sb(st, "e_cb%d" % i, [128, 8, DM], BF16) for i in range(2)]
            n = 0
            for (src, off) in ((self.eu, 0), (self.ev, DM)):
                for i in range(16):
                    a, bb = f32t[n % 2], b16t[n % 2]
                    rs = slice(i * 1024, (i + 1) * 1024)
                    P.dma("sp", lambda e, a=a, src=src, rs=rs: e.dma_start(out=a[:], in_=src[l][rs, :].rearrange("(p r) d -> p r d", r=8)),
                          writes=[a])
                    if n % 2 == 0:
                        P.op("act", lambda e, a=a, bb=bb: e.copy(out=bb[:], in_=a[:]), reads=[a], writes=[bb])
                    else:
                        P.op("pool", lambda e, a=a, bb=bb: e.tensor_copy(out=bb[:], in_=a[:]), reads=[a], writes=[bb])
                    P.dma("sp", lambda e, bb=bb, off=off, rs=rs: e.dma_start(
                        out=self.UVB[rs, off:off + DM].rearrange("(p r) d -> p r d", r=8), in_=bb[:]), reads=[bb])
                    n += 1
            P.barrier()
        with contextlib.ExitStack() as st:
            wq = self.sb(st, "e_wq", [128, 8, 2048], BF16)
            skb = self.sb(st, "e_skb", [128, 16, 128], BF16)
            with contextlib.ExitStack() as st2:
                stg = [self.sb(st2, "e_stg%d" % i, [128, 2048]) for i in range(2)]
                for k in range(8):
                    s = stg[k % 2]
                    P.dma("sp", lambda e, s=s, k=k: e.dma_start(out=s[:], in_=self.w_q[l][k * 128:(k + 1) * 128, :]), writes=[s])
                    if k % 2 == 0:
                        P.op("act", lambda e, s=s, k=k: e.copy(out=wq[:, k, :], in_=s[:]), reads=[s], writes=[wq])
                    else:
                        P.op("pool", lambda e, s=s, k=k: e.tensor_copy(out=wq[:, k, :], in_=s[:]), reads=[s], writes=[wq])
                s = stg[0]
                P.dma("sp", lambda e, s=s: e.dma_start(out=s[:].rearrange("p (g n) -> p g n", g=16), in_=self.skT[l].rearrange("g d n -> d g n")),
                      writes=[s])
                P.op("act", lambda e, s=s: e.copy(out=skb[:], in_=s[:].rearrange("p (g n) -> p g n", g=16)), reads=[s], writes=[skb])
                P.barrier()
            A2 = self.sb(st, "e_A2", [128, DM])
            B2 = self.sb(st, "e_B2", [128, DM])
            GA2 = [self.sb(st, "e_GA2_%d" % r, [128, DM]) for r in range(2)]
            for r in range(2):
                P.dma("sp", lambda e, r=r: e.dma_start(out=GA2[r][:], in_=self.AUX[r, 3:4, :].partition_broadcast(128)), writes=[GA2[r]])
            gfin = None
            if final:
                gfin = self.sb(st, "e_gfin", [128, DM])
                P.dma("sp", lambda e: e.dma_start(out=gfin[:], in_=self.gfin.rearrange("(o d) -> o d", o=1).partition_broadcast(128)),
                      writes=[gfin])
            xt = [self.sb(st, "e_x%d" % i, [128, DM]) for i in range(2)]
            ss = [self.sb(st, "e_ss%d" % i, [128, 8]) for i in range(2)]
            h2p = [self.sb(st, "e_h2_%d" % i, [128, DM]) for i in range(2)]
            m32 = self.sb(st, "e_m32", [128, DM])
            junkb = self.sb(st, "e_junkb", [128, DM], BF16)
            junkd = self.sb(st, "e_junkd", [128, DM], BF16)
            h2T = self.sb(st, "e_h2T", [128, 8, 128], BF16)
            qpT = self.sb(st, "e_qpT", [128, 16, 128], BF16)
            sc = self.sb(st, "e_sc", [128, 16, 128])
            s8 = self.sb(st, "e_s8", [128, 16, 16])
            i8 = self.sb(st, "e_i8", [128, 16, 16], U32)
            i8f = self.sb(st, "e_i8f", [128, 16, 16])
            i0s = self.sb(st, "e_i0s", [128, 8, 16])
            cand = self.sb(st, "e_cand", [128, 8, 16, 16])
            cidx = self.sb(st, "e_cidx", [128, 8, 16, 16])
            j256 = self.sb(st, "e_j256", [128, 256])
            best = self.sb(st, "e_best", [128, 8, 16])
            eidf = self.sb(st, "e_eidf", [128, 128])
            eidx2 = [self.sb(st, "e_eidx%d" % i, [128, 128], I32) for i in range(2)]
            ex = self.sb(st, "e_ex", [128, 8, 16])
            sm = self.sb(st, "e_sm", [128, 8, 2])
            ggp = [self.sb(st, "e_g%d" % i, [128, 128]) for i in range(2)]
            actb = [self.sb(st, "e_act%d" % i, [128, BS]) for i in range(2)]
            actc = [[T(None, "actc") for _ in range(BS)] for _ in range(2)]
            ta = [self.sb(st, "e_ta%d" % i, [128, BS]) for i in range(2)]
            tb = [self.sb(st, "e_tb%d" % i, [128, BS]) for i in range(2)]
            wg = [self.sb(st, "e_wg%d" % i, [128, BS]) for i in range(2)]
            Dk = [self.sb(st, "e_Dk%d" % i, [128, BS, 128], BF16) for i in range(2)]
            ring = [self.sb(st, "e_ring%d" % i, [128, 2 * DM], BF16) for i in range(NBUF)]
            acc_ps = self.ps(st, "e_acc", [128, DM])
            tp_ps = self.ps(st, "e_tp", [128, 8, 128])
            qp_ps = [self.ps(st, "e_qp%d" % i, [128, 4, 128]) for i in range(2)]
            sc_ps = self.ps(st, "e_scps", [128, 8, 128])
            tiles = ([0, 1] if with_ctx else []) + list(range(2, c.nt))
            state = {"cur_r": None, "nring": 0}

            def S1(n_):
                t = tiles[n_]
                b = n_ % 2
                r = 1 if t < 2 else 0
                rows = slice(t * 128, (t + 1) * 128)
                eidx = eidx2[b]
                gg = ggp[b]
                h2 = h2p[b]
                if state["cur_r"] != r:
                    for i_, dst in ((0, A2), (1, B2)):
                        P.dma("sp", lambda e, i_=i_, dst=dst, r=r: e.dma_start(out=dst[:], in_=self.AUX[r, i_:i_ + 1, :].partition_broadcast(128)),
                              writes=[dst])
                    state["cur_r"] = r
                x_, ss_ = xt[b], ss[b]
                P.dma("sp", lambda e: e.dma_start(out=x_[:], in_=self.X[rows, :]), writes=[x_])
                P.op("act", lambda e: e.activation(out=junkb[:], in_=x_[:], func=AF.Square, accum_out=ss_[:, 0:1]),
                     reads=[x_], writes=[junkb, ss_])
                P.op("dve", lambda e: e.tensor_scalar(out=ss_[:, 1:2], in0=ss_[:, 0:1], scalar1=1.0 / DM, scalar2=EPS,
                                                      op0=ALU.mult, op1=ALU.add), reads=[ss_], writes=[ss_])
                P.op("act", lambda e: e.activation(out=ss_[:, 2:3], in_=ss_[:, 1:2], func=AF.Sqrt), reads=[ss_], writes=[ss_])
                P.op("dve", lambda e: e.reciprocal(out=ss_[:, 3:4], in_=ss_[:, 2:3]), reads=[ss_], writes=[ss_])
                yield
                P.op("dve", lambda e: e.scalar_tensor_tensor(out=h2[:], in0=x_[:], scalar=ss_[:, 3:4], in1=A2[:],
                                                             op0=ALU.mult, op1=ALU.mult), reads=[x_, ss_, A2], writes=[h2])
                yield
                P.op("dve", lambda e: e.tensor_tensor(out=h2[:], in0=h2[:], in1=B2[:], op=ALU.add), reads=[h2, B2], writes=[h2])
                for k in range(8):
                    P.op("pe", lambda e, k=k: e.transpose(out=tp_ps[:, k, :], in_=h2[:, k * 128:(k + 1) * 128], identity=self.identf[:]),
                         reads=[h2, self.identf], writes=[tp_ps])
                P.op("act", lambda e: e.copy(out=h2T[:, 0:4, :], in_=tp_ps[:, 0:4, :]), reads=[tp_ps], writes=[h2T])
                P.op("act", lambda e: e.copy(out=h2T[:, 4:8, :], in_=tp_ps[:, 4:8, :]), reads=[tp_ps], writes=[h2T])
                yield
                for q4 in range(4):
                    pq = qp_ps[q4 % 2]
                    for bi in range(4):
                        blk = q4 * 4 + bi
                        for k in range(8):
                            P.op("pe", lambda e, pq=pq, bi=bi, blk=blk, k=k: e.matmul(
                                pq[:, bi, :], lhsT=wq[:, k, blk * 128:(blk + 1) * 128], rhs=h2T[:, k, :], start=(k == 0), stop=(k == 7)),
                                reads=[wq, h2T], writes=[pq])
                    P.op("act", lambda e, pq=pq, q4=q4: e.copy(out=qpT[:, q4 * 4:(q4 + 1) * 4, :], in_=pq[:]), reads=[pq], writes=[qpT])
                yield
                for half in range(2):
                    for g8 in range(8):
                        g = half * 8 + g8
                        P.op("pe", lambda e, g=g, g8=g8: e.matmul(sc_ps[:, g8, :], lhsT=qpT[:, g, :], rhs=skb[:, g, :], start=True, stop=True),
                             reads=[qpT, skb], writes=[sc_ps])
                    for q in range(2):
                        P.op("act", lambda e, half=half, q=q: e.copy(out=sc[:, half * 8 + q * 4:half * 8 + (q + 1) * 4, :],
                                                                     in_=sc_ps[:, q * 4:(q + 1) * 4, :]), reads=[sc_ps], writes=[sc])
                yield
                for g in range(16):
                    P.op("dve", lambda e, g=g: e.max(out=s8[:, g, 0:8], in_=sc[:, g, :]), reads=[sc], writes=[s8])
                    P.op("dve", lambda e, g=g: e.max_index(out=i8[:, g, 0:8], in_max=s8[:, g, 0:8], in_values=sc[:, g, :]),
                         reads=[sc, s8], writes=[i8])
                    P.op("dve", lambda e, g=g: e.match_replace(out=sc[:, g, :], in_to_replace=s8[:, g, 0:8], in_values=sc[:, g, :],
                                                               imm_value=-1e30), reads=[sc, s8], writes=[sc])
                    P.op("dve", lambda e, g=g: e.max(out=s8[:, g, 8:16], in_=sc[:, g, :]), reads=[sc], writes=[s8])
                    P.op("dve", lambda e, g=g: e.max_index(out=i8[:, g, 8:16], in_max=s8[:, g, 8:16], in_values=sc[:, g, :]),
                         reads=[sc, s8], writes=[i8])
                    yield
                P.op("dve", lambda e: e.tensor_copy(out=i8f[:], in_=i8[:]), reads=[i8], writes=[i8f])
                s8v = s8[:].rearrange("p (h two) k -> p h two k", two=2)
                i8v = i8f[:].rearrange("p (h two) k -> p h two k", two=2)
                P.op("dve", lambda e: e.tensor_tensor(out=cand[:], in0=s8v[:, :, 0, :].unsqueeze(3).to_broadcast([128, 8, 16, 16]),
                                                      in1=s8v[:, :, 1, :].unsqueeze(2).to_broadcast([128, 8, 16, 16]), op=ALU.add),
                     reads=[s8], writes=[cand])
                yield
                P.op("dve", lambda e: e.tensor_scalar(out=i0s[:], in0=i8v[:, :, 0, :], scalar1=128.0, scalar2=None, op0=ALU.mult),
                     reads=[i8f], writes=[i0s])
                P.op("dve", lambda e: e.tensor_tensor(out=cidx[:], in0=i0s[:].unsqueeze(3).to_broadcast([128, 8, 16, 16]),
                                                      in1=i8v[:, :, 1, :].unsqueeze(2).to_broadcast([128, 8, 16, 16]), op=ALU.add),
                     reads=[i0s, i8f], writes=[cidx])
                yield
                cand3 = cand[:].rearrange("p h a b -> p h (a b)")
                cidx3 = cidx[:].rearrange("p h a b -> p h (a b)")

                def decode(h, k0):
                    for k in range(k0, k0 + 8):
                        P.op("dve", lambda e, h=h, k=k: e.scalar_tensor_tensor(
                            out=j256[:], in0=cand3[:, h, :], scalar=best[:, h, k:k + 1], in1=cidx3[:, h, :], op0=ALU.is_equal, op1=ALU.mult,
                            accum_out=eidf[:, h * 16 + k:h * 16 + k + 1]), reads=[cand, best, cidx], writes=[j256, eidf])

                for h in range(8):
                    P.op("dve", lambda e, h=h: e.max(out=best[:, h, 0:8], in_=cand3[:, h, :]), reads=[cand], writes=[best])
                    decode(h, 0)
                    yield
                    P.op("dve", lambda e, h=h: e.match_replace(out=cand3[:, h, :], in_to_replace=best[:, h, 0:8], in_values=cand3[:, h, :],
                                                               imm_value=-1e30), reads=[cand, best], writes=[cand])
                    P.op("dve", lambda e, h=h: e.max(out=best[:, h, 8:16], in_=cand3[:, h, :]), reads=[cand], writes=[best])
                    decode(h, 8)
                    yield
                P.op("dve", lambda e: e.tensor_scalar(out=eidf[:], in0=eidf[:], scalar1=16383.0, scalar2=0.0, op0=ALU.min, op1=ALU.max),
                     reads=[eidf], writes=[eidf])
                P.op("dve", lambda e: e.tensor_copy(out=eidx[:], in_=eidf[:]), reads=[eidf], writes=[eidx])
                P.op("dve", lambda e: e.tensor_tensor(out=ex[:], in0=best[:], in1=best[:, :, 0:1].to_broadcast([128, 8, 16]), op=ALU.subtract),
                     reads=[best], writes=[ex])
                P.op("act", lambda e: e.activation(out=ex[:], in_=ex[:], func=AF.Exp), reads=[ex], writes=[ex])
                yield
                P.op("dve", lambda e: e.tensor_reduce(out=sm[:, :, 0], in_=ex[:], axis=AX.X, op=ALU.add), reads=[ex], writes=[sm])
                P.op("dve", lambda e: e.reciprocal(out=sm[:, :, 1], in_=sm[:, :, 0]), reads=[sm], writes=[sm])
                P.op("dve", lambda e: e.tensor_tensor(out=gg[:].rearrange("p (h k) -> p h k", h=8), in0=ex[:],
                                                      in1=sm[:, :, 1:2].to_broadcast([128, 8, 16]), op=ALU.mult), reads=[ex, sm], writes=[gg])

            def drain(gen, nmax=None):
                if gen is None:
                    return None
                k = 0
                while nmax is None or k < nmax:
                    try:
                        next(gen)
                    except StopIteration:
                        return None
                    k += 1
                return gen

            def batch_front(n_, k):
                eidx = eidx2[n_ % 2]
                h2 = h2p[n_ % 2]
                kp = k % 2
                bufs = []
                for jj in range(BS):
                    j = k * BS + jj
                    rb = ring[state["nring"] % NBUF]
                    state["nring"] += 1
                    bufs.append(rb)
                    P.dma("pool", lambda e, rb=rb, j=j: e.indirect_dma_start(
                        out=rb[:], out_offset=None, in_=self.UVB[:, :], in_offset=bass.IndirectOffsetOnAxis(ap=eidx[:, j:j + 1], axis=0)),
                        reads=[eidx], writes=[rb])
                for jj in range(BS):
                    rb = bufs[jj]
                    P.op("dve", lambda e, rb=rb, jj=jj: e.scalar_tensor_tensor(
                        out=junkd[:], in0=rb[:, 0:DM], scalar=1.0, in1=h2[:], op0=ALU.mult, op1=ALU.mult, accum_out=actb[kp][:, jj:jj + 1]),
                        reads=[rb, h2], writes=[actc[kp][jj]])
                a_, ta_, tb_ = actb[kp], ta[kp], tb[kp]
                P.op("dve", lambda e: e.tensor_tensor(out=ta_[:], in0=a_[:], in1=a_[:], op=ALU.mult), reads=actc[kp], writes=[ta_])
                P.op("dve", lambda e: e.tensor_scalar(out=ta_[:], in0=ta_[:], scalar1=0.044715, scalar2=1.0, op0=ALU.mult, op1=ALU.add),
                     reads=[ta_], writes=[ta_])
                P.op("dve", lambda e: e.tensor_tensor(out=ta_[:], in0=ta_[:], in1=a_[:], op=ALU.mult), reads=[ta_] + actc[kp], writes=[ta_])
                P.op("act", lambda e: e.activation(out=tb_[:], in_=ta_[:], func=AF.Sigmoid, scale=1.5957691216057308), reads=[ta_], writes=[tb_])
                return bufs

            def batch_back(n_, k, bufs):
                gg = ggp[n_ % 2]
                kp = k % 2
                a_, tb_, wg_, Dk_ = actb[kp], tb[kp], wg[kp], Dk[kp]
                P.op("dve", lambda e: e.tensor_tensor(out=tb_[:], in0=tb_[:], in1=a_[:], op=ALU.mult), reads=[tb_] + actc[kp], writes=[tb_])
                P.op("dve", lambda e: e.tensor_tensor(out=wg_[:], in0=tb_[:], in1=gg[:, k * BS:(k + 1) * BS], op=ALU.mult),
                     reads=[tb_, gg], writes=[wg_] + actc[kp])
                P.op("dve", lambda e: e.tensor_tensor(out=Dk_[:], in0=self.identb[:].unsqueeze(1).to_broadcast([128, BS, 128]),
                                                      in1=wg_[:].unsqueeze(2).to_broadcast([128, BS, 128]), op=ALU.mult),
                     reads=[self.identb, wg_], writes=[Dk_])
                for jj in range(BS):
                    j = k * BS + jj
                    rb = bufs[jj]
                    for hf in range(2):
                        P.op("pe", lambda e, rb=rb, jj=jj, j=j, hf=hf: e.matmul(
                            acc_ps[:, hf * 512:(hf + 1) * 512], lhsT=Dk_[:, jj, :], rhs=rb[:, DM + hf * 512:DM + (hf + 1) * 512],
                            start=(j == 0), stop=(j == 127)), reads=[Dk_, rb], writes=[acc_ps])

            def S3(n_):
                t = tiles[n_]
                b = n_ % 2
                r = 1 if t < 2 else 0
                rows = slice(t * 128, (t + 1) * 128)
                x_, ss_ = xt[b], ss[b]
                P.op("dve", lambda e: e.tensor_tensor(out=m32[:], in0=acc_ps[:], in1=GA2[r][:], op=ALU.mult), reads=[acc_ps, GA2[r]], writes=[m32])
                P.op("dve", lambda e: e.tensor_tensor(out=x_[:], in0=x_[:], in1=m32[:], op=ALU.add), reads=[x_, m32], writes=[x_])
                if final and t >= 2:
                    P.op("act", lambda e: e.activation(out=junkb[:], in_=x_[:], func=AF.Square, accum_out=ss_[:, 4:5]),
                         reads=[x_], writes=[junkb, ss_])
                    P.op("dve", lambda e: e.tensor_scalar(out=ss_[:, 5:6], in0=ss_[:, 4:5], scalar1=1.0 / DM, scalar2=EPS,
                                                          op0=ALU.mult, op1=ALU.add), reads=[ss_], writes=[ss_])
                    P.op("act", lambda e: e.activation(out=ss_[:, 6:7], in_=ss_[:, 5:6], func=AF.Sqrt), reads=[ss_], writes=[ss_])
                    P.op("dve", lambda e: e.reciprocal(out=ss_[:, 7:8], in_=ss_[:, 6:7]), reads=[ss_], writes=[ss_])
                    P.op("dve", lambda e: e.scalar_tensor_tensor(out=x_[:], in0=x_[:], scalar=ss_[:, 7:8], in1=gfin[:],
                                                                 op0=ALU.mult, op1=ALU.mult), reads=[x_, ss_, gfin], writes=[x_])
                    P.dma("sp", lambda e: e.dma_start(out=self.Y[(t - 2) * 128:(t - 1) * 128, :], in_=x_[:]), reads=[x_])
                else:
                    P.dma("sp", lambda e: e.dma_start(out=self.X[rows, :], in_=x_[:]), reads=[x_])

            nT = len(tiles)
            drain(S1(0))
            for n_ in range(nT):
                gen = S1(n_ + 1) if n_ + 1 < nT else None
                prev = None
                for k in range(NBATCH):
                    bufs = batch_front(n_, k)
                    if prev is not None:
                        batch_back(n_, k - 1, prev)
                    prev = bufs
                    gen = drain(gen, 3)
                batch_back(n_, NBATCH - 1, prev)
                drain(gen)
                S3(n_)
            P.barrier()

    def build(self):
        c = self.cfg
        with contextlib.ExitStack() as st:
            self.consts(st)
            for l in c.layers:
                last = (l == c.depth - 1)
                with contextlib.ExitStack() as modst:
                    if "M" in c.phases:
                        self.phase_mod(l, modst)
                    if "A" in c.phases:
                        self.phase_a(l)
                if "B" in c.phases:
                    self.phase_b(l)
                if "C" in c.phases:
                    self.phase_c(l, not last)
                if "D" in c.phases:
                    self.phase_d(l, not last)
                if "E" in c.phases:
                    self.phase_e(l, not last, last and c.final)
            self.P.barrier()
            with self.nc.allow_non_contiguous_dma(reason='small strided layout DMAs'):
                self.P.build()
        return self.nc


def host_consts(n_lat):
    p = np.arange(64)
    f = p % 64
    i = f % 16
    freq = (10000.0 ** (-(i.astype(np.float32)) / 16.0)).astype(np.float32)
    rope = np.zeros((n_lat, 64, 2, 128), np.float32)
    j = np.arange(128)
    for m in range(n_lat):
        row = (2 * m + j // 64).astype(np.float32)
        col = (j % 64).astype(np.float32)
        pos = np.where((f < 32)[:, None], row[None, :], col[None, :]).astype(np.float32)
        ang = pos * freq[:, None]
        rope[m, :, 0, :] = np.cos(ang)
        rope[m, :, 1, :] = np.sin(ang)
    rotm = np.zeros((64, 64), np.float32)
    for m_ in range(64):
        fm = m_ % 64
        if (fm % 32) < 16:
            rotm[m_ + 16, m_] = -1.0
        else:
            rotm[m_ - 16, m_] = 1.0
    return rope, rotm


def host_bias(rpb, pats):
    L = rpb.shape[0]
    out = np.empty((L, len(pats), 128, 8, 5, 128), np.float32)
    for pi, (valid, roff, coff) in enumerate(pats):
        g = rpb[:, :, roff, coff]
        g = np.where(valid[None, None], g, np.float32(NEG))
        out[:, pi] = g.transpose(0, 3, 1, 2, 4)
    return out


_CACHE = {}


def make_inputs(inputs, cfg, cores):
    n_lat = cfg.n_lat
    rope, rotm = host_consts(n_lat)
    _, _, pats = na_patterns(n_lat)
    shared = dict(
        w_mod=inputs["w_mod"], b_mod=inputs["b_mod"], g_norm1=inputs["g_norm1"], w_in=inputs["w_in"],
        w_alpha_f=inputs["w_alpha_f"], b_alpha_f=inputs["b_alpha_f"], w_alpha_b=inputs["w_alpha_b"], b_alpha_b=inputs["b_alpha_b"],
        g_gla=inputs["g_gla"], biasT=host_bias(np.asarray(inputs["rpb"]), pats), w_proj_gla=inputs["w_proj_gla"],
        w_proj_na=inputs["w_proj_na"], w_out=inputs["w_out"], g_norm2=inputs["g_norm2"], w_query=inputs["w_query"],
        skT=np.ascontiguousarray(np.asarray(inputs["sub_keys"]).reshape(-1, 16, 128, 128).transpose(0, 1, 3, 2)),
        expert_u=inputs["expert_u"], expert_v=inputs["expert_v"], g_final=inputs["g_final"], rope=rope, rotm=rotm)
    shared = {k: np.ascontiguousarray(np.asarray(v, dtype=np.float32)) for k, v in shared.items()}
    maps = []
    for b in cores:
        xin = np.concatenate([np.asarray(inputs["ctx"][b]), np.asarray(inputs["x"][b][:n_lat * 128])], axis=0).astype(np.float32)
        cv = np.stack([np.asarray(inputs["c"][b]), np.asarray(inputs["c_ctx"])], axis=-1).astype(np.float32)
        cvec = np.ascontiguousarray(cv.reshape(8, 128, 2).transpose(1, 0, 2))
        m = dict(shared)
        m["xin"] = np.ascontiguousarray(xin)
        m["cvec"] = cvec
        maps.append(m)
    return maps


def kernel(**inputs):
    cfg = Cfg()
    if "nc" not in _CACHE:
        _CACHE["nc"] = Builder(cfg).build()
    nc = _CACHE["nc"]
    maps = make_inputs(inputs, cfg, list(range(8)))
    res = run_bass_kernel_spmd(nc, maps, core_ids=list(range(8)))
    out = np.stack([np.asarray(r["Y"]) for r in res.results], axis=0).astype(np.float32)
    return out
```

```python
import contextlib
import numpy as np
import concourse.bass as bass
import concourse.mybir as mybir
from concourse.bass_utils import run_bass_kernel_spmd

F32 = mybir.dt.float32
BF16 = mybir.dt.bfloat16
I32 = mybir.dt.int32
U32 = mybir.dt.uint32
AF = mybir.ActivationFunctionType
ALU = mybir.AluOpType
AX = mybir.AxisListType

N_DMA_SLOTS = 20
DM = 1024
EPS = 1e-6
NEG = -30000.0


class T:
    __slots__ = ("h", "w", "r", "name")

    def __init__(self, h=None, name=""):
        self.h = h
        self.w = None
        self.r = []
        self.name = name

    def __getitem__(self, k):
        return self.h[k]


class Prog:
    ENG = ("pe", "act", "dve", "pool", "sp")

    def __init__(self, nc):
        self.nc = nc
        self.ops = {e: [] for e in self.ENG}
        self.seq = {e: 0 for e in self.ENG}
        self.known = {e: {} for e in self.ENG}
        self.known_ver = {e: None for e in self.ENG}
        self.dcount = {}
        self.dnext = {e: 0 for e in self.ENG}
        self.last_dma = {}
        self.n_instr = 0

    def _snap(self, eng):
        s = self.known_ver[eng]
        if s is None:
            s = dict(self.known[eng])
            self.known_ver[eng] = s
        return s

    def _learn(self, eng, d):
        kn = self.known[eng]
        ch = False
        for k, v in d.items():
            if kn.get(k, 0) < v:
                kn[k] = v
                ch = True
        if ch:
            self.known_ver[eng] = None

    @staticmethod
    def _dep_events(reads, writes):
        ev = []
        for r in reads:
            if r.w is not None:
                ev.append(r.w)
        for w in writes:
            if w.w is not None:
                ev.append(w.w)
            ev.extend(w.r)
        return ev

    def _waits_for(self, eng, events):
        kn = self.known[eng]
        waits = []
        for ev in sorted(events, key=lambda e: -e[1]):
            sk, val, src, snap = ev
            if src == eng and eng == "pe":
                continue
            if kn.get(sk, 0) >= val:
                continue
            waits.append((sk, val))
            kn[sk] = val
            self.known_ver[eng] = None
            if snap is not None:
                self._learn(eng, snap)
        out = []
        seen = {}
        for sk, val in waits:
            if seen.get(sk, 0) >= val:
                continue
            seen[sk] = val
            out.append((sk, val))
        return out

    def op(self, eng, fn, reads=(), writes=()):
        events = self._dep_events(reads, writes)
        waits = self._waits_for(eng, events)
        self.seq[eng] += 1
        me = (("e", eng), self.seq[eng], eng, self._snap(eng))
        self.ops[eng].append((waits, fn, (("e", eng), 1)))
        for r in reads:
            r.r.append(me)
        for w in writes:
            w.w = me
            w.r = []
        self.n_instr += 1 + max(0, len(waits) - 1)
        return me

    def dma(self, q, fn, reads=(), writes=()):
        events = self._dep_events(reads, writes)
        slot = self.dnext[q] % N_DMA_SLOTS
        self.dnext[q] += 1
        sk = ("d", q, slot)
        cnt = self.dcount.get(sk, 0)
        if cnt > 0:
            events = list(events) + [self.last_dma[sk]]
        waits = self._waits_for(q, events)
        self.dcount[sk] = cnt + 16
        me = (sk, cnt + 16, "dma", self._snap(q))
        self.last_dma[sk] = me
        self.ops[q].append((waits, fn, (sk, 16)))
        for r in reads:
            r.r.append(me)
        for w in writes:
            w.w = me
            w.r = []
        self.n_instr += 1 + max(0, len(waits) - 1)
        return me

    def barrier(self):
        events = []
        for e in self.ENG:
            if self.seq[e] > 0:
                events.append((("e", e), self.seq[e], e, None))
        for sk, ev in self.last_dma.items():
            events.append(ev)
        for e in self.ENG:
            kn = self.known[e]
            waits = []
            for sk, val, src, snap in events:
                if kn.get(sk, 0) >= val:
                    continue
                kn[sk] = val
                waits.append((sk, val))
            self.known_ver[e] = None
            if waits:
                self.ops[e].append((waits, None, None))
                self.n_instr += len(waits)

    def build(self):
        nc = self.nc
        keys = set()
        for e in self.ENG:
            for waits, fn, inc in self.ops[e]:
                for sk, _ in waits:
                    keys.add(sk)
                if inc is not None:
                    keys.add(inc[0])
        keys = sorted(keys, key=str)
        with contextlib.ExitStack() as st:
            semh = {}
            for i, k in enumerate(keys):
                semh[k] = st.enter_context(nc.semaphore("s%d" % i))
            block = st.enter_context(nc.Block())

            def runner(e):
                def run(engh):
                    for waits, fn, inc in self.ops[e]:
                        if fn is None:
                            for sk, val in waits:
                                engh.wait_ge(semh[sk], val)
                            continue
                        for sk, val in waits[1:]:
                            engh.wait_ge(semh[sk], val)
                        ins = fn(engh)
                        if waits:
                            ins._wait_ge(semh[waits[0][0]], waits[0][1])
                        ins.then_inc(semh[inc[0]], inc[1])
                return run

            block.tensor(runner("pe"))
            block.scalar(runner("act"))
            block.vector(runner("dve"))
            block.gpsimd(runner("pool"))
            block.sync(runner("sp"))
        return nc


class Cfg:
    def __init__(self, n_lat=64, layers=(0, 1, 2, 3), depth=4, debug=False, phases="MABCDE", final=True):
        self.n_lat = n_lat
        self.nt = n_lat + 2
        self.layers = tuple(layers)
        self.depth = depth
        self.debug = debug
        self.phases = phases
        self.final = final


def na_patterns(n_lat):
    rows = n_lat * 2
    pats, ids, starts, keymap = [], [], [], {}
    kp = np.arange(128)
    for m in range(n_lat):
        st = int(np.clip(m - 2, 0, n_lat - 5))
        j = np.arange(128)
        r = 2 * m + j // 64
        c = j % 64
        r0 = np.clip(r - 4, 0, rows - 8)
        c0 = np.clip(c - 8, 0, 64 - 16)
        kb = np.arange(5)
        kr = (st * 2 + kb[:, None] * 2 + (kp[None, :] // 64))[:, :, None]
        kc = (kp % 64)[None, :, None] + np.zeros((5, 1, 1), np.int64)
        valid = (kr >= r0[None, None, :]) & (kr < r0[None, None, :] + 8) & (kc >= c0[None, None, :]) & (kc < c0[None, None, :] + 16)
        roff = np.where(valid, kr - r[None, None, :] + 7, 0)
        coff = np.where(valid, kc - c[None, None, :] + 15, 0)
        key = (valid.tobytes(), roff.tobytes(), coff.tobytes())
        if key not in keymap:
            keymap[key] = len(pats)
            pats.append((valid, roff, coff))
        ids.append(keymap[key])
        starts.append(st)
    return ids, starts, pats


class Builder:
    def __init__(self, cfg):
        self.cfg = cfg
        nc = bass.Bass("TRN2", target_bir_lowering=False)
        self.nc = nc
        self.P = Prog(nc)
        self.out_names = []
        self.pat_ids, self.pat_starts, self.pats = na_patterns(cfg.n_lat)
        self.npat = len(self.pats)
        cnt = np.bincount(self.pat_ids)
        self.pat_main = int(np.argmax(cnt))
        self._declare()

    def dram_in(self, name, shape, dt=F32):
        return self.nc.dram_tensor(name, list(shape), dt, kind="ExternalInput").ap()

    def dram_scr(self, name, shape, dt=F32):
        kind = "ExternalOutput" if self.cfg.debug else "Internal"
        if self.cfg.debug:
            self.out_names.append(name)
        return self.nc.dram_tensor(name, list(shape), dt, kind=kind).ap()

    def _declare(self):
        c = self.cfg
        NT, NL = c.nt, c.depth
        TOK = NT * 128
        di = self.dram_in
        self.xin = di("xin", [TOK, DM])
        self.cvec = di("cvec", [128, 8, 2])
        self.w_mod = di("w_mod", [NL, DM, 6 * DM])
        self.b_mod = di("b_mod", [NL, 6 * DM])
        self.g1 = di("g_norm1", [NL, DM])
        self.w_in = di("w_in", [NL, DM, 5152])
        self.waf = di("w_alpha_f", [NL, 16, 256])
        self.baf = di("b_alpha_f", [NL, 256])
        self.wab = di("w_alpha_b", [NL, 16, 256])
        self.bab = di("b_alpha_b", [NL, 256])
        self.ggla = di("g_gla", [NL, 128])
        self.biasT = di("biasT", [NL, self.npat, 128, 8, 5, 128])
        self.w_pg = di("w_proj_gla", [NL, 512, DM])
        self.w_pn = di("w_proj_na", [NL, 512, DM])
        self.w_o = di("w_out", [NL, DM, DM])
        self.g2 = di("g_norm2", [NL, DM])
        self.w_q = di("w_query", [NL, DM, 2048])
        self.skT = di("skT", [NL, 16, 128, 128])
        self.eu = di("expert_u", [NL, 16384, DM])
        self.ev = di("expert_v", [NL, 16384, DM])
        self.gfin = di("g_final", [DM])
        self.rope = di("rope", [c.n_lat, 64, 2, 128])
        self.rotm = di("rotm", [64, 64])
        ds = self.dram_scr
        self.X = ds("X", [TOK, DM])
        self.AUX = ds("AUX", [2, 4, DM])
        self.QKT = ds("QKT", [NT, 64, 8, 128], BF16)
        self.ZT = ds("ZT", [32, TOK])
        self.V = ds("V", [TOK, 512], BF16)
        self.RG = ds("RG", [TOK, 512], BF16)
        self.NQT = ds("NQT", [NT, 128, 4, 128], BF16)
        self.NKT = ds("NKT", [NT, 128, 4, 128], BF16)
        self.NVX = ds("NVX", [TOK, 520], BF16)
        self.G = ds("G", [TOK, 2048], BF16)
        self.OF = ds("OF", [TOK, 512])
        self.OB = ds("OB", [TOK, 512])
        self.NAO = ds("NAO", [TOK, 512], BF16)
        self.UVB = ds("UVB", [16384, 2 * DM], BF16)
        self.Y = self.nc.dram_tensor("Y", [c.n_lat * 128, DM], F32, kind="ExternalOutput").ap()

    def sb(self, st, name, shape, dt=F32):
        self._uid = getattr(self, "_uid", 0) + 1
        name = "%s_u%d" % (name, self._uid)
        h = st.enter_context(self.nc.sbuf_tensor(name, list(shape), dt))
        return T(h, name)

    def ps(self, st, name, shape, dt=F32):
        self._uid = getattr(self, "_uid", 0) + 1
        name = "%s_u%d" % (name, self._uid)
        h = st.enter_context(self.nc.psum_tensor(name, list(shape), dt))
        return T(h, name)

    def consts(self, st):
        P = self.P
        sb = self.sb
        self.identf = sb(st, "identf", [128, 128], F32)
        self.identb = sb(st, "identb", [128, 128], BF16)
        self.ones_row = sb(st, "ones_row", [1, 128], F32)
        self.triF = sb(st, "triF", [128, 128], F32)
        self.triB = sb(st, "triB", [128, 128], F32)
        self.mF = sb(st, "mF", [64, 64], F32)
        self.mB = sb(st, "mB", [64, 64], F32)
        self.rotb = sb(st, "rotb", [64, 64], BF16)
        self.cact = sb(st, "cact", [128, 8, 2], F32)
        idf, idb = self.identf, self.identb
        P.op("pool", lambda e: e.memset(idf[:], 1.0), writes=[idf])
        P.op("pool", lambda e: e.affine_select(out=idf[:], in_=idf[:], pattern=[[-1, 128]], compare_op=ALU.is_equal,
                                               fill=0.0, base=0, channel_multiplier=1), reads=[idf], writes=[idf])
        P.op("pool", lambda e: e.tensor_copy(out=idb[:], in_=idf[:]), reads=[idf], writes=[idb])
        P.op("pool", lambda e: e.memset(self.ones_row[:], 1.0), writes=[self.ones_row])
        v = -1.0 / 16.0
        tF, tB = self.triF, self.triB
        P.op("pool", lambda e: e.memset(tF[:], v), writes=[tF])
        P.op("pool", lambda e: e.affine_select(out=tF[:], in_=tF[:], pattern=[[1, 128]], compare_op=ALU.is_ge,
                                               fill=0.0, base=0, channel_multiplier=-1), reads=[tF], writes=[tF])
        P.op("pool", lambda e: e.memset(tF[0:64, 64:128], 0.0), reads=[tF], writes=[tF])
        P.op("pool", lambda e: e.memset(tB[:], v), writes=[tB])
        P.op("pool", lambda e: e.affine_select(out=tB[:], in_=tB[:], pattern=[[-1, 128]], compare_op=ALU.is_ge,
                                               fill=0.0, base=0, channel_multiplier=1), reads=[tB], writes=[tB])
        P.op("pool", lambda e: e.memset(tB[64:128, 0:64], 0.0), reads=[tB], writes=[tB])
        mF, mB = self.mF, self.mB
        P.op("pool", lambda e: e.memset(mF[:], 1.0), writes=[mF])
        P.op("pool", lambda e: e.affine_select(out=mF[:], in_=mF[:], pattern=[[1, 64]], compare_op=ALU.is_ge,
                                               fill=0.0, base=0, channel_multiplier=-1), reads=[mF], writes=[mF])
        P.op("pool", lambda e: e.memset(mB[:], 1.0), writes=[mB])
        P.op("pool", lambda e: e.affine_select(out=mB[:], in_=mB[:], pattern=[[-1, 64]], compare_op=ALU.is_ge,
                                               fill=0.0, base=0, channel_multiplier=1), reads=[mB], writes=[mB])
        with contextlib.ExitStack() as s2:
            rf = self.sb(s2, "rotf", [64, 64], F32)
            cv = self.sb(s2, "cvt", [128, 8, 2], F32)
            sg = self.sb(s2, "csg", [128, 8, 2], F32)
            P.dma("sp", lambda e: e.dma_start(out=rf[:], in_=self.rotm[:, :]), writes=[rf])
            P.op("dve", lambda e: e.tensor_copy(out=self.rotb[:], in_=rf[:]), reads=[rf], writes=[self.rotb])
            P.dma("sp", lambda e: e.dma_start(out=cv[:], in_=self.cvec[:, :, :]), writes=[cv])
            P.op("act", lambda e: e.activation(out=sg[:], in_=cv[:], func=AF.Sigmoid), reads=[cv], writes=[sg])
            P.op("dve", lambda e: e.tensor_tensor(out=self.cact[:], in0=cv[:], in1=sg[:], op=ALU.mult),
                 reads=[cv, sg], writes=[self.cact])
            P.barrier()

    def phase_mod(self, l, modst):
        P, nc = self.P, self.nc
        self.A1T = self.sb(modst, "A1T", [128, 8, 2])
        self.B1T = self.sb(modst, "B1T", [128, 8, 2])
        with contextlib.ExitStack() as st:
            wm = [self.sb(st, "wm%d" % i, [128, 8, 1024]) for i in range(2)]
            bmT = self.sb(st, "bmT", [128, 48])
            g1T = self.sb(st, "g1T", [128, 8])
            g2T = self.sb(st, "g2T", [128, 8])
            modT = self.sb(st, "modT", [128, 48, 2])
            tmp = self.sb(st, "modtmp", [128, 8, 2])
            A2T = self.sb(st, "A2T", [128, 8, 2])
            mps = self.ps(st, "mod_ps", [128, 48, 2])
            ncd = nc.allow_non_contiguous_dma(reason="tiny per-feature vectors")
            st.enter_context(ncd)
            P.dma("sp", lambda e: e.dma_start(out=bmT[:], in_=self.b_mod[l].rearrange("(c p) -> p c", p=128)), writes=[bmT])
            P.dma("sp", lambda e: e.dma_start(out=g1T[:], in_=self.g1[l].rearrange("(c p) -> p c", p=128)), writes=[g1T])
            P.dma("sp", lambda e: e.dma_start(out=g2T[:], in_=self.g2[l].rearrange("(c p) -> p c", p=128)), writes=[g2T])
            for g in range(6):
                w = wm[g % 2]
                P.dma("sp", lambda e, w=w, g=g: e.dma_start(
                    out=w[:], in_=self.w_mod[l][:, g * 1024:(g + 1) * 1024].rearrange("(k p) n -> p k n", p=128)), writes=[w])
                for b in range(8):
                    blk = g * 8 + b
                    for k in range(8):
                        P.op("pe", lambda e, w=w, b=b, k=k, blk=blk: e.matmul(
                            mps[:, blk, :], lhsT=w[:, k, b * 128:(b + 1) * 128], rhs=self.cact[:, k, :],
                            start=(k == 0), stop=(k == 7)), reads=[w, self.cact], writes=[mps])
            P.op("dve", lambda e: e.tensor_tensor(out=modT[:], in0=mps[:], in1=bmT[:].unsqueeze(2).to_broadcast([128, 48, 2]),
                                                  op=ALU.add), reads=[mps, bmT], writes=[modT])
            P.op("dve", lambda e: e.tensor_scalar(out=tmp[:], in0=modT[:, 8:16, :], scalar1=1.0, scalar2=None, op0=ALU.add),
                 reads=[modT], writes=[tmp])
            P.op("dve", lambda e: e.tensor_tensor(out=self.A1T[:], in0=tmp[:], in1=g1T[:].unsqueeze(2).to_broadcast([128, 8, 2]),
                                                  op=ALU.mult), reads=[tmp, g1T], writes=[self.A1T])
            P.op("dve", lambda e: e.tensor_copy(out=self.B1T[:], in_=modT[:, 0:8, :]), reads=[modT], writes=[self.B1T])
            P.op("dve", lambda e: e.tensor_scalar(out=tmp[:], in0=modT[:, 32:40, :], scalar1=1.0, scalar2=None, op0=ALU.add),
                 reads=[modT], writes=[tmp])
            P.op("dve", lambda e: e.tensor_tensor(out=A2T[:], in0=tmp[:], in1=g2T[:].unsqueeze(2).to_broadcast([128, 8, 2]),
                                                  op=ALU.mult), reads=[tmp, g2T], writes=[A2T])
            srcs = [(A2T, None), (modT, 24), (modT, 16), (modT, 40)]
            for i, (t, off) in enumerate(srcs):
                for r in range(2):
                    if off is None:
                        P.dma("sp", lambda e, t=t, i=i, r=r: e.dma_start(
                            out=self.AUX[r, i, :].rearrange("(k p) -> p k", p=128), in_=t[:, :, r]), reads=[t])
                    else:
                        P.dma("sp", lambda e, t=t, i=i, r=r, off=off: e.dma_start(
                            out=self.AUX[r, i, :].rearrange("(k p) -> p k", p=128), in_=t[:, off:off + 8, r]), reads=[t])
            P.barrier()

    def phase_a(self, l):
        P, nc, c = self.P, self.nc, self.cfg
        NT = c.nt
        with contextlib.ExitStack() as st:
            wb = self.sb(st, "a_wb", [128, 8, 5152], BF16)
            stg = [self.sb(st, "a_stg%d" % i, [128, 2576]) for i in range(2)]
            for k in range(8):
                for hf in range(2):
                    s = stg[hf]
                    cs = slice(hf * 2576, (hf + 1) * 2576)
                    P.dma("sp", lambda e, s=s, k=k, cs=cs: e.dma_start(out=s[:], in_=self.w_in[l][k * 128:(k + 1) * 128, cs]), writes=[s])
                    if hf == 0:
                        P.op("act", lambda e, s=s, k=k, cs=cs: e.copy(out=wb[:, k, cs], in_=s[:]), reads=[s], writes=[wb])
                    else:
                        P.op("pool", lambda e, s=s, k=k, cs=cs: e.tensor_copy(out=wb[:, k, cs], in_=s[:]), reads=[s], writes=[wb])
            xt = [self.sb(st, "a_x%d" % i, [128, DM]) for i in range(2)]
            junk = self.sb(st, "a_junk", [128, DM], BF16)
            ss = [self.sb(st, "a_ss%d" % i, [128, 4]) for i in range(2)]
            xn = [self.sb(st, "a_xn%d" % i, [128, DM]) for i in range(2)]
            hT = [self.sb(st, "a_hT%d" % i, [128, 8, 128], BF16) for i in range(2)]
            vt = [self.sb(st, "a_v%d" % i, [128, 512], BF16) for i in range(2)]
            rt = [self.sb(st, "a_r%d" % i, [128, 512], BF16) for i in range(2)]
            rsg = [self.sb(st, "a_rsg%d" % i, [128, 512]) for i in range(2)]
            nvx = [self.sb(st, "a_nvx%d" % i, [128, 8, 65], BF16) for i in range(2)]
            gt = [self.sb(st, "a_g%d" % i, [128, 2048], BF16) for i in range(2)]
            qkb = [self.sb(st, "a_qkb%d" % i, [64, 8, 128], BF16) for i in range(2)]
            qkr = [self.sb(st, "a_qkr%d" % i, [64, 8, 128], BF16) for i in range(2)]
            t1 = [self.sb(st, "a_t1%d" % i, [64, 8, 128]) for i in range(2)]
            t2 = [self.sb(st, "a_t2%d" % i, [64, 8, 128]) for i in range(2)]
            zt = [self.sb(st, "a_z%d" % i, [32, 128]) for i in range(2)]
            nqt = [self.sb(st, "a_nq%d" % i, [128, 4, 128], BF16) for i in range(2)]
            nkt = [self.sb(st, "a_nk%d" % i, [128, 4, 128], BF16) for i in range(2)]
            rp = [self.sb(st, "a_rp%d" % i, [64, 2, 128]) for i in range(2)]
            tps = self.ps(st, "a_tps", [128, 8, 128])
            tok = [self.ps(st, "a_tok%d" % i, [128, 512]) for i in range(2)]
            fm = [self.ps(st, "a_fm%d" % i, [128, 4, 128]) for i in range(2)]
            rot = self.ps(st, "a_rot", [64, 8, 128])
            for i in range(2):
                P.op("pool", lambda e, i=i: e.memset(nvx[i][:], 1.0), writes=[nvx[i]])
            C_Q, C_K, C_V, C_Z, C_R, C_NQ, C_NK, C_NV, C_G = 0, 256, 512, 1024, 1056, 1568, 2080, 2592, 3104
            ntok = 0
            nfm = 0
            for t in range(NT):
                b = t % 2
                r = 1 if t < 2 else 0
                src = self.xin if l == 0 else self.X
                x_, ss_, xn_, hT_ = xt[b], ss[b], xn[b], hT[b]
                P.dma("sp", lambda e, x_=x_, t=t, src=src: e.dma_start(out=x_[:], in_=src[t * 128:(t + 1) * 128, :]), writes=[x_])
                P.op("act", lambda e, x_=x_, ss_=ss_: e.activation(out=junk[:], in_=x_[:], func=AF.Square, accum_out=ss_[:, 0:1]),
                     reads=[x_], writes=[junk, ss_])
                P.op("dve", lambda e, ss_=ss_: e.tensor_scalar(out=ss_[:, 1:2], in0=ss_[:, 0:1], scalar1=1.0 / DM, scalar2=EPS,
                                                               op0=ALU.mult, op1=ALU.add), reads=[ss_], writes=[ss_])
                P.op("act", lambda e, ss_=ss_: e.activation(out=ss_[:, 2:3], in_=ss_[:, 1:2], func=AF.Sqrt), reads=[ss_], writes=[ss_])
                P.op("dve", lambda e, ss_=ss_: e.reciprocal(out=ss_[:, 3:4], in_=ss_[:, 2:3]), reads=[ss_], writes=[ss_])
                P.op("dve", lambda e, x_=x_, xn_=xn_, ss_=ss_: e.tensor_scalar(out=xn_[:], in0=x_[:], scalar1=ss_[:, 3:4], scalar2=None,
                                                                               op0=ALU.mult), reads=[x_, ss_], writes=[xn_])
                for k in range(8):
                    P.op("pe", lambda e, xn_=xn_, k=k: e.transpose(out=tps[:, k, :], in_=xn_[:, k * 128:(k + 1) * 128],
                                                                   identity=self.identf[:]), reads=[xn_, self.identf], writes=[tps])
                for k in range(8):
                    P.op("act", lambda e, hT_=hT_, k=k, r=r: e.activation(
                        out=hT_[:, k, :], in_=tps[:, k, :], func=AF.Identity, scale=self.A1T[:, k, r:r + 1],
                        bias=self.B1T[:, k, r:r + 1]), reads=[tps, self.A1T, self.B1T], writes=[hT_])
                tm = [("v", C_V), ("r", C_R), ("nv", C_NV), ("g0", C_G), ("g1", C_G + 512), ("g2", C_G + 1024), ("g3", C_G + 1536)]
                for nm, c0 in tm:
                    pt = tok[ntok % 2]
                    ntok += 1
                    for k in range(8):
                        P.op("pe", lambda e, pt=pt, hT_=hT_, k=k, c0=c0: e.matmul(
                            pt[:], lhsT=hT_[:, k, :], rhs=wb[:, k, c0:c0 + 512], start=(k == 0), stop=(k == 7)),
                            reads=[hT_, wb], writes=[pt])
                    if nm == "v":
                        P.op("dve", lambda e, pt=pt, b=b: e.tensor_copy(out=vt[b][:], in_=pt[:]), reads=[pt], writes=[vt[b]])
                    elif nm == "r":
                        P.op("act", lambda e, pt=pt, b=b: e.activation(out=rsg[b][:], in_=pt[:], func=AF.Sigmoid),
                             reads=[pt], writes=[rsg[b]])
                        P.op("dve", lambda e, pt=pt, b=b: e.tensor_tensor(out=rt[b][:], in0=pt[:], in1=rsg[b][:], op=ALU.mult),
                             reads=[pt, rsg[b]], writes=[rt[b]])
                    elif nm == "nv":
                        P.op("dve", lambda e, pt=pt, b=b: e.tensor_copy(
                            out=nvx[b][:, :, 0:64], in_=pt[:].rearrange("p (h d) -> p h d", h=8)), reads=[pt], writes=[nvx[b]])
                    else:
                        gi = int(nm[1])
                        P.op("act", lambda e, pt=pt, b=b, gi=gi: e.activation(
                            out=gt[b][:, gi * 512:(gi + 1) * 512], in_=pt[:], func=AF.Sigmoid), reads=[pt], writes=[gt[b]])
                groups = [("q", [C_Q + i * 64 for i in range(4)]), ("k", [C_K + i * 64 for i in range(4)]), ("z", [C_Z]),
                          ("nq", [C_NQ + i * 128 for i in range(4)]), ("nk", [C_NK + i * 128 for i in range(4)])]
                for nm, cols in groups:
                    pf = fm[nfm % 2]
                    nfm += 1
                    for bi, c0 in enumerate(cols):
                        M = 32 if nm == "z" else (64 if nm in ("q", "k") else 128)
                        for k in range(8):
                            P.op("pe", lambda e, pf=pf, hT_=hT_, k=k, c0=c0, bi=bi, M=M: e.matmul(
                                pf[0:M, bi, :], lhsT=wb[:, k, c0:c0 + M], rhs=hT_[:, k, :], start=(k == 0), stop=(k == 7)),
                                reads=[hT_, wb], writes=[pf])
                    if nm in ("q", "k"):
                        o4 = 0 if nm == "q" else 4
                        P.op("act", lambda e, pf=pf, b=b, o4=o4: e.copy(out=qkb[b][:, o4:o4 + 4, :], in_=pf[0:64, :, :]), reads=[pf], writes=[qkb[b]])
                    elif nm == "z":
                        P.op("dve", lambda e, pf=pf, b=b: e.tensor_copy(out=zt[b][:], in_=pf[0:32, 0, :]), reads=[pf], writes=[zt[b]])
                    elif nm == "nq":
                        P.op("act", lambda e, pf=pf, b=b: e.copy(out=nqt[b][:], in_=pf[:]), reads=[pf], writes=[nqt[b]])
                    else:
                        P.op("dve", lambda e, pf=pf, b=b: e.tensor_copy(out=nkt[b][:], in_=pf[:]), reads=[pf], writes=[nkt[b]])
                if t >= 2:
                    P.dma("sp", lambda e, b=b, t=t: e.dma_start(out=rp[b][:], in_=self.rope[t - 2]), writes=[rp[b]])
                    for bi in range(8):
                        P.op("pe", lambda e, b=b, bi=bi: e.matmul(rot[:, bi, :], lhsT=self.rotb[:], rhs=qkb[b][:, bi, :],
                                                                 start=True, stop=True), reads=[qkb[b], self.rotb], writes=[rot])
                    P.op("dve", lambda e, b=b: e.tensor_tensor(out=t1[b][:], in0=qkb[b][:],
                                                               in1=rp[b][:, 0:1, :].to_broadcast([64, 8, 128]), op=ALU.mult),
                         reads=[qkb[b], rp[b]], writes=[t1[b]])
                    P.op("dve", lambda e, b=b: e.tensor_tensor(out=t2[b][:], in0=rot[:],
                                                               in1=rp[b][:, 1:2, :].to_broadcast([64, 8, 128]), op=ALU.mult),
                         reads=[rot, rp[b]], writes=[t2[b]])
                    P.op("pool", lambda e, b=b: e.tensor_tensor(out=qkr[b][:], in0=t1[b][:], in1=t2[b][:], op=ALU.add),
                         reads=[t1[b], t2[b]], writes=[qkr[b]])
                    qsrc = qkr[b]
                else:
                    qsrc = qkb[b]
                rows = slice(t * 128, (t + 1) * 128)
                P.dma("pool", lambda e, qsrc=qsrc, t=t: e.dma_start(out=self.QKT[t], in_=qsrc[:]), reads=[qsrc])
                P.dma("pool", lambda e, b=b, rows=rows: e.dma_start(out=self.ZT[:, rows], in_=zt[b][:]), reads=[zt[b]])
                P.dma("pool", lambda e, b=b, rows=rows: e.dma_start(out=self.V[rows, :], in_=vt[b][:]), reads=[vt[b]])
                P.dma("pool", lambda e, b=b, rows=rows: e.dma_start(out=self.RG[rows, :], in_=rt[b][:]), reads=[rt[b]])
                P.dma("pool", lambda e, b=b, t=t: e.dma_start(out=self.NQT[t], in_=nqt[b][:]), reads=[nqt[b]])
                P.dma("pool", lambda e, b=b, t=t: e.dma_start(out=self.NKT[t], in_=nkt[b][:]), reads=[nkt[b]])
                P.dma("pool", lambda e, b=b, rows=rows: e.dma_start(out=self.NVX[rows, :], in_=nvx[b][:].rearrange("p h d -> p (h d)")),
                      reads=[nvx[b]])
                P.dma("pool", lambda e, b=b, rows=rows: e.dma_start(out=self.G[rows, :], in_=gt[b][:]), reads=[gt[b]])
            P.barrier()

    def phase_b(self, l):
        P, nc, c = self.P, self.nc, self.cfg
        NT = c.nt
        with contextlib.ExitStack() as st:
            wa = {}
            ba = {}
            for d, (wsrc, bsrc) in (("f", (self.waf, self.baf)), ("b", (self.wab, self.bab))):
                wa[d] = self.sb(st, "b_wa" + d, [16, 256])
                ba[d] = self.sb(st, "b_ba" + d, [1, 256])
                P.dma("sp", lambda e, d=d, wsrc=wsrc: e.dma_start(out=wa[d][:], in_=wsrc[l]), writes=[wa[d]])
                P.dma("sp", lambda e, d=d, bsrc=bsrc: e.dma_start(out=ba[d][:], in_=bsrc[l:l + 1, :]), writes=[ba[d]])
            S32 = {d: self.sb(st, "b_S32" + d, [64, 4, 128]) for d in "fb"}
            Sbf = {d: self.sb(st, "b_Sbf" + d, [64, 4, 128], BF16) for d in "fb"}
            for d in "fb":
                P.op("pool", lambda e, d=d: e.memset(S32[d][:], 0.0), writes=[S32[d]])
                P.op("pool", lambda e, d=d: e.memset(Sbf[d][:], 0.0), writes=[Sbf[d]])
            mk = lambda nm, shp, dt=F32: {d: [self.sb(st, "b_%s%s%d" % (nm, d, i), shp, dt) for i in range(2)] for d in "fb"}
            zt = mk("z", [16, 128])
            qk = mk("qk", [64, 8, 128], BF16)
            vv = mk("v", [64, 2, 512], BF16)
            e1 = mk("e1", [128, 256])
            Lt = mk("L", [128, 256])
            eb = mk("eb", [64, 4, 128])
            enb = mk("enb", [64, 4, 128])
            qe = mk("qe", [64, 4, 128], BF16)
            ke = mk("ke", [64, 4, 128], BF16)
            ktok = mk("ktok", [64, 4, 64], BF16)
            Am = mk("Am", [64, 4, 64], BF16)
            Ot = mk("Ot", [64, 512])
            lb_ps = {d: self.ps(st, "b_lb" + d, [128, 512]) for d in "fb"}
            kT_ps = self.ps(st, "b_kT", [64, 4, 64], BF16)
            A_ps = {d: self.ps(st, "b_A" + d, [64, 4, 64]) for d in "fb"}
            O_ps = {d: self.ps(st, "b_O" + d, [64, 512]) for d in "fb"}
            dS_ps = self.ps(st, "b_dS", [64, 4, 128])
            tri = {"f": self.triF, "b": self.triB}
            msk = {"f": self.mF, "b": self.mB}
            OD = {"f": self.OF, "b": self.OB}
            order_f = list(range(NT))
            order_b = [1, 0] + list(range(NT - 1, 1, -1))
            cnt = {"f": 0, "b": 0}

            def emit(d, t):
                i = cnt[d] % 2
                cnt[d] += 1
                z_, qk_, v_, e1_, L_, eb_, enb_, qe_, ke_ = zt[d][i], qk[d][i], vv[d][i], e1[d][i], Lt[d][i], eb[d][i], enb[d][i], qe[d][i], ke[d][i]
                rows = slice(t * 128, (t + 1) * 128)
                zr = slice(0, 16) if d == "f" else slice(16, 32)
                P.dma("sp", lambda e: e.dma_start(out=z_[:], in_=self.ZT[zr, rows]), writes=[z_])
                P.dma("sp", lambda e: e.dma_start(out=qk_[:], in_=self.QKT[t]), writes=[qk_])
                P.dma("sp", lambda e: e.dma_start(out=v_[:], in_=self.V[rows, :].rearrange("(c p) n -> p c n", p=64)), writes=[v_])
                lp = lb_ps[d]
                la = lp[:, 0:256]
                bp = lp[0:64, :].rearrange("p (h n) -> p h n", h=4)
                P.op("pe", lambda e: e.matmul(la, lhsT=z_[:], rhs=wa[d][:], start=True, stop=False), reads=[z_, wa[d]], writes=[lp])
                P.op("pe", lambda e: e.matmul(la, lhsT=self.ones_row[:], rhs=ba[d][:], start=False, stop=True),
                     reads=[self.ones_row, ba[d]], writes=[lp])
                P.op("act", lambda e: e.activation(out=e1_[:], in_=la, func=AF.Exp, scale=-1.0), reads=[lp], writes=[e1_])
                P.op("act", lambda e: e.activation(out=L_[:], in_=e1_[:], func=AF.Ln, bias=1.0), reads=[e1_], writes=[L_])
                for h in range(4):
                    P.op("pe", lambda e, h=h: e.matmul(bp[:, h, :], lhsT=L_[:, h * 64:(h + 1) * 64], rhs=tri[d][:], start=True, stop=True),
                         reads=[L_, tri[d]], writes=[lp])
                P.op("act", lambda e: e.activation(out=eb_[:], in_=bp, func=AF.Exp), reads=[lp], writes=[eb_])
                P.op("act", lambda e: e.activation(out=enb_[:], in_=bp, func=AF.Exp, scale=-1.0), reads=[lp], writes=[enb_])
                P.op("dve", lambda e: e.scalar_tensor_tensor(out=qe_[:], in0=qk_[:, 0:4, :], scalar=0.125, in1=eb_[:],
                                                             op0=ALU.mult, op1=ALU.mult), reads=[qk_, eb_], writes=[qe_])
                P.op("dve", lambda e: e.tensor_tensor(out=ke_[:], in0=qk_[:, 4:8, :], in1=enb_[:], op=ALU.mult),
                     reads=[qk_, enb_], writes=[ke_])
                def chunk(c_):
                    cs = slice(c_ * 64, (c_ + 1) * 64)
                    kt_, Am_, Ot_ = ktok[d][c_], Am[d][c_], Ot[d][c_]
                    for h in range(4):
                        P.op("pe", lambda e, h=h: e.transpose(out=kT_ps[:, h, :], in_=ke_[:, h, cs], identity=self.identb[0:64, 0:64]),
                             reads=[ke_, self.identb], writes=[kT_ps])
                    P.op("act", lambda e: e.copy(out=kt_[:], in_=kT_ps[:]), reads=[kT_ps], writes=[kt_])
                    for h in range(4):
                        P.op("pe", lambda e, h=h: e.matmul(A_ps[d][:, h, :], lhsT=ke_[:, h, cs], rhs=qe_[:, h, cs], start=True, stop=True),
                             reads=[ke_, qe_], writes=[A_ps[d]])
                    P.op("dve", lambda e: e.tensor_tensor(out=Am_[:], in0=A_ps[d][:],
                                                          in1=msk[d][:].unsqueeze(1).to_broadcast([64, 4, 64]), op=ALU.mult),
                         reads=[A_ps[d], msk[d]], writes=[Am_])
                    for h in range(4):
                        P.op("pe", lambda e, h=h: e.matmul(O_ps[d][:, h * 128:(h + 1) * 128], lhsT=Am_[:, h, :],
                                                          rhs=v_[:, c_, h * 128:(h + 1) * 128], start=True, stop=False),
                             reads=[Am_, v_], writes=[O_ps[d]])
                        P.op("pe", lambda e, h=h: e.matmul(O_ps[d][:, h * 128:(h + 1) * 128], lhsT=qe_[:, h, cs], rhs=Sbf[d][:, h, :],
                                                          start=False, stop=True), reads=[qe_, Sbf[d]], writes=[O_ps[d]])
                    for h in range(4):
                        P.op("pe", lambda e, h=h: e.matmul(dS_ps[:, h, :], lhsT=kt_[:, h, :], rhs=v_[:, c_, h * 128:(h + 1) * 128],
                                                          start=True, stop=True), reads=[kt_, v_], writes=[dS_ps])
                    P.op("act", lambda e: e.copy(out=Ot_[:], in_=O_ps[d][:]), reads=[O_ps[d]], writes=[Ot_])
                    r0 = t * 128 + c_ * 64
                    P.dma("pool", lambda e: e.dma_start(out=OD[d][r0:r0 + 64, :], in_=Ot_[:]), reads=[Ot_])
                    P.op("dve", lambda e: e.tensor_tensor(out=S32[d][:], in0=S32[d][:], in1=dS_ps[:], op=ALU.add),
                         reads=[S32[d], dS_ps], writes=[S32[d]])
                    tcol = c_ * 64 + 63 if d == "f" else c_ * 64
                    P.op("dve", lambda e, tcol=tcol: e.tensor_tensor(
                        out=S32[d][:], in0=S32[d][:], in1=eb_[:, :, tcol:tcol + 1].to_broadcast([64, 4, 128]), op=ALU.mult),
                        reads=[S32[d], eb_], writes=[S32[d]])
                    P.op("act", lambda e: e.copy(out=Sbf[d][:], in_=S32[d][:]), reads=[S32[d]], writes=[Sbf[d]])

                for c_ in ((0, 1) if d == "f" else (1, 0)):
                    chunk(c_)

            f32t = [self.sb(st, "b_cf%d" % i, [128, 8, DM]) for i in range(2)]
            b16t = [self.sb(st, "b_cb%d" % i, [128, 8, DM], BF16) for i in range(2)]
            conv = [(src, off, i) for (src, off) in ((self.eu, 0), (self.ev, DM)) for i in range(16)]

            def conv_chunk(n):
                src, off, i = conv[n]
                a, bb = f32t[n % 2], b16t[n % 2]
                rs = slice(i * 1024, (i + 1) * 1024)
                P.dma("sp", lambda e: e.dma_start(out=a[:], in_=src[l][rs, :].rearrange("(p r) d -> p r d", r=8)), writes=[a])
                P.op("pool", lambda e: e.tensor_copy(out=bb[:], in_=a[:]), reads=[a], writes=[bb])
                P.dma("pool", lambda e: e.dma_start(out=self.UVB[rs, off:off + DM].rearrange("(p r) d -> p r d", r=8), in_=bb[:]), reads=[bb])

            nconv = 0
            for s_ in range(NT):
                emit("f", order_f[s_])
                emit("b", order_b[s_])
                if nconv < len(conv) and (s_ % 2 == 0 or NT - s_ <= len(conv) - nconv):
                    conv_chunk(nconv)
                    nconv += 1
            while nconv < len(conv):
                conv_chunk(nconv)
                nconv += 1
            P.barrier()

    def phase_c(self, l, with_ctx):
        P, nc, c = self.P, self.nc, self.cfg
        with contextlib.ExitStack() as st:
            st.enter_context(nc.allow_non_contiguous_dma(reason="tile-blocked K layout"))
            kTc = self.sb(st, "c_kTc", [128, 2, 4, 128], BF16)
            vxc = self.sb(st, "c_vxc", [128, 2, 520], BF16)
            biasM = self.sb(st, "c_biasM", [128, 8, 5, 128])
            biasE = self.sb(st, "c_biasE", [128, 8, 5, 128])
            P.dma("sp", lambda e: e.dma_start(out=kTc[:], in_=self.NKT[0:2].rearrange("t p b k -> p t b k")), writes=[kTc])
            P.dma("sp", lambda e: e.dma_start(out=vxc[:], in_=self.NVX[0:256, :].rearrange("(t p) d -> p t d", p=128)), writes=[vxc])
            P.dma("sp", lambda e: e.dma_start(out=biasM[:], in_=self.biasT[l, self.pat_main]), writes=[biasM])
            qT = [self.sb(st, "c_qT%d" % i, [128, 4, 128], BF16) for i in range(2)]
            kT = [self.sb(st, "c_kT%d" % i, [128, 5, 4, 128], BF16) for i in range(2)]
            vx = [self.sb(st, "c_vx%d" % i, [128, 5, 520], BF16) for i in range(2)]
            Tt = [self.sb(st, "c_T%d" % i, [128, 5, 128]) for i in range(2)]
            PT = [self.sb(st, "c_PT%d" % i, [128, 7, 128], BF16) for i in range(2)]
            rec = [self.sb(st, "c_rec%d" % i, [128, 8]) for i in range(2)]
            ot = [self.sb(st, "c_o%d" % i, [128, 8, 64], BF16) for i in range(2)]
            S_ps = [self.ps(st, "c_S%d" % i, [128, 8, 128]) for i in range(2)]
            O_ps = [[self.ps(st, "c_O%d%d" % (i, j), [128, 4, 65]) for j in range(2)] for i in range(2)]
            tiles = ([0, 1] if with_ctx else []) + list(range(2, c.nt))
            cur_edge = None
            nh = 0
            for n_, t in enumerate(tiles):
                b = n_ % 2
                win = t >= 2
                P.dma("sp", lambda e, b=b, t=t: e.dma_start(out=qT[b][:], in_=self.NQT[t]), writes=[qT[b]])
                if win:
                    m = t - 2
                    s0 = 2 + self.pat_starts[m]
                    P.dma("sp", lambda e, b=b, s0=s0: e.dma_start(out=kT[b][:], in_=self.NKT[s0:s0 + 5].rearrange("t p b k -> p t b k")),
                          writes=[kT[b]])
                    P.dma("sp", lambda e, b=b, s0=s0: e.dma_start(
                        out=vx[b][:], in_=self.NVX[s0 * 128:(s0 + 5) * 128, :].rearrange("(t p) d -> p t d", p=128)), writes=[vx[b]])
                    pid = self.pat_ids[m]
                    if pid == self.pat_main:
                        bias = biasM
                    else:
                        if cur_edge != pid:
                            P.dma("sp", lambda e, pid=pid: e.dma_start(out=biasE[:], in_=self.biasT[l, pid]), writes=[biasE])
                            cur_edge = pid
                        bias = biasE
                for h in range(8):
                    blk, po = h // 2, (h % 2) * 64
                    sp_ = S_ps[nh % 2]
                    T_, PT_ = Tt[nh % 2], PT[nh % 2]
                    nh += 1
                    if win:
                        for kb in range(5):
                            P.op("pe", lambda e, kb=kb, b=b, blk=blk, po=po, sp_=sp_: e.matmul(
                                sp_[:, kb, :], lhsT=kT[b][po:po + 64, kb, blk, :], rhs=qT[b][po:po + 64, blk, :], start=True, stop=True),
                                reads=[kT[b], qT[b]], writes=[sp_])
                    for cb in range(2):
                        P.op("pe", lambda e, cb=cb, b=b, blk=blk, po=po, sp_=sp_: e.matmul(
                            sp_[:, 5 + cb, :], lhsT=kTc[po:po + 64, cb, blk, :], rhs=qT[b][po:po + 64, blk, :], start=True, stop=True),
                            reads=[kTc, qT[b]], writes=[sp_])
                    if win:
                        P.op("dve", lambda e, sp_=sp_, T_=T_, h=h, bias=bias: e.scalar_tensor_tensor(
                            out=T_[:], in0=sp_[:, 0:5, :], scalar=0.125, in1=bias[:, h, :, :], op0=ALU.mult, op1=ALU.add),
                            reads=[sp_, bias], writes=[T_])
                        P.op("act", lambda e, T_=T_, PT_=PT_: e.activation(out=PT_[:, 0:5, :], in_=T_[:], func=AF.Exp),
                             reads=[T_], writes=[PT_])
                    P.op("act", lambda e, sp_=sp_, PT_=PT_: e.activation(out=PT_[:, 5:7, :], in_=sp_[:, 5:7, :], func=AF.Exp, scale=0.125),
                         reads=[sp_], writes=[PT_])
                    op_ = O_ps[b][h // 4]
                    hl = h % 4
                    if win:
                        for kb in range(5):
                            P.op("pe", lambda e, kb=kb, b=b, h=h, op_=op_, hl=hl, PT_=PT_: e.matmul(
                                op_[:, hl, :], lhsT=PT_[:, kb, :], rhs=vx[b][:, kb, h * 65:(h + 1) * 65], start=(kb == 0), stop=False),
                                reads=[PT_, vx[b]], writes=[op_])
                    for cb in range(2):
                        P.op("pe", lambda e, cb=cb, h=h, op_=op_, hl=hl, PT_=PT_, win=win: e.matmul(
                            op_[:, hl, :], lhsT=PT_[:, 5 + cb, :], rhs=vxc[:, cb, h * 65:(h + 1) * 65],
                            start=(cb == 0 and not win), stop=(cb == 1)), reads=[PT_, vxc], writes=[op_])
                for j in range(2):
                    op_ = O_ps[b][j]
                    P.op("dve", lambda e, b=b, j=j, op_=op_: e.reciprocal(out=rec[b][:, j * 4:(j + 1) * 4], in_=op_[:, :, 64]),
                         reads=[op_], writes=[rec[b]])
                    P.op("dve", lambda e, b=b, j=j, op_=op_: e.tensor_tensor(
                        out=ot[b][:, j * 4:(j + 1) * 4, :], in0=op_[:, :, 0:64],
                        in1=rec[b][:, j * 4:(j + 1) * 4].unsqueeze(2).to_broadcast([128, 4, 64]), op=ALU.mult),
                        reads=[op_, rec[b]], writes=[ot[b]])
                P.dma("pool", lambda e, b=b, t=t: e.dma_start(out=self.NAO[t * 128:(t + 1) * 128, :],
                                                            in_=ot[b][:].rearrange("p h d -> p (h d)")), reads=[ot[b]])
            P.barrier()

    def phase_d(self, l, with_ctx):
        P, nc, c = self.P, self.nc, self.cfg
        with contextlib.ExitStack() as st:
            wpg = self.sb(st, "d_wpg", [128, 4, DM], BF16)
            wpn = self.sb(st, "d_wpn", [128, 4, DM], BF16)
            wo = self.sb(st, "d_wo", [128, 8, DM], BF16)
            stg = [self.sb(st, "d_stg%d" % i, [128, DM]) for i in range(2)]
            n = 0
            for (dst, src, nk) in ((wpg, self.w_pg, 4), (wpn, self.w_pn, 4), (wo, self.w_o, 8)):
                for k in range(nk):
                    s = stg[n % 2]
                    P.dma("sp", lambda e, s=s, k=k, src=src: e.dma_start(out=s[:], in_=src[l][k * 128:(k + 1) * 128, :]), writes=[s])
                    if n % 2 == 0:
                        P.op("act", lambda e, s=s, k=k, dst=dst: e.copy(out=dst[:, k, :], in_=s[:]), reads=[s], writes=[dst])
                    else:
                        P.op("pool", lambda e, s=s, k=k, dst=dst: e.tensor_copy(out=dst[:, k, :], in_=s[:]), reads=[s], writes=[dst])
                    n += 1
            gg = self.sb(st, "d_gg", [128, 128])
            ga1 = [self.sb(st, "d_ga1_%d" % r, [128, DM]) for r in range(2)]
            P.dma("sp", lambda e: e.dma_start(out=gg[:], in_=self.ggla[l:l + 1, :].partition_broadcast(128)), writes=[gg])
            for r in range(2):
                P.dma("sp", lambda e, r=r: e.dma_start(out=ga1[r][:], in_=self.AUX[r, 2:3, :].partition_broadcast(128)), writes=[ga1[r]])
            of = [self.sb(st, "d_of%d" % i, [128, 4, 128]) for i in range(2)]
            ob = [self.sb(st, "d_ob%d" % i, [128, 4, 128]) for i in range(2)]
            rg = [self.sb(st, "d_rg%d" % i, [128, 512], BF16) for i in range(2)]
            nao = [self.sb(st, "d_nao%d" % i, [128, 512], BF16) for i in range(2)]
            gt = [self.sb(st, "d_g%d" % i, [128, 2048], BF16) for i in range(2)]
            xt = [self.sb(st, "d_x%d" % i, [128, DM]) for i in range(2)]
            junk = self.sb(st, "d_junk", [128, 128], BF16)
            ss = [self.sb(st, "d_ss%d" % i, [128, 4, 4]) for i in range(2)]
            onb2 = [self.sb(st, "d_onb%d" % i, [128, 512], BF16) for i in range(2)]
            onT2 = [self.sb(st, "d_onT%d" % i, [128, 4, 128], BF16) for i in range(2)]
            naT2 = [self.sb(st, "d_naT%d" % i, [128, 4, 128], BF16) for i in range(2)]
            m1_2 = [self.sb(st, "d_m1%d" % i, [128, DM]) for i in range(2)]
            m2_2 = [self.sb(st, "d_m2%d" % i, [128, DM]) for i in range(2)]
            mb2 = [self.sb(st, "d_mb%d" % i, [128, DM], BF16) for i in range(2)]
            mT2 = [self.sb(st, "d_mT%d" % i, [128, 8, 128], BF16) for i in range(2)]
            tpA = self.ps(st, "d_tpA", [128, 8, 128], BF16)
            tpB = self.ps(st, "d_tpB", [128, 8, 128], BF16)
            ya = self.ps(st, "d_ya", [128, DM])
            yb = self.ps(st, "d_yb", [128, DM])
            yy = self.ps(st, "d_yy", [128, DM])
            tiles = ([0, 1] if with_ctx else []) + list(range(2, c.nt))

            def d_tile(n_, t):
                b = n_ % 2
                r = 1 if t < 2 else 0
                rows = slice(t * 128, (t + 1) * 128)
                of_, ob_, rg_, nao_, g_, x_, ss_ = of[b], ob[b], rg[b], nao[b], gt[b], xt[b], ss[b]
                onb, onT, naT, m1, m2, mb, mT = onb2[b], onT2[b], naT2[b], m1_2[b], m2_2[b], mb2[b], mT2[b]
                P.dma("sp", lambda e, of_=of_, rows=rows: e.dma_start(out=of_[:].rearrange("p h d -> p (h d)"), in_=self.OF[rows, :]), writes=[of_])
                P.dma("sp", lambda e, ob_=ob_, rows=rows: e.dma_start(out=ob_[:].rearrange("p h d -> p (h d)"), in_=self.OB[rows, :]), writes=[ob_])
                P.dma("sp", lambda e, rg_=rg_, rows=rows: e.dma_start(out=rg_[:], in_=self.RG[rows, :]), writes=[rg_])
                P.dma("sp", lambda e, nao_=nao_, rows=rows: e.dma_start(out=nao_[:], in_=self.NAO[rows, :]), writes=[nao_])
                P.dma("sp", lambda e, g_=g_, rows=rows: e.dma_start(out=g_[:], in_=self.G[rows, :]), writes=[g_])
                P.dma("sp", lambda e, x_=x_, rows=rows, l=l: e.dma_start(out=x_[:], in_=(self.xin if l == 0 else self.X)[rows, :]), writes=[x_])
                P.op("dve", lambda e, of_=of_, ob_=ob_: e.tensor_tensor(out=of_[:], in0=of_[:], in1=ob_[:], op=ALU.add),
                     reads=[of_, ob_], writes=[of_])
                for h in range(4):
                    P.op("act", lambda e, of_=of_, ss_=ss_, h=h: e.activation(out=junk[:], in_=of_[:, h, :], func=AF.Square,
                                                                             accum_out=ss_[:, 0, h:h + 1]), reads=[of_], writes=[junk, ss_])
                P.op("dve", lambda e, ss_=ss_: e.tensor_scalar(out=ss_[:, 1, :], in0=ss_[:, 0, :], scalar1=1.0 / 128, scalar2=EPS,
                                                               op0=ALU.mult, op1=ALU.add), reads=[ss_], writes=[ss_])
                P.op("act", lambda e, ss_=ss_: e.activation(out=ss_[:, 2, :], in_=ss_[:, 1, :], func=AF.Sqrt), reads=[ss_], writes=[ss_])
                P.op("dve", lambda e, ss_=ss_: e.reciprocal(out=ss_[:, 3, :], in_=ss_[:, 2, :]), reads=[ss_], writes=[ss_])
                P.op("dve", lambda e, of_=of_, ss_=ss_: e.tensor_tensor(
                    out=of_[:], in0=of_[:], in1=ss_[:, 3, :].unsqueeze(2).to_broadcast([128, 4, 128]), op=ALU.mult),
                    reads=[of_, ss_], writes=[of_])
                P.op("pool", lambda e, of_=of_: e.tensor_tensor(
                    out=of_[:], in0=of_[:], in1=gg[:].unsqueeze(1).to_broadcast([128, 4, 128]), op=ALU.mult),
                    reads=[of_, gg], writes=[of_])
                P.op("dve", lambda e, of_=of_, rg_=rg_: e.tensor_tensor(out=onb[:], in0=of_[:].rearrange("p h d -> p (h d)"),
                                                                      in1=rg_[:], op=ALU.mult), reads=[of_, rg_], writes=[onb])
                for k in range(4):
                    P.op("pe", lambda e, k=k: e.transpose(out=tpA[:, k, :], in_=onb[:, k * 128:(k + 1) * 128], identity=self.identb[:]),
                         reads=[onb, self.identb], writes=[tpA])
                for k in range(4):
                    P.op("pe", lambda e, k=k, nao_=nao_: e.transpose(out=tpA[:, 4 + k, :], in_=nao_[:, k * 128:(k + 1) * 128],
                                                                    identity=self.identb[:]), reads=[nao_, self.identb], writes=[tpA])
                P.op("act", lambda e: e.copy(out=onT[:], in_=tpA[:, 0:4, :]), reads=[tpA], writes=[onT])
                P.op("act", lambda e: e.copy(out=naT[:], in_=tpA[:, 4:8, :]), reads=[tpA], writes=[naT])
                for (yp, xT, w) in ((ya, onT, wpg), (yb, naT, wpn)):
                    for cc in range(2):
                        for k in range(4):
                            P.op("pe", lambda e, yp=yp, xT=xT, w=w, cc=cc, k=k: e.matmul(
                                yp[:, cc * 512:(cc + 1) * 512], lhsT=xT[:, k, :], rhs=w[:, k, cc * 512:(cc + 1) * 512],
                                start=(k == 0), stop=(k == 3)), reads=[xT, w], writes=[yp])
                P.op("dve", lambda e, g_=g_: e.tensor_tensor(out=m1[:], in0=ya[:], in1=g_[:, 0:DM], op=ALU.mult),
                     reads=[ya, g_], writes=[m1])
                P.op("dve", lambda e, g_=g_: e.tensor_tensor(out=m2[:], in0=yb[:], in1=g_[:, DM:2 * DM], op=ALU.mult),
                     reads=[yb, g_], writes=[m2])
                P.op("pool", lambda e: e.tensor_tensor(out=mb[:], in0=m1[:], in1=m2[:], op=ALU.add), reads=[m1, m2], writes=[mb])
                for k in range(8):
                    P.op("pe", lambda e, k=k: e.transpose(out=tpB[:, k, :], in_=mb[:, k * 128:(k + 1) * 128], identity=self.identb[:]),
                         reads=[mb, self.identb], writes=[tpB])
                P.op("act", lambda e: e.copy(out=mT[:], in_=tpB[:]), reads=[tpB], writes=[mT])
                for cc in range(2):
                    for k in range(8):
                        P.op("pe", lambda e, cc=cc, k=k: e.matmul(yy[:, cc * 512:(cc + 1) * 512], lhsT=mT[:, k, :],
                                                                 rhs=wo[:, k, cc * 512:(cc + 1) * 512], start=(k == 0), stop=(k == 7)),
                             reads=[mT, wo], writes=[yy])
                P.op("dve", lambda e, r=r: e.tensor_tensor(out=m1[:], in0=yy[:], in1=ga1[r][:], op=ALU.mult),
                     reads=[yy, ga1[r]], writes=[m1])
                P.op("pool", lambda e, x_=x_: e.tensor_tensor(out=x_[:], in0=x_[:], in1=m1[:], op=ALU.add), reads=[x_, m1], writes=[x_])
                P.dma("pool", lambda e, x_=x_, rows=rows: e.dma_start(out=self.X[rows, :], in_=x_[:]), reads=[x_])

            for n_, t in enumerate(tiles):
                d_tile(n_, t)
            P.barrier()

    def phase_e(self, l, with_ctx, final):
        P, nc, c = self.P, self.nc, self.cfg
        NBUF = 18
        BS = 8
        NBATCH = 128 // BS
        with contextlib.ExitStack() as st:
            wq = self.sb(st, "e_wq", [128, 8, 2048], BF16)
            skb = self.sb(st, "e_skb", [128, 16, 128], BF16)
            with contextlib.ExitStack() as st2:
                stg = [self.sb(st2, "e_stg%d" % i, [128, 2048]) for i in range(2)]
                for k in range(8):
                    s = stg[k % 2]
                    P.dma("sp", lambda e, s=s, k=k: e.dma_start(out=s[:], in_=self.w_q[l][k * 128:(k + 1) * 128, :]), writes=[s])
                    if k % 2 == 0:
                        P.op("act", lambda e, s=s, k=k: e.copy(out=wq[:, k, :], in_=s[:]), reads=[s], writes=[wq])
                    else:
                        P.op("pool", lambda e, s=s, k=k: e.tensor_copy(out=wq[:, k, :], in_=s[:]), reads=[s], writes=[wq])
                s = stg[0]
                P.dma("sp", lambda e, s=s: e.dma_start(out=s[:].rearrange("p (g n) -> p g n", g=16), in_=self.skT[l].rearrange("g d n -> d g n")),
                      writes=[s])
                P.op("act", lambda e, s=s: e.copy(out=skb[:], in_=s[:].rearrange("p (g n) -> p g n", g=16)), reads=[s], writes=[skb])
                P.barrier()
            A2 = self.sb(st, "e_A2", [128, DM])
            B2 = self.sb(st, "e_B2", [128, DM])
            GA2 = [self.sb(st, "e_GA2_%d" % r, [128, DM]) for r in range(2)]
            for r in range(2):
                P.dma("sp", lambda e, r=r: e.dma_start(out=GA2[r][:], in_=self.AUX[r, 3:4, :].partition_broadcast(128)), writes=[GA2[r]])
            gfin = None
            if final:
                gfin = self.sb(st, "e_gfin", [128, DM])
                P.dma("sp", lambda e: e.dma_start(out=gfin[:], in_=self.gfin.rearrange("(o d) -> o d", o=1).partition_broadcast(128)),
                      writes=[gfin])
            xt = [self.sb(st, "e_x%d" % i, [128, DM]) for i in range(2)]
            ss = [self.sb(st, "e_ss%d" % i, [128, 8]) for i in range(2)]
            h2p = [self.sb(st, "e_h2_%d" % i, [128, DM]) for i in range(2)]
            m32 = self.sb(st, "e_m32", [128, DM])
            junkb = self.sb(st, "e_junkb", [128, DM], BF16)
            junkd = self.sb(st, "e_junkd", [128, DM], BF16)
            h2T = self.sb(st, "e_h2T", [128, 8, 128], BF16)
            qpT = self.sb(st, "e_qpT", [128, 16, 128], BF16)
            sc = self.sb(st, "e_sc", [128, 16, 128])
            s8 = self.sb(st, "e_s8", [128, 16, 16])
            i8 = self.sb(st, "e_i8", [128, 16, 16], U32)
            i8f = self.sb(st, "e_i8f", [128, 16, 16])
            i0s = self.sb(st, "e_i0s", [128, 8, 16])
            cand = self.sb(st, "e_cand", [128, 8, 16, 16])
            cidx = self.sb(st, "e_cidx", [128, 8, 16, 16])
            j256 = self.sb(st, "e_j256", [128, 256])
            best = self.sb(st, "e_best", [128, 8, 16])
            eidf = self.sb(st, "e_eidf", [128, 128])
            eidx2 = [self.sb(st, "e_eidx%d" % i, [128, 128], I32) for i in range(2)]
            ex = self.sb(st, "e_ex", [128, 8, 16])
            sm = self.sb(st, "e_sm", [128, 8, 2])
            ggp = [self.sb(st, "e_g%d" % i, [128, 128]) for i in range(2)]
            actb = [self.sb(st, "e_act%d" % i, [128, BS]) for i in range(2)]
            actc = [[T(None, "actc") for _ in range(BS)] for _ in range(2)]
            dkc = [[T(None, "dkc") for _ in range(BS)] for _ in range(2)]
            scg = [T(None, "scg") for _ in range(16)]
            candh = [T(None, "candh") for _ in range(8)]
            besth = [T(None, "besth") for _ in range(8)]
            eidc = [T(None, "eidc") for _ in range(128)]
            cidxr = T(None, "cidxr")
            i8fr = T(None, "i8fr")
            ta = [self.sb(st, "e_ta%d" % i, [128, BS]) for i in range(2)]
            tb = [self.sb(st, "e_tb%d" % i, [128, BS]) for i in range(2)]
            wg = [self.sb(st, "e_wg%d" % i, [128, BS]) for i in range(2)]
            Dk = [self.sb(st, "e_Dk%d" % i, [128, BS, 128], BF16) for i in range(2)]
            ring = [self.sb(st, "e_ring%d" % i, [128, 2 * DM], BF16) for i in range(NBUF)]
            acc_ps = self.ps(st, "e_acc", [128, DM])
            tp_ps = self.ps(st, "e_tp", [128, 8, 128])
            qp_ps = [self.ps(st, "e_qp%d" % i, [128, 4, 128]) for i in range(2)]
            sc_ps = self.ps(st, "e_scps", [128, 8, 128])
            tiles = ([0, 1] if with_ctx else []) + list(range(2, c.nt))
            state = {"cur_r": None, "nring": 0}

            def S1(n_):
                t = tiles[n_]
                b = n_ % 2
                r = 1 if t < 2 else 0
                rows = slice(t * 128, (t + 1) * 128)
                eidx = eidx2[b]
                gg = ggp[b]
                h2 = h2p[b]
                if state["cur_r"] != r:
                    for i_, dst in ((0, A2), (1, B2)):
                        P.dma("sp", lambda e, i_=i_, dst=dst, r=r: e.dma_start(out=dst[:], in_=self.AUX[r, i_:i_ + 1, :].partition_broadcast(128)),
                              writes=[dst])
                    state["cur_r"] = r
                x_, ss_ = xt[b], ss[b]
                P.dma("sp", lambda e: e.dma_start(out=x_[:], in_=self.X[rows, :]), writes=[x_])
                P.op("act", lambda e: e.activation(out=junkb[:], in_=x_[:], func=AF.Square, accum_out=ss_[:, 0:1]),
                     reads=[x_], writes=[junkb, ss_])
                P.op("dve", lambda e: e.tensor_scalar(out=ss_[:, 1:2], in0=ss_[:, 0:1], scalar1=1.0 / DM, scalar2=EPS,
                                                      op0=ALU.mult, op1=ALU.add), reads=[ss_], writes=[ss_])
                P.op("act", lambda e: e.activation(out=ss_[:, 2:3], in_=ss_[:, 1:2], func=AF.Sqrt), reads=[ss_], writes=[ss_])
                P.op("dve", lambda e: e.reciprocal(out=ss_[:, 3:4], in_=ss_[:, 2:3]), reads=[ss_], writes=[ss_])
                yield
                P.op("dve", lambda e: e.scalar_tensor_tensor(out=h2[:], in0=x_[:], scalar=ss_[:, 3:4], in1=A2[:],
                                                             op0=ALU.mult, op1=ALU.mult), reads=[x_, ss_, A2], writes=[h2])
                yield
                P.op("dve", lambda e: e.tensor_tensor(out=h2[:], in0=h2[:], in1=B2[:], op=ALU.add), reads=[h2, B2], writes=[h2])
                for k in range(8):
                    P.op("pe", lambda e, k=k: e.transpose(out=tp_ps[:, k, :], in_=h2[:, k * 128:(k + 1) * 128], identity=self.identf[:]),
                         reads=[h2, self.identf], writes=[tp_ps])
                P.op("act", lambda e: e.copy(out=h2T[:, 0:4, :], in_=tp_ps[:, 0:4, :]), reads=[tp_ps], writes=[h2T])
                P.op("act", lambda e: e.copy(out=h2T[:, 4:8, :], in_=tp_ps[:, 4:8, :]), reads=[tp_ps], writes=[h2T])
                yield
                for q4 in range(4):
                    pq = qp_ps[q4 % 2]
                    for bi in range(4):
                        blk = q4 * 4 + bi
                        for k in range(8):
                            P.op("pe", lambda e, pq=pq, bi=bi, blk=blk, k=k: e.matmul(
                                pq[:, bi, :], lhsT=wq[:, k, blk * 128:(blk + 1) * 128], rhs=h2T[:, k, :], start=(k == 0), stop=(k == 7)),
                                reads=[wq, h2T], writes=[pq])
                    P.op("act", lambda e, pq=pq, q4=q4: e.copy(out=qpT[:, q4 * 4:(q4 + 1) * 4, :], in_=pq[:]), reads=[pq], writes=[qpT])
                yield
                for half in range(2):
                    for g8 in range(8):
                        g = half * 8 + g8
                        P.op("pe", lambda e, g=g, g8=g8: e.matmul(sc_ps[:, g8, :], lhsT=qpT[:, g, :], rhs=skb[:, g, :], start=True, stop=True),
                             reads=[qpT, skb], writes=[sc_ps])
                    for q in range(2):
                        g0 = half * 8 + q * 4
                        P.op("act", lambda e, g0=g0, q=q: e.copy(out=sc[:, g0:g0 + 4, :], in_=sc_ps[:, q * 4:(q + 1) * 4, :]),
                             reads=[sc_ps], writes=scg[g0:g0 + 4])
                yield
                for g2 in range(0, 16, 2):
                    gs = (g2, g2 + 1)
                    for g in gs:
                        P.op("dve", lambda e, g=g: e.max(out=s8[:, g, 0:8], in_=sc[:, g, :]), reads=[scg[g]], writes=[scg[g]])
                    for g in gs:
                        P.op("dve", lambda e, g=g: e.max_index(out=i8[:, g, 0:8], in_max=s8[:, g, 0:8], in_values=sc[:, g, :]),
                             reads=[scg[g]], writes=[scg[g]])
                    for g in gs:
                        P.op("dve", lambda e, g=g: e.match_replace(out=sc[:, g, :], in_to_replace=s8[:, g, 0:8], in_values=sc[:, g, :],
                                                                   imm_value=-1e30), reads=[scg[g]], writes=[scg[g]])
                    for g in gs:
                        P.op("dve", lambda e, g=g: e.max(out=s8[:, g, 8:16], in_=sc[:, g, :]), reads=[scg[g]], writes=[scg[g]])
                    for g in gs:
                        P.op("dve", lambda e, g=g: e.max_index(out=i8[:, g, 8:16], in_max=s8[:, g, 8:16], in_values=sc[:, g, :]),
                             reads=[scg[g]], writes=[scg[g]])
                    yield
                P.op("dve", lambda e: e.tensor_copy(out=i8f[:], in_=i8[:]), reads=scg, writes=[i8fr])
                s8v = s8[:].rearrange("p (h two) k -> p h two k", two=2)
                i8v = i8f[:].rearrange("p (h two) k -> p h two k", two=2)
                P.op("dve", lambda e: e.tensor_tensor(out=cand[:], in0=s8v[:, :, 0, :].unsqueeze(3).to_broadcast([128, 8, 16, 16]),
                                                      in1=s8v[:, :, 1, :].unsqueeze(2).to_broadcast([128, 8, 16, 16]), op=ALU.add),
                     reads=scg, writes=candh)
                yield
                P.op("dve", lambda e: e.tensor_scalar(out=i0s[:], in0=i8v[:, :, 0, :], scalar1=128.0, scalar2=None, op0=ALU.mult),
                     reads=[i8fr], writes=[i0s])
                P.op("dve", lambda e: e.tensor_tensor(out=cidx[:], in0=i0s[:].unsqueeze(3).to_broadcast([128, 8, 16, 16]),
                                                      in1=i8v[:, :, 1, :].unsqueeze(2).to_broadcast([128, 8, 16, 16]), op=ALU.add),
                     reads=[i0s, i8fr], writes=[cidxr])
                yield
                cand3 = cand[:].rearrange("p h a b -> p h (a b)")
                cidx3 = cidx[:].rearrange("p h a b -> p h (a b)")

                def decode(h, k0):
                    for k in range(k0, k0 + 8):
                        P.op("dve", lambda e, h=h, k=k: e.scalar_tensor_tensor(
                            out=j256[:], in0=cand3[:, h, :], scalar=best[:, h, k:k + 1], in1=cidx3[:, h, :], op0=ALU.is_equal, op1=ALU.mult,
                            accum_out=eidf[:, h * 16 + k:h * 16 + k + 1]), reads=[candh[h], besth[h], cidxr], writes=[eidc[h * 16 + k]])

                for h2_ in range(0, 8, 2):
                    hs = (h2_, h2_ + 1)
                    for h in hs:
                        P.op("dve", lambda e, h=h: e.max(out=best[:, h, 0:8], in_=cand3[:, h, :]), reads=[candh[h]], writes=[besth[h]])
                    for h in hs:
                        decode(h, 0)
                    yield
                    for h in hs:
                        P.op("dve", lambda e, h=h: e.match_replace(out=cand3[:, h, :], in_to_replace=best[:, h, 0:8], in_values=cand3[:, h, :],
                                                                   imm_value=-1e30), reads=[candh[h], besth[h]], writes=[candh[h]])
                    for h in hs:
                        P.op("dve", lambda e, h=h: e.max(out=best[:, h, 8:16], in_=cand3[:, h, :]), reads=[candh[h]], writes=[besth[h]])
                    for h in hs:
                        decode(h, 8)
                    yield
                P.op("dve", lambda e: e.tensor_scalar(out=eidf[:], in0=eidf[:], scalar1=16383.0, scalar2=0.0, op0=ALU.min, op1=ALU.max),
                     reads=eidc, writes=[eidf] + eidc)
                P.op("dve", lambda e: e.tensor_copy(out=eidx[:], in_=eidf[:]), reads=[eidf], writes=[eidx])
                P.op("dve", lambda e: e.tensor_tensor(out=ex[:], in0=best[:], in1=best[:, :, 0:1].to_broadcast([128, 8, 16]), op=ALU.subtract),
                     reads=besth, writes=[ex])
                P.op("act", lambda e: e.activation(out=ex[:], in_=ex[:], func=AF.Exp), reads=[ex], writes=[ex])
                yield
                P.op("dve", lambda e: e.tensor_reduce(out=sm[:, :, 0], in_=ex[:], axis=AX.X, op=ALU.add), reads=[ex], writes=[sm])
                P.op("dve", lambda e: e.reciprocal(out=sm[:, :, 1], in_=sm[:, :, 0]), reads=[sm], writes=[sm])
                P.op("dve", lambda e: e.tensor_tensor(out=gg[:].rearrange("p (h k) -> p h k", h=8), in0=ex[:],
                                                      in1=sm[:, :, 1:2].to_broadcast([128, 8, 16]), op=ALU.mult), reads=[ex, sm], writes=[gg])

            def drain(gen, nmax=None):
                if gen is None:
                    return None
                k = 0
                while nmax is None or k < nmax:
                    try:
                        next(gen)
                    except StopIteration:
                        return None
                    k += 1
                return gen

            def batch_front(n_, k):
                eidx = eidx2[n_ % 2]
                h2 = h2p[n_ % 2]
                kp = k % 2
                bufs = []
                for jj in range(BS):
                    j = k * BS + jj
                    rb = ring[state["nring"] % NBUF]
                    state["nring"] += 1
                    bufs.append(rb)
                    P.dma("pool", lambda e, rb=rb, j=j: e.indirect_dma_start(
                        out=rb[:], out_offset=None, in_=self.UVB[:, :], in_offset=bass.IndirectOffsetOnAxis(ap=eidx[:, j:j + 1], axis=0)),
                        reads=[eidx], writes=[rb])
                for jj in range(BS):
                    rb = bufs[jj]
                    P.op("dve", lambda e, rb=rb, jj=jj: e.scalar_tensor_tensor(
                        out=junkd[:], in0=rb[:, 0:DM], scalar=1.0, in1=h2[:], op0=ALU.mult, op1=ALU.mult, accum_out=actb[kp][:, jj:jj + 1]),
                        reads=[rb, h2], writes=[actc[kp][jj]])
                a_, ta_, tb_ = actb[kp], ta[kp], tb[kp]
                P.op("dve", lambda e: e.tensor_tensor(out=ta_[:], in0=a_[:], in1=a_[:], op=ALU.mult), reads=actc[kp], writes=[ta_])
                P.op("dve", lambda e: e.tensor_scalar(out=ta_[:], in0=ta_[:], scalar1=0.044715, scalar2=1.0, op0=ALU.mult, op1=ALU.add),
                     reads=[ta_], writes=[ta_])
                P.op("dve", lambda e: e.tensor_tensor(out=ta_[:], in0=ta_[:], in1=a_[:], op=ALU.mult), reads=[ta_] + actc[kp], writes=[ta_])
                P.op("act", lambda e: e.activation(out=tb_[:], in_=ta_[:], func=AF.Sigmoid, scale=1.5957691216057308), reads=[ta_], writes=[tb_])
                return bufs

            def batch_back(n_, k, bufs):
                gg = ggp[n_ % 2]
                kp = k % 2
                a_, tb_, wg_, Dk_ = actb[kp], tb[kp], wg[kp], Dk[kp]
                P.op("dve", lambda e: e.tensor_tensor(out=tb_[:], in0=tb_[:], in1=a_[:], op=ALU.mult), reads=[tb_] + actc[kp], writes=[tb_])
                P.op("dve", lambda e: e.tensor_tensor(out=wg_[:], in0=tb_[:], in1=gg[:, k * BS:(k + 1) * BS], op=ALU.mult),
                     reads=[tb_, gg], writes=[wg_] + actc[kp])
                for jj in range(BS):
                    P.op("act", lambda e, jj=jj: e.activation(out=Dk_[:, jj, :], in_=self.identb[:], func=AF.Copy, scale=wg_[:, jj:jj + 1]),
                         reads=[self.identb, wg_], writes=[dkc[kp][jj]])
                for jj in range(BS):
                    j = k * BS + jj
                    rb = bufs[jj]
                    for hf in range(2):
                        P.op("pe", lambda e, rb=rb, jj=jj, j=j, hf=hf: e.matmul(
                            acc_ps[:, hf * 512:(hf + 1) * 512], lhsT=Dk_[:, jj, :], rhs=rb[:, DM + hf * 512:DM + (hf + 1) * 512],
                            start=(j == 0), stop=(j == 127)), reads=[dkc[kp][jj], rb], writes=[acc_ps])

            def S3(n_):
                t = tiles[n_]
                b = n_ % 2
                r = 1 if t < 2 else 0
                rows = slice(t * 128, (t + 1) * 128)
                x_, ss_ = xt[b], ss[b]
                P.op("dve", lambda e: e.tensor_tensor(out=m32[:], in0=acc_ps[:], in1=GA2[r][:], op=ALU.mult), reads=[acc_ps, GA2[r]], writes=[m32])
                P.op("dve", lambda e: e.tensor_tensor(out=x_[:], in0=x_[:], in1=m32[:], op=ALU.add), reads=[x_, m32], writes=[x_])
                if final and t >= 2:
                    P.op("act", lambda e: e.activation(out=junkb[:], in_=x_[:], func=AF.Square, accum_out=ss_[:, 4:5]),
                         reads=[x_], writes=[junkb, ss_])
                    P.op("dve", lambda e: e.tensor_scalar(out=ss_[:, 5:6], in0=ss_[:, 4:5], scalar1=1.0 / DM, scalar2=EPS,
                                                          op0=ALU.mult, op1=ALU.add), reads=[ss_], writes=[ss_])
                    P.op("act", lambda e: e.activation(out=ss_[:, 6:7], in_=ss_[:, 5:6], func=AF.Sqrt), reads=[ss_], writes=[ss_])
                    P.op("dve", lambda e: e.reciprocal(out=ss_[:, 7:8], in_=ss_[:, 6:7]), reads=[ss_], writes=[ss_])
                    P.op("dve", lambda e: e.scalar_tensor_tensor(out=x_[:], in0=x_[:], scalar=ss_[:, 7:8], in1=gfin[:],
                                                                 op0=ALU.mult, op1=ALU.mult), reads=[x_, ss_, gfin], writes=[x_])
                    P.dma("sp", lambda e: e.dma_start(out=self.Y[(t - 2) * 128:(t - 1) * 128, :], in_=x_[:]), reads=[x_])
                else:
                    P.dma("sp", lambda e: e.dma_start(out=self.X[rows, :], in_=x_[:]), reads=[x_])

            nT = len(tiles)
            drain(S1(0))
            for n_ in range(nT):
                gen = S1(n_ + 1) if n_ + 1 < nT else None
                prev = None
                for k in range(NBATCH):
                    bufs = batch_front(n_, k)
                    if prev is not None:
                        batch_back(n_, k - 1, prev)
                    prev = bufs
                    gen = drain(gen, 3)
                batch_back(n_, NBATCH - 1, prev)
                drain(gen)
                S3(n_)
            P.barrier()

    def build(self):
        c = self.cfg
        with contextlib.ExitStack() as st:
            self.consts(st)
            for l in c.layers:
                last = (l == c.depth - 1)
                with contextlib.ExitStack() as modst:
                    if "M" in c.phases:
                        self.phase_mod(l, modst)
                    if "A" in c.phases:
                        self.phase_a(l)
                if "B" in c.phases:
                    self.phase_b(l)
                if "C" in c.phases:
                    self.phase_c(l, not last)
                if "D" in c.phases:
                    self.phase_d(l, not last)
                if "E" in c.phases:
                    self.phase_e(l, not last, last and c.final)
            self.P.barrier()
            with self.nc.allow_non_contiguous_dma(reason='small strided layout DMAs'):
                self.P.build()
        return self.nc


def host_consts(n_lat):
    p = np.arange(64)
    f = p % 64
    i = f % 16
    freq = (10000.0 ** (-(i.astype(np.float32)) / 16.0)).astype(np.float32)
    rope = np.zeros((n_lat, 64, 2, 128), np.float32)
    j = np.arange(128)
    for m in range(n_lat):
        row = (2 * m + j // 64).astype(np.float32)
        col = (j % 64).astype(np.float32)
        pos = np.where((f < 32)[:, None], row[None, :], col[None, :]).astype(np.float32)
        ang = pos * freq[:, None]
        rope[m, :, 0, :] = np.cos(ang)
        rope[m, :, 1, :] = np.sin(ang)
    rotm = np.zeros((64, 64), np.float32)
    for m_ in range(64):
        fm = m_ % 64
        if (fm % 32) < 16:
            rotm[m_ + 16, m_] = -1.0
        else:
            rotm[m_ - 16, m_] = 1.0
    return rope, rotm


def host_bias(rpb, pats):
    L = rpb.shape[0]
    out = np.empty((L, len(pats), 128, 8, 5, 128), np.float32)
    for pi, (valid, roff, coff) in enumerate(pats):
        g = rpb[:, :, roff, coff]
        g = np.where(valid[None, None], g, np.float32(NEG))
        out[:, pi] = g.transpose(0, 3, 1, 2, 4)
    return out


_CACHE = {}


def make_inputs(inputs, cfg, cores):
    n_lat = cfg.n_lat
    rope, rotm = host_consts(n_lat)
    _, _, pats = na_patterns(n_lat)
    shared = dict(
        w_mod=inputs["w_mod"], b_mod=inputs["b_mod"], g_norm1=inputs["g_norm1"], w_in=inputs["w_in"],
        w_alpha_f=inputs["w_alpha_f"], b_alpha_f=inputs["b_alpha_f"], w_alpha_b=inputs["w_alpha_b"], b_alpha_b=inputs["b_alpha_b"],
        g_gla=inputs["g_gla"], biasT=host_bias(np.asarray(inputs["rpb"]), pats), w_proj_gla=inputs["w_proj_gla"],
        w_proj_na=inputs["w_proj_na"], w_out=inputs["w_out"], g_norm2=inputs["g_norm2"], w_query=inputs["w_query"],
        skT=np.ascontiguousarray(np.asarray(inputs["sub_keys"]).reshape(-1, 16, 128, 128).transpose(0, 1, 3, 2)),
        expert_u=inputs["expert_u"], expert_v=inputs["expert_v"], g_final=inputs["g_final"], rope=rope, rotm=rotm)
    shared = {k: np.ascontiguousarray(np.asarray(v, dtype=np.float32)) for k, v in shared.items()}
    maps = []
    for b in cores:
        xin = np.concatenate([np.asarray(inputs["ctx"][b]), np.asarray(inputs["x"][b][:n_lat * 128])], axis=0).astype(np.float32)
        cv = np.stack([np.asarray(inputs["c"][b]), np.asarray(inputs["c_ctx"])], axis=-1).astype(np.float32)
        cvec = np.ascontiguousarray(cv.reshape(8, 128, 2).transpose(1, 0, 2))
        m = dict(shared)
        m["xin"] = np.ascontiguousarray(xin)
        m["cvec"] = cvec
        maps.append(m)
    return maps


def kernel(**inputs):
    cfg = Cfg()
    if "nc" not in _CACHE:
        _CACHE["nc"] = Builder(cfg).build()
    nc = _CACHE["nc"]
    maps = make_inputs(inputs, cfg, list(range(8)))
    res = run_bass_kernel_spmd(nc, maps, core_ids=list(range(8)))
    out = np.stack([np.asarray(r["Y"]) for r in res.results], axis=0).astype(np.float32)
    return out
```

```python
import contextlib
import numpy as np
import concourse.bass as bass
import concourse.mybir as mybir
from concourse.bass_utils import run_bass_kernel_spmd

F32 = mybir.dt.float32
BF16 = mybir.dt.bfloat16
I32 = mybir.dt.int32
U32 = mybir.dt.uint32
AF = mybir.ActivationFunctionType
ALU = mybir.AluOpType
AX = mybir.AxisListType

N_DMA_SLOTS = 20
DM = 1024
EPS = 1e-6
NEG = -30000.0


class T:
    __slots__ = ("h", "w", "r", "name")

    def __init__(self, h=None, name=""):
        self.h = h
        self.w = None
        self.r = []
        self.name = name

    def __getitem__(self, k):
        return self.h[k]


class Prog:
    ENG = ("pe", "act", "dve", "pool", "sp")

    def __init__(self, nc):
        self.nc = nc
        self.ops = {e: [] for e in self.ENG}
        self.seq = {e: 0 for e in self.ENG}
        self.known = {e: {} for e in self.ENG}
        self.known_ver = {e: None for e in self.ENG}
        self.dcount = {}
        self.dnext = {e: 0 for e in self.ENG}
        self.last_dma = {}
        self.n_instr = 0

    def _snap(self, eng):
        s = self.known_ver[eng]
        if s is None:
            s = dict(self.known[eng])
            self.known_ver[eng] = s
        return s

    def _learn(self, eng, d):
        kn = self.known[eng]
        ch = False
        for k, v in d.items():
            if kn.get(k, 0) < v:
                kn[k] = v
                ch = True
        if ch:
            self.known_ver[eng] = None

    @staticmethod
    def _dep_events(reads, writes):
        ev = []
        for r in reads:
            if r.w is not None:
                ev.append(r.w)
        for w in writes:
            if w.w is not None:
                ev.append(w.w)
            ev.extend(w.r)
        return ev

    def _waits_for(self, eng, events):
        kn = self.known[eng]
        waits = []
        for ev in sorted(events, key=lambda e: -e[1]):
            sk, val, src, snap = ev
            if src == eng and eng == "pe":
                continue
            if kn.get(sk, 0) >= val:
                continue
            waits.append((sk, val))
            kn[sk] = val
            self.known_ver[eng] = None
            if snap is not None:
                self._learn(eng, snap)
        out = []
        seen = {}
        for sk, val in waits:
            if seen.get(sk, 0) >= val:
                continue
            seen[sk] = val
            out.append((sk, val))
        return out

    def op(self, eng, fn, reads=(), writes=()):
        events = self._dep_events(reads, writes)
        waits = self._waits_for(eng, events)
        self.seq[eng] += 1
        me = (("e", eng), self.seq[eng], eng, self._snap(eng))
        self.ops[eng].append((waits, fn, (("e", eng), 1)))
        for r in reads:
            r.r.append(me)
        for w in writes:
            w.w = me
            w.r = []
        self.n_instr += 1 + max(0, len(waits) - 1)
        return me

    def dma(self, q, fn, reads=(), writes=()):
        events = self._dep_events(reads, writes)
        slot = self.dnext[q] % N_DMA_SLOTS
        self.dnext[q] += 1
        sk = ("d", q, slot)
        cnt = self.dcount.get(sk, 0)
        if cnt > 0:
            events = list(events) + [self.last_dma[sk]]
        waits = self._waits_for(q, events)
        self.dcount[sk] = cnt + 16
        me = (sk, cnt + 16, "dma", self._snap(q))
        self.last_dma[sk] = me
        self.ops[q].append((waits, fn, (sk, 16)))
        for r in reads:
            r.r.append(me)
        for w in writes:
            w.w = me
            w.r = []
        self.n_instr += 1 + max(0, len(waits) - 1)
        return me

    def barrier(self):
        events = []
        for e in self.ENG:
            if self.seq[e] > 0:
                events.append((("e", e), self.seq[e], e, None))
        for sk, ev in self.last_dma.items():
            events.append(ev)
        for e in self.ENG:
            kn = self.known[e]
            waits = []
            for sk, val, src, snap in events:
                if kn.get(sk, 0) >= val:
                    continue
                kn[sk] = val
                waits.append((sk, val))
            self.known_ver[e] = None
            if waits:
                self.ops[e].append((waits, None, None))
                self.n_instr += len(waits)

    def build(self):
        nc = self.nc
        keys = set()
        for e in self.ENG:
            for waits, fn, inc in self.ops[e]:
                for sk, _ in waits:
                    keys.add(sk)
                if inc is not None:
                    keys.add(inc[0])
        keys = sorted(keys, key=str)
        with contextlib.ExitStack() as st:
            semh = {}
            for i, k in enumerate(keys):
                semh[k] = st.enter_context(nc.semaphore("s%d" % i))
            block = st.enter_context(nc.Block())

            def runner(e):
                def run(engh):
                    for waits, fn, inc in self.ops[e]:
                        if fn is None:
                            for sk, val in waits:
                                engh.wait_ge(semh[sk], val)
                            continue
                        for sk, val in waits[1:]:
                            engh.wait_ge(semh[sk], val)
                        ins = fn(engh)
                        if waits:
                            ins._wait_ge(semh[waits[0][0]], waits[0][1])
                        ins.then_inc(semh[inc[0]], inc[1])
                return run

            block.tensor(runner("pe"))
            block.scalar(runner("act"))
            block.vector(runner("dve"))
            block.gpsimd(runner("pool"))
            block.sync(runner("sp"))
        return nc


class Cfg:
    def __init__(self, n_lat=64, layers=(0, 1, 2, 3), depth=4, debug=False, phases="MABCDE", final=True):
        self.n_lat = n_lat
        self.nt = n_lat + 2
        self.layers = tuple(layers)
        self.depth = depth
        self.debug = debug
        self.phases = phases
        self.final = final


def na_patterns(n_lat):
    rows = n_lat * 2
    pats, ids, starts, keymap = [], [], [], {}
    kp = np.arange(128)
    for m in range(n_lat):
        st = int(np.clip(m - 2, 0, n_lat - 5))
        j = np.arange(128)
        r = 2 * m + j // 64
        c = j % 64
        r0 = np.clip(r - 4, 0, rows - 8)
        c0 = np.clip(c - 8, 0, 64 - 16)
        kb = np.arange(5)
        kr = (st * 2 + kb[:, None] * 2 + (kp[None, :] // 64))[:, :, None]
        kc = (kp % 64)[None, :, None] + np.zeros((5, 1, 1), np.int64)
        valid = (kr >= r0[None, None, :]) & (kr < r0[None, None, :] + 8) & (kc >= c0[None, None, :]) & (kc < c0[None, None, :] + 16)
        roff = np.where(valid, kr - r[None, None, :] + 7, 0)
        coff = np.where(valid, kc - c[None, None, :] + 15, 0)
        key = (valid.tobytes(), roff.tobytes(), coff.tobytes())
        if key not in keymap:
            keymap[key] = len(pats)
            pats.append((valid, roff, coff))
        ids.append(keymap[key])
        starts.append(st)
    return ids, starts, pats


class Builder:
    def __init__(self, cfg):
        self.cfg = cfg
        nc = bass.Bass("TRN2", target_bir_lowering=False)
        self.nc = nc
        self.P = Prog(nc)
        self.out_names = []
        self.pat_ids, self.pat_starts, self.pats = na_patterns(cfg.n_lat)
        self.npat = len(self.pats)
        cnt = np.bincount(self.pat_ids)
        self.pat_main = int(np.argmax(cnt))
        self._declare()

    def dram_in(self, name, shape, dt=F32):
        return self.nc.dram_tensor(name, list(shape), dt, kind="ExternalInput").ap()

    def dram_scr(self, name, shape, dt=F32):
        kind = "ExternalOutput" if self.cfg.debug else "Internal"
        if self.cfg.debug:
            self.out_names.append(name)
        return self.nc.dram_tensor(name, list(shape), dt, kind=kind).ap()

    def _declare(self):
        c = self.cfg
        NT, NL = c.nt, c.depth
        TOK = NT * 128
        di = self.dram_in
        self.xin = di("xin", [TOK, DM])
        self.cvec = di("cvec", [128, 8, 2])
        self.w_mod = di("w_mod", [NL, DM, 6 * DM])
        self.b_mod = di("b_mod", [NL, 6 * DM])
        self.g1 = di("g_norm1", [NL, DM])
        self.w_in = di("w_in", [NL, DM, 5152])
        self.waf = di("w_alpha_f", [NL, 16, 256])
        self.baf = di("b_alpha_f", [NL, 256])
        self.wab = di("w_alpha_b", [NL, 16, 256])
        self.bab = di("b_alpha_b", [NL, 256])
        self.ggla = di("g_gla", [NL, 128])
        self.biasT = di("biasT", [NL, self.npat, 128, 8, 5, 128])
        self.w_pg = di("w_proj_gla", [NL, 512, DM])
        self.w_pn = di("w_proj_na", [NL, 512, DM])
        self.w_o = di("w_out", [NL, DM, DM])
        self.g2 = di("g_norm2", [NL, DM])
        self.w_q = di("w_query", [NL, DM, 2048])
        self.skT = di("skT", [NL, 16, 128, 128])
        self.eu = di("expert_u", [NL, 16384, DM])
        self.ev = di("expert_v", [NL, 16384, DM])
        self.gfin = di("g_final", [DM])
        self.rope = di("rope", [c.n_lat, 64, 2, 128])
        self.rotm = di("rotm", [64, 64])
        ds = self.dram_scr
        self.X = ds("X", [TOK, DM])
        self.AUX = ds("AUX", [2, 4, DM])
        self.QKT = ds("QKT", [NT, 64, 8, 128], BF16)
        self.ZT = ds("ZT", [32, TOK])
        self.V = ds("V", [TOK, 512], BF16)
        self.RG = ds("RG", [TOK, 512], BF16)
        self.NQT = ds("NQT", [NT, 128, 4, 128], BF16)
        self.NKT = ds("NKT", [NT, 128, 4, 128], BF16)
        self.NVX = ds("NVX", [TOK, 520], BF16)
        self.G = ds("G", [TOK, 2048], BF16)
        self.OF = ds("OF", [TOK, 512])
        self.OB = ds("OB", [TOK, 512])
        self.NAO = ds("NAO", [TOK, 512], BF16)
        self.UVB = ds("UVB", [16384, 2 * DM], BF16)
        self.Y = self.nc.dram_tensor("Y", [c.n_lat * 128, DM], F32, kind="ExternalOutput").ap()

    def sb(self, st, name, shape, dt=F32):
        self._uid = getattr(self, "_uid", 0) + 1
        name = "%s_u%d" % (name, self._uid)
        h = st.enter_context(self.nc.sbuf_tensor(name, list(shape), dt))
        return T(h, name)

    def ps(self, st, name, shape, dt=F32):
        self._uid = getattr(self, "_uid", 0) + 1
        name = "%s_u%d" % (name, self._uid)
        h = st.enter_context(self.nc.psum_tensor(name, list(shape), dt))
        return T(h, name)

    def consts(self, st):
        P = self.P
        sb = self.sb
        self.identf = sb(st, "identf", [128, 128], F32)
        self.identb = sb(st, "identb", [128, 128], BF16)
        self.ones_row = sb(st, "ones_row", [1, 128], F32)
        self.triF = sb(st, "triF", [128, 128], F32)
        self.triB = sb(st, "triB", [128, 128], F32)
        self.mF = sb(st, "mF", [64, 64], F32)
        self.mB = sb(st, "mB", [64, 64], F32)
        self.rotb = sb(st, "rotb", [64, 64], BF16)
        self.cact = sb(st, "cact", [128, 8, 2], F32)
        idf, idb = self.identf, self.identb
        P.op("pool", lambda e: e.memset(idf[:], 1.0), writes=[idf])
        P.op("pool", lambda e: e.affine_select(out=idf[:], in_=idf[:], pattern=[[-1, 128]], compare_op=ALU.is_equal,
                                               fill=0.0, base=0, channel_multiplier=1), reads=[idf], writes=[idf])
        P.op("pool", lambda e: e.tensor_copy(out=idb[:], in_=idf[:]), reads=[idf], writes=[idb])
        P.op("pool", lambda e: e.memset(self.ones_row[:], 1.0), writes=[self.ones_row])
        v = -1.0 / 16.0
        tF, tB = self.triF, self.triB
        P.op("pool", lambda e: e.memset(tF[:], v), writes=[tF])
        P.op("pool", lambda e: e.affine_select(out=tF[:], in_=tF[:], pattern=[[1, 128]], compare_op=ALU.is_ge,
                                               fill=0.0, base=0, channel_multiplier=-1), reads=[tF], writes=[tF])
        P.op("pool", lambda e: e.memset(tF[0:64, 64:128], 0.0), reads=[tF], writes=[tF])
        P.op("pool", lambda e: e.memset(tB[:], v), writes=[tB])
        P.op("pool", lambda e: e.affine_select(out=tB[:], in_=tB[:], pattern=[[-1, 128]], compare_op=ALU.is_ge,
                                               fill=0.0, base=0, channel_multiplier=1), reads=[tB], writes=[tB])
        P.op("pool", lambda e: e.memset(tB[64:128, 0:64], 0.0), reads=[tB], writes=[tB])
        mF, mB = self.mF, self.mB
        P.op("pool", lambda e: e.memset(mF[:], 1.0), writes=[mF])
        P.op("pool", lambda e: e.affine_select(out=mF[:], in_=mF[:], pattern=[[1, 64]], compare_op=ALU.is_ge,
                                               fill=0.0, base=0, channel_multiplier=-1), reads=[mF], writes=[mF])
        P.op("pool", lambda e: e.memset(mB[:], 1.0), writes=[mB])
        P.op("pool", lambda e: e.affine_select(out=mB[:], in_=mB[:], pattern=[[-1, 64]], compare_op=ALU.is_ge,
                                               fill=0.0, base=0, channel_multiplier=1), reads=[mB], writes=[mB])
        with contextlib.ExitStack() as s2:
            rf = self.sb(s2, "rotf", [64, 64], F32)
            cv = self.sb(s2, "cvt", [128, 8, 2], F32)
            sg = self.sb(s2, "csg", [128, 8, 2], F32)
            P.dma("sp", lambda e: e.dma_start(out=rf[:], in_=self.rotm[:, :]), writes=[rf])
            P.op("dve", lambda e: e.tensor_copy(out=self.rotb[:], in_=rf[:]), reads=[rf], writes=[self.rotb])
            P.dma("sp", lambda e: e.dma_start(out=cv[:], in_=self.cvec[:, :, :]), writes=[cv])
            P.op("act", lambda e: e.activation(out=sg[:], in_=cv[:], func=AF.Sigmoid), reads=[cv], writes=[sg])
            P.op("dve", lambda e: e.tensor_tensor(out=self.cact[:], in0=cv[:], in1=sg[:], op=ALU.mult),
                 reads=[cv, sg], writes=[self.cact])
            P.barrier()

    def phase_mod(self, l, modst):
        P, nc = self.P, self.nc
        self.A1T = self.sb(modst, "A1T", [128, 8, 2])
        self.B1T = self.sb(modst, "B1T", [128, 8, 2])
        with contextlib.ExitStack() as st:
            wm = [self.sb(st, "wm%d" % i, [128, 8, 1024]) for i in range(2)]
            bmT = self.sb(st, "bmT", [128, 48])
            g1T = self.sb(st, "g1T", [128, 8])
            g2T = self.sb(st, "g2T", [128, 8])
            modT = self.sb(st, "modT", [128, 48, 2])
            tmp = self.sb(st, "modtmp", [128, 8, 2])
            A2T = self.sb(st, "A2T", [128, 8, 2])
            mps = self.ps(st, "mod_ps", [128, 48, 2])
            ncd = nc.allow_non_contiguous_dma(reason="tiny per-feature vectors")
            st.enter_context(ncd)
            P.dma("sp", lambda e: e.dma_start(out=bmT[:], in_=self.b_mod[l].rearrange("(c p) -> p c", p=128)), writes=[bmT])
            P.dma("sp", lambda e: e.dma_start(out=g1T[:], in_=self.g1[l].rearrange("(c p) -> p c", p=128)), writes=[g1T])
            P.dma("sp", lambda e: e.dma_start(out=g2T[:], in_=self.g2[l].rearrange("(c p) -> p c", p=128)), writes=[g2T])
            for g in range(6):
                w = wm[g % 2]
                P.dma("sp", lambda e, w=w, g=g: e.dma_start(
                    out=w[:], in_=self.w_mod[l][:, g * 1024:(g + 1) * 1024].rearrange("(k p) n -> p k n", p=128)), writes=[w])
                for b in range(8):
                    blk = g * 8 + b
                    for k in range(8):
                        P.op("pe", lambda e, w=w, b=b, k=k, blk=blk: e.matmul(
                            mps[:, blk, :], lhsT=w[:, k, b * 128:(b + 1) * 128], rhs=self.cact[:, k, :],
                            start=(k == 0), stop=(k == 7)), reads=[w, self.cact], writes=[mps])
            P.op("dve", lambda e: e.tensor_tensor(out=modT[:], in0=mps[:], in1=bmT[:].unsqueeze(2).to_broadcast([128, 48, 2]),
                                                  op=ALU.add), reads=[mps, bmT], writes=[modT])
            P.op("dve", lambda e: e.tensor_scalar(out=tmp[:], in0=modT[:, 8:16, :], scalar1=1.0, scalar2=None, op0=ALU.add),
                 reads=[modT], writes=[tmp])
            P.op("dve", lambda e: e.tensor_tensor(out=self.A1T[:], in0=tmp[:], in1=g1T[:].unsqueeze(2).to_broadcast([128, 8, 2]),
                                                  op=ALU.mult), reads=[tmp, g1T], writes=[self.A1T])
            P.op("dve", lambda e: e.tensor_copy(out=self.B1T[:], in_=modT[:, 0:8, :]), reads=[modT], writes=[self.B1T])
            P.op("dve", lambda e: e.tensor_scalar(out=tmp[:], in0=modT[:, 32:40, :], scalar1=1.0, scalar2=None, op0=ALU.add),
                 reads=[modT], writes=[tmp])
            P.op("dve", lambda e: e.tensor_tensor(out=A2T[:], in0=tmp[:], in1=g2T[:].unsqueeze(2).to_broadcast([128, 8, 2]),
                                                  op=ALU.mult), reads=[tmp, g2T], writes=[A2T])
            srcs = [(A2T, None), (modT, 24), (modT, 16), (modT, 40)]
            for i, (t, off) in enumerate(srcs):
                for r in range(2):
                    if off is None:
                        P.dma("sp", lambda e, t=t, i=i, r=r: e.dma_start(
                            out=self.AUX[r, i, :].rearrange("(k p) -> p k", p=128), in_=t[:, :, r]), reads=[t])
                    else:
                        P.dma("sp", lambda e, t=t, i=i, r=r, off=off: e.dma_start(
                            out=self.AUX[r, i, :].rearrange("(k p) -> p k", p=128), in_=t[:, off:off + 8, r]), reads=[t])
            P.barrier()

    def phase_a(self, l):
        P, nc, c = self.P, self.nc, self.cfg
        NT = c.nt
        with contextlib.ExitStack() as st:
            wb = self.sb(st, "a_wb", [128, 8, 5152], BF16)
            stg = [self.sb(st, "a_stg%d" % i, [128, 2576]) for i in range(2)]
            for k in range(8):
                for hf in range(2):
                    s = stg[hf]
                    cs = slice(hf * 2576, (hf + 1) * 2576)
                    P.dma("sp", lambda e, s=s, k=k, cs=cs: e.dma_start(out=s[:], in_=self.w_in[l][k * 128:(k + 1) * 128, cs]), writes=[s])
                    if hf == 0:
                        P.op("act", lambda e, s=s, k=k, cs=cs: e.copy(out=wb[:, k, cs], in_=s[:]), reads=[s], writes=[wb])
                    else:
                        P.op("pool", lambda e, s=s, k=k, cs=cs: e.tensor_copy(out=wb[:, k, cs], in_=s[:]), reads=[s], writes=[wb])
            xt = [self.sb(st, "a_x%d" % i, [128, DM]) for i in range(2)]
            junk = self.sb(st, "a_junk", [128, DM], BF16)
            ss = [self.sb(st, "a_ss%d" % i, [128, 4]) for i in range(2)]
            xn = [self.sb(st, "a_xn%d" % i, [128, DM]) for i in range(2)]
            hT = [self.sb(st, "a_hT%d" % i, [128, 8, 128], BF16) for i in range(2)]
            vt = [self.sb(st, "a_v%d" % i, [128, 512], BF16) for i in range(2)]
            rt = [self.sb(st, "a_r%d" % i, [128, 512], BF16) for i in range(2)]
            rsg = [self.sb(st, "a_rsg%d" % i, [128, 512]) for i in range(2)]
            nvx = [self.sb(st, "a_nvx%d" % i, [128, 8, 65], BF16) for i in range(2)]
            gt = [self.sb(st, "a_g%d" % i, [128, 2048], BF16) for i in range(2)]
            qkb = [self.sb(st, "a_qkb%d" % i, [64, 8, 128], BF16) for i in range(2)]
            qkr = [self.sb(st, "a_qkr%d" % i, [64, 8, 128], BF16) for i in range(2)]
            t1 = [self.sb(st, "a_t1%d" % i, [64, 8, 128]) for i in range(2)]
            t2 = [self.sb(st, "a_t2%d" % i, [64, 8, 128]) for i in range(2)]
            zt = [self.sb(st, "a_z%d" % i, [32, 128]) for i in range(2)]
            nqt = [self.sb(st, "a_nq%d" % i, [128, 4, 128], BF16) for i in range(2)]
            nkt = [self.sb(st, "a_nk%d" % i, [128, 4, 128], BF16) for i in range(2)]
            rp = [self.sb(st, "a_rp%d" % i, [64, 2, 128]) for i in range(2)]
            tps = self.ps(st, "a_tps", [128, 8, 128])
            tok = [self.ps(st, "a_tok%d" % i, [128, 512]) for i in range(2)]
            fm = [self.ps(st, "a_fm%d" % i, [128, 4, 128]) for i in range(2)]
            rot = self.ps(st, "a_rot", [64, 8, 128])
            for i in range(2):
                P.op("pool", lambda e, i=i: e.memset(nvx[i][:], 1.0), writes=[nvx[i]])
            C_Q, C_K, C_V, C_Z, C_R, C_NQ, C_NK, C_NV, C_G = 0, 256, 512, 1024, 1056, 1568, 2080, 2592, 3104
            ntok = 0
            nfm = 0
            for t in range(NT):
                b = t % 2
                r = 1 if t < 2 else 0
                src = self.xin if l == 0 else self.X
                x_, ss_, xn_, hT_ = xt[b], ss[b], xn[b], hT[b]
                P.dma("sp", lambda e, x_=x_, t=t, src=src: e.dma_start(out=x_[:], in_=src[t * 128:(t + 1) * 128, :]), writes=[x_])
                P.op("act", lambda e, x_=x_, ss_=ss_: e.activation(out=junk[:], in_=x_[:], func=AF.Square, accum_out=ss_[:, 0:1]),
                     reads=[x_], writes=[junk, ss_])
                P.op("dve", lambda e, ss_=ss_: e.tensor_scalar(out=ss_[:, 1:2], in0=ss_[:, 0:1], scalar1=1.0 / DM, scalar2=EPS,
                                                               op0=ALU.mult, op1=ALU.add), reads=[ss_], writes=[ss_])
                P.op("act", lambda e, ss_=ss_: e.activation(out=ss_[:, 2:3], in_=ss_[:, 1:2], func=AF.Sqrt), reads=[ss_], writes=[ss_])
                P.op("dve", lambda e, ss_=ss_: e.reciprocal(out=ss_[:, 3:4], in_=ss_[:, 2:3]), reads=[ss_], writes=[ss_])
                P.op("dve", lambda e, x_=x_, xn_=xn_, ss_=ss_: e.tensor_scalar(out=xn_[:], in0=x_[:], scalar1=ss_[:, 3:4], scalar2=None,
                                                                               op0=ALU.mult), reads=[x_, ss_], writes=[xn_])
                for k in range(8):
                    P.op("pe", lambda e, xn_=xn_, k=k: e.transpose(out=tps[:, k, :], in_=xn_[:, k * 128:(k + 1) * 128],
                                                                   identity=self.identf[:]), reads=[xn_, self.identf], writes=[tps])
                for k in range(8):
                    P.op("act", lambda e, hT_=hT_, k=k, r=r: e.activation(
                        out=hT_[:, k, :], in_=tps[:, k, :], func=AF.Identity, scale=self.A1T[:, k, r:r + 1],
                        bias=self.B1T[:, k, r:r + 1]), reads=[tps, self.A1T, self.B1T], writes=[hT_])
                tm = [("v", C_V), ("r", C_R), ("nv", C_NV), ("g0", C_G), ("g1", C_G + 512), ("g2", C_G + 1024), ("g3", C_G + 1536)]
                for nm, c0 in tm:
                    pt = tok[ntok % 2]
                    ntok += 1
                    for k in range(8):
                        P.op("pe", lambda e, pt=pt, hT_=hT_, k=k, c0=c0: e.matmul(
                            pt[:], lhsT=hT_[:, k, :], rhs=wb[:, k, c0:c0 + 512], start=(k == 0), stop=(k == 7)),
                            reads=[hT_, wb], writes=[pt])
                    if nm == "v":
                        P.op("dve", lambda e, pt=pt, b=b: e.tensor_copy(out=vt[b][:], in_=pt[:]), reads=[pt], writes=[vt[b]])
                    elif nm == "r":
                        P.op("act", lambda e, pt=pt, b=b: e.activation(out=rsg[b][:], in_=pt[:], func=AF.Sigmoid),
                             reads=[pt], writes=[rsg[b]])
                        P.op("dve", lambda e, pt=pt, b=b: e.tensor_tensor(out=rt[b][:], in0=pt[:], in1=rsg[b][:], op=ALU.mult),
                             reads=[pt, rsg[b]], writes=[rt[b]])
                    elif nm == "nv":
                        P.op("dve", lambda e, pt=pt, b=b: e.tensor_copy(
                            out=nvx[b][:, :, 0:64], in_=pt[:].rearrange("p (h d) -> p h d", h=8)), reads=[pt], writes=[nvx[b]])
                    else:
                        gi = int(nm[1])
                        P.op("act", lambda e, pt=pt, b=b, gi=gi: e.activation(
                            out=gt[b][:, gi * 512:(gi + 1) * 512], in_=pt[:], func=AF.Sigmoid), reads=[pt], writes=[gt[b]])
                groups = [("q", [C_Q + i * 64 for i in range(4)]), ("k", [C_K + i * 64 for i in range(4)]), ("z", [C_Z]),
                          ("nq", [C_NQ + i * 128 for i in range(4)]), ("nk", [C_NK + i * 128 for i in range(4)])]
                for nm, cols in groups:
                    pf = fm[nfm % 2]
                    nfm += 1
                    for bi, c0 in enumerate(cols):
                        M = 32 if nm == "z" else (64 if nm in ("q", "k") else 128)
                        for k in range(8):
                            P.op("pe", lambda e, pf=pf, hT_=hT_, k=k, c0=c0, bi=bi, M=M: e.matmul(
                                pf[0:M, bi, :], lhsT=wb[:, k, c0:c0 + M], rhs=hT_[:, k, :], start=(k == 0), stop=(k == 7)),
                                reads=[hT_, wb], writes=[pf])
                    if nm in ("q", "k"):
                        o4 = 0 if nm == "q" else 4
                        P.op("act", lambda e, pf=pf, b=b, o4=o4: e.copy(out=qkb[b][:, o4:o4 + 4, :], in_=pf[0:64, :, :]), reads=[pf], writes=[qkb[b]])
                    elif nm == "z":
                        P.op("dve", lambda e, pf=pf, b=b: e.tensor_copy(out=zt[b][:], in_=pf[0:32, 0, :]), reads=[pf], writes=[zt[b]])
                    elif nm == "nq":
                        P.op("act", lambda e, pf=pf, b=b: e.copy(out=nqt[b][:], in_=pf[:]), reads=[pf], writes=[nqt[b]])
                    else:
                        P.op("dve", lambda e, pf=pf, b=b: e.tensor_copy(out=nkt[b][:], in_=pf[:]), reads=[pf], writes=[nkt[b]])
                if t >= 2:
                    P.dma("sp", lambda e, b=b, t=t: e.dma_start(out=rp[b][:], in_=self.rope[t - 2]), writes=[rp[b]])
                    for bi in range(8):
                        P.op("pe", lambda e, b=b, bi=bi: e.matmul(rot[:, bi, :], lhsT=self.rotb[:], rhs=qkb[b][:, bi, :],
                                                                 start=True, stop=True), reads=[qkb[b], self.rotb], writes=[rot])
                    P.op("dve", lambda e, b=b: e.tensor_tensor(out=t1[b][:], in0=qkb[b][:],
                                                               in1=rp[b][:, 0:1, :].to_broadcast([64, 8, 128]), op=ALU.mult),
                         reads=[qkb[b], rp[b]], writes=[t1[b]])
                    P.op("dve", lambda e, b=b: e.tensor_tensor(out=t2[b][:], in0=rot[:],
                                                               in1=rp[b][:, 1:2, :].to_broadcast([64, 8, 128]), op=ALU.mult),
                         reads=[rot, rp[b]], writes=[t2[b]])
                    P.op("pool", lambda e, b=b: e.tensor_tensor(out=qkr[b][:], in0=t1[b][:], in1=t2[b][:], op=ALU.add),
                         reads=[t1[b], t2[b]], writes=[qkr[b]])
                    qsrc = qkr[b]
                else:
                    qsrc = qkb[b]
                rows = slice(t * 128, (t + 1) * 128)
                P.dma("pool", lambda e, qsrc=qsrc, t=t: e.dma_start(out=self.QKT[t], in_=qsrc[:]), reads=[qsrc])
                P.dma("pool", lambda e, b=b, rows=rows: e.dma_start(out=self.ZT[:, rows], in_=zt[b][:]), reads=[zt[b]])
                P.dma("pool", lambda e, b=b, rows=rows: e.dma_start(out=self.V[rows, :], in_=vt[b][:]), reads=[vt[b]])
                P.dma("pool", lambda e, b=b, rows=rows: e.dma_start(out=self.RG[rows, :], in_=rt[b][:]), reads=[rt[b]])
                P.dma("pool", lambda e, b=b, t=t: e.dma_start(out=self.NQT[t], in_=nqt[b][:]), reads=[nqt[b]])
                P.dma("pool", lambda e, b=b, t=t: e.dma_start(out=self.NKT[t], in_=nkt[b][:]), reads=[nkt[b]])
                P.dma("pool", lambda e, b=b, rows=rows: e.dma_start(out=self.NVX[rows, :], in_=nvx[b][:].rearrange("p h d -> p (h d)")),
                      reads=[nvx[b]])
                P.dma("pool", lambda e, b=b, rows=rows: e.dma_start(out=self.G[rows, :], in_=gt[b][:]), reads=[gt[b]])
            P.barrier()

    def phase_b(self, l):
        P, nc, c = self.P, self.nc, self.cfg
        NT = c.nt
        with contextlib.ExitStack() as st:
            wa = {}
            ba = {}
            for d, (wsrc, bsrc) in (("f", (self.waf, self.baf)), ("b", (self.wab, self.bab))):
                wa[d] = self.sb(st, "b_wa" + d, [16, 256])
                ba[d] = self.sb(st, "b_ba" + d, [1, 256])
                P.dma("sp", lambda e, d=d, wsrc=wsrc: e.dma_start(out=wa[d][:], in_=wsrc[l]), writes=[wa[d]])
                P.dma("sp", lambda e, d=d, bsrc=bsrc: e.dma_start(out=ba[d][:], in_=bsrc[l:l + 1, :]), writes=[ba[d]])
            S32 = {d: self.sb(st, "b_S32" + d, [64, 4, 128]) for d in "fb"}
            Sbf = {d: self.sb(st, "b_Sbf" + d, [64, 4, 128], BF16) for d in "fb"}
            for d in "fb":
                P.op("pool", lambda e, d=d: e.memset(S32[d][:], 0.0), writes=[S32[d]])
                P.op("pool", lambda e, d=d: e.memset(Sbf[d][:], 0.0), writes=[Sbf[d]])
            mk = lambda nm, shp, dt=F32: {d: [self.sb(st, "b_%s%s%d" % (nm, d, i), shp, dt) for i in range(2)] for d in "fb"}
            zt = mk("z", [16, 128])
            qk = mk("qk", [64, 8, 128], BF16)
            vv = mk("v", [64, 2, 512], BF16)
            e1 = mk("e1", [128, 256])
            Lt = mk("L", [128, 256])
            eb = mk("eb", [64, 4, 128])
            enb = mk("enb", [64, 4, 128])
            qe = mk("qe", [64, 4, 128], BF16)
            ke = mk("ke", [64, 4, 128], BF16)
            ktok = mk("ktok", [64, 4, 64], BF16)
            Am = mk("Am", [64, 4, 64], BF16)
            Ot = mk("Ot", [64, 512])
            lb_ps = {d: self.ps(st, "b_lb" + d, [128, 512]) for d in "fb"}
            kT_ps = self.ps(st, "b_kT", [64, 4, 64], BF16)
            A_ps = {d: self.ps(st, "b_A" + d, [64, 4, 64]) for d in "fb"}
            O_ps = {d: self.ps(st, "b_O" + d, [64, 512]) for d in "fb"}
            dS_ps = self.ps(st, "b_dS", [64, 4, 128])
            tri = {"f": self.triF, "b": self.triB}
            msk = {"f": self.mF, "b": self.mB}
            OD = {"f": self.OF, "b": self.OB}
            order_f = list(range(NT))
            order_b = [1, 0] + list(range(NT - 1, 1, -1))
            cnt = {"f": 0, "b": 0}

            def emit(d, t):
                i = cnt[d] % 2
                cnt[d] += 1
                z_, qk_, v_, e1_, L_, eb_, enb_, qe_, ke_ = zt[d][i], qk[d][i], vv[d][i], e1[d][i], Lt[d][i], eb[d][i], enb[d][i], qe[d][i], ke[d][i]
                rows = slice(t * 128, (t + 1) * 128)
                zr = slice(0, 16) if d == "f" else slice(16, 32)
                P.dma("sp", lambda e: e.dma_start(out=z_[:], in_=self.ZT[zr, rows]), writes=[z_])
                P.dma("sp", lambda e: e.dma_start(out=qk_[:], in_=self.QKT[t]), writes=[qk_])
                P.dma("sp", lambda e: e.dma_start(out=v_[:], in_=self.V[rows, :].rearrange("(c p) n -> p c n", p=64)), writes=[v_])
                lp = lb_ps[d]
                la = lp[:, 0:256]
                bp = lp[0:64, :].rearrange("p (h n) -> p h n", h=4)
                P.op("pe", lambda e: e.matmul(la, lhsT=z_[:], rhs=wa[d][:], start=True, stop=False), reads=[z_, wa[d]], writes=[lp])
                P.op("pe", lambda e: e.matmul(la, lhsT=self.ones_row[:], rhs=ba[d][:], start=False, stop=True),
                     reads=[self.ones_row, ba[d]], writes=[lp])
                P.op("act", lambda e: e.activation(out=e1_[:], in_=la, func=AF.Exp, scale=-1.0), reads=[lp], writes=[e1_])
                P.op("act", lambda e: e.activation(out=L_[:], in_=e1_[:], func=AF.Ln, bias=1.0), reads=[e1_], writes=[L_])
                for h in range(4):
                    P.op("pe", lambda e, h=h: e.matmul(bp[:, h, :], lhsT=L_[:, h * 64:(h + 1) * 64], rhs=tri[d][:], start=True, stop=True),
                         reads=[L_, tri[d]], writes=[lp])
                P.op("act", lambda e: e.activation(out=eb_[:], in_=bp, func=AF.Exp), reads=[lp], writes=[eb_])
                P.op("act", lambda e: e.activation(out=enb_[:], in_=bp, func=AF.Exp, scale=-1.0), reads=[lp], writes=[enb_])
                P.op("dve", lambda e: e.scalar_tensor_tensor(out=qe_[:], in0=qk_[:, 0:4, :], scalar=0.125, in1=eb_[:],
                                                             op0=ALU.mult, op1=ALU.mult), reads=[qk_, eb_], writes=[qe_])
                P.op("dve", lambda e: e.tensor_tensor(out=ke_[:], in0=qk_[:, 4:8, :], in1=enb_[:], op=ALU.mult),
                     reads=[qk_, enb_], writes=[ke_])
                def chunk(c_):
                    cs = slice(c_ * 64, (c_ + 1) * 64)
                    kt_, Am_, Ot_ = ktok[d][c_], Am[d][c_], Ot[d][c_]
                    for h in range(4):
                        P.op("pe", lambda e, h=h: e.transpose(out=kT_ps[:, h, :], in_=ke_[:, h, cs], identity=self.identb[0:64, 0:64]),
                             reads=[ke_, self.identb], writes=[kT_ps])
                    P.op("act", lambda e: e.copy(out=kt_[:], in_=kT_ps[:]), reads=[kT_ps], writes=[kt_])
                    for h in range(4):
                        P.op("pe", lambda e, h=h: e.matmul(A_ps[d][:, h, :], lhsT=ke_[:, h, cs], rhs=qe_[:, h, cs], start=True, stop=True),
                             reads=[ke_, qe_], writes=[A_ps[d]])
                    P.op("dve", lambda e: e.tensor_tensor(out=Am_[:], in0=A_ps[d][:],
                                                          in1=msk[d][:].unsqueeze(1).to_broadcast([64, 4, 64]), op=ALU.mult),
                         reads=[A_ps[d], msk[d]], writes=[Am_])
                    for h in range(4):
                        P.op("pe", lambda e, h=h: e.matmul(O_ps[d][:, h * 128:(h + 1) * 128], lhsT=Am_[:, h, :],
                                                          rhs=v_[:, c_, h * 128:(h + 1) * 128], start=True, stop=False),
                             reads=[Am_, v_], writes=[O_ps[d]])
                        P.op("pe", lambda e, h=h: e.matmul(O_ps[d][:, h * 128:(h + 1) * 128], lhsT=qe_[:, h, cs], rhs=Sbf[d][:, h, :],
                                                          start=False, stop=True), reads=[qe_, Sbf[d]], writes=[O_ps[d]])
                    for h in range(4):
                        P.op("pe", lambda e, h=h: e.matmul(dS_ps[:, h, :], lhsT=kt_[:, h, :], rhs=v_[:, c_, h * 128:(h + 1) * 128],
                                                          start=True, stop=True), reads=[kt_, v_], writes=[dS_ps])
                    P.op("act", lambda e: e.copy(out=Ot_[:], in_=O_ps[d][:]), reads=[O_ps[d]], writes=[Ot_])
                    r0 = t * 128 + c_ * 64
                    P.dma("pool", lambda e: e.dma_start(out=OD[d][r0:r0 + 64, :], in_=Ot_[:]), reads=[Ot_])
                    P.op("dve", lambda e: e.tensor_tensor(out=S32[d][:], in0=S32[d][:], in1=dS_ps[:], op=ALU.add),
                         reads=[S32[d], dS_ps], writes=[S32[d]])
                    tcol = c_ * 64 + 63 if d == "f" else c_ * 64
                    P.op("dve", lambda e, tcol=tcol: e.tensor_tensor(
                        out=S32[d][:], in0=S32[d][:], in1=eb_[:, :, tcol:tcol + 1].to_broadcast([64, 4, 128]), op=ALU.mult),
                        reads=[S32[d], eb_], writes=[S32[d]])
                    P.op("act", lambda e: e.copy(out=Sbf[d][:], in_=S32[d][:]), reads=[S32[d]], writes=[Sbf[d]])

                for c_ in ((0, 1) if d == "f" else (1, 0)):
                    chunk(c_)

            f32t = [self.sb(st, "b_cf%d" % i, [128, 8, DM]) for i in range(2)]
            b16t = [self.sb(st, "b_cb%d" % i, [128, 8, DM], BF16) for i in range(2)]
            conv = [(src, off, i) for (src, off) in ((self.eu, 0), (self.ev, DM)) for i in range(16)]

            def conv_chunk(n):
                src, off, i = conv[n]
                a, bb = f32t[n % 2], b16t[n % 2]
                rs = slice(i * 1024, (i + 1) * 1024)
                P.dma("sp", lambda e: e.dma_start(out=a[:], in_=src[l][rs, :].rearrange("(p r) d -> p r d", r=8)), writes=[a])
                P.op("pool", lambda e: e.tensor_copy(out=bb[:], in_=a[:]), reads=[a], writes=[bb])
                P.dma("pool", lambda e: e.dma_start(out=self.UVB[rs, off:off + DM].rearrange("(p r) d -> p r d", r=8), in_=bb[:]), reads=[bb])

            nconv = 0
            for s_ in range(NT):
                emit("f", order_f[s_])
                emit("b", order_b[s_])
                if nconv < len(conv) and (s_ % 2 == 0 or NT - s_ <= len(conv) - nconv):
                    conv_chunk(nconv)
                    nconv += 1
            while nconv < len(conv):
                conv_chunk(nconv)
                nconv += 1
            P.barrier()

    def phase_c(self, l, with_ctx):
        P, nc, c = self.P, self.nc, self.cfg
        with contextlib.ExitStack() as st:
            st.enter_context(nc.allow_non_contiguous_dma(reason="tile-blocked K layout"))
            kTc = self.sb(st, "c_kTc", [128, 2, 4, 128], BF16)
            vxc = self.sb(st, "c_vxc", [128, 2, 520], BF16)
            biasM = self.sb(st, "c_biasM", [128, 8, 5, 128])
            biasE = self.sb(st, "c_biasE", [128, 8, 5, 128])
            P.dma("sp", lambda e: e.dma_start(out=kTc[:], in_=self.NKT[0:2].rearrange("t p b k -> p t b k")), writes=[kTc])
            P.dma("sp", lambda e: e.dma_start(out=vxc[:], in_=self.NVX[0:256, :].rearrange("(t p) d -> p t d", p=128)), writes=[vxc])
            P.dma("sp", lambda e: e.dma_start(out=biasM[:], in_=self.biasT[l, self.pat_main]), writes=[biasM])
            qT = [self.sb(st, "c_qT%d" % i, [128, 4, 128], BF16) for i in range(2)]
            kT = [self.sb(st, "c_kT%d" % i, [128, 5, 4, 128], BF16) for i in range(2)]
            vx = [self.sb(st, "c_vx%d" % i, [128, 5, 520], BF16) for i in range(2)]
            Tt = [self.sb(st, "c_T%d" % i, [128, 5, 128]) for i in range(2)]
            PT = [self.sb(st, "c_PT%d" % i, [128, 7, 128], BF16) for i in range(2)]
            rec = [self.sb(st, "c_rec%d" % i, [128, 8]) for i in range(2)]
            ot = [self.sb(st, "c_o%d" % i, [128, 8, 64], BF16) for i in range(2)]
            S_ps = [self.ps(st, "c_S%d" % i, [128, 8, 128]) for i in range(2)]
            O_ps = [[self.ps(st, "c_O%d%d" % (i, j), [128, 4, 65]) for j in range(2)] for i in range(2)]
            tiles = ([0, 1] if with_ctx else []) + list(range(2, c.nt))
            cur_edge = None
            nh = 0
            for n_, t in enumerate(tiles):
                b = n_ % 2
                win = t >= 2
                P.dma("sp", lambda e, b=b, t=t: e.dma_start(out=qT[b][:], in_=self.NQT[t]), writes=[qT[b]])
                if win:
                    m = t - 2
                    s0 = 2 + self.pat_starts[m]
                    P.dma("sp", lambda e, b=b, s0=s0: e.dma_start(out=kT[b][:], in_=self.NKT[s0:s0 + 5].rearrange("t p b k -> p t b k")),
                          writes=[kT[b]])
                    P.dma("sp", lambda e, b=b, s0=s0: e.dma_start(
                        out=vx[b][:], in_=self.NVX[s0 * 128:(s0 + 5) * 128, :].rearrange("(t p) d -> p t d", p=128)), writes=[vx[b]])
                    pid = self.pat_ids[m]
                    if pid == self.pat_main:
                        bias = biasM
                    else:
                        if cur_edge != pid:
                            P.dma("sp", lambda e, pid=pid: e.dma_start(out=biasE[:], in_=self.biasT[l, pid]), writes=[biasE])
                            cur_edge = pid
                        bias = biasE
                for h in range(8):
                    blk, po = h // 2, (h % 2) * 64
                    sp_ = S_ps[nh % 2]
                    T_, PT_ = Tt[nh % 2], PT[nh % 2]
                    nh += 1
                    if win:
                        for kb in range(5):
                            P.op("pe", lambda e, kb=kb, b=b, blk=blk, po=po, sp_=sp_: e.matmul(
                                sp_[:, kb, :], lhsT=kT[b][po:po + 64, kb, blk, :], rhs=qT[b][po:po + 64, blk, :], start=True, stop=True),
                                reads=[kT[b], qT[b]], writes=[sp_])
                    for cb in range(2):
                        P.op("pe", lambda e, cb=cb, b=b, blk=blk, po=po, sp_=sp_: e.matmul(
                            sp_[:, 5 + cb, :], lhsT=kTc[po:po + 64, cb, blk, :], rhs=qT[b][po:po + 64, blk, :], start=True, stop=True),
                            reads=[kTc, qT[b]], writes=[sp_])
                    if win:
                        P.op("dve", lambda e, sp_=sp_, T_=T_, h=h, bias=bias: e.scalar_tensor_tensor(
                            out=T_[:], in0=sp_[:, 0:5, :], scalar=0.125, in1=bias[:, h, :, :], op0=ALU.mult, op1=ALU.add),
                            reads=[sp_, bias], writes=[T_])
                        P.op("act", lambda e, T_=T_, PT_=PT_: e.activation(out=PT_[:, 0:5, :], in_=T_[:], func=AF.Exp),
                             reads=[T_], writes=[PT_])
                    P.op("act", lambda e, sp_=sp_, PT_=PT_: e.activation(out=PT_[:, 5:7, :], in_=sp_[:, 5:7, :], func=AF.Exp, scale=0.125),
                         reads=[sp_], writes=[PT_])
                    op_ = O_ps[b][h // 4]
                    hl = h % 4
                    if win:
                        for kb in range(5):
                            P.op("pe", lambda e, kb=kb, b=b, h=h, op_=op_, hl=hl, PT_=PT_: e.matmul(
                                op_[:, hl, :], lhsT=PT_[:, kb, :], rhs=vx[b][:, kb, h * 65:(h + 1) * 65], start=(kb == 0), stop=False),
                                reads=[PT_, vx[b]], writes=[op_])
                    for cb in range(2):
                        P.op("pe", lambda e, cb=cb, h=h, op_=op_, hl=hl, PT_=PT_, win=win: e.matmul(
                            op_[:, hl, :], lhsT=PT_[:, 5 + cb, :], rhs=vxc[:, cb, h * 65:(h + 1) * 65],
                            start=(cb == 0 and not win), stop=(cb == 1)), reads=[PT_, vxc], writes=[op_])
                for j in range(2):
                    op_ = O_ps[b][j]
                    P.op("dve", lambda e, b=b, j=j, op_=op_: e.reciprocal(out=rec[b][:, j * 4:(j + 1) * 4], in_=op_[:, :, 64]),
                         reads=[op_], writes=[rec[b]])
                    P.op("dve", lambda e, b=b, j=j, op_=op_: e.tensor_tensor(
                        out=ot[b][:, j * 4:(j + 1) * 4, :], in0=op_[:, :, 0:64],
                        in1=rec[b][:, j * 4:(j + 1) * 4].unsqueeze(2).to_broadcast([128, 4, 64]), op=ALU.mult),
                        reads=[op_, rec[b]], writes=[ot[b]])
                P.dma("pool", lambda e, b=b, t=t: e.dma_start(out=self.NAO[t * 128:(t + 1) * 128, :],
                                                            in_=ot[b][:].rearrange("p h d -> p (h d)")), reads=[ot[b]])
            P.barrier()

    def phase_d(self, l, with_ctx):
        P, nc, c = self.P, self.nc, self.cfg
        with contextlib.ExitStack() as st:
            wpg = self.sb(st, "d_wpg", [128, 4, DM], BF16)
            wpn = self.sb(st, "d_wpn", [128, 4, DM], BF16)
            wo = self.sb(st, "d_wo", [128, 8, DM], BF16)
            stg = [self.sb(st, "d_stg%d" % i, [128, DM]) for i in range(2)]
            n = 0
            for (dst, src, nk) in ((wpg, self.w_pg, 4), (wpn, self.w_pn, 4), (wo, self.w_o, 8)):
                for k in range(nk):
                    s = stg[n % 2]
                    P.dma("sp", lambda e, s=s, k=k, src=src: e.dma_start(out=s[:], in_=src[l][k * 128:(k + 1) * 128, :]), writes=[s])
                    if n % 2 == 0:
                        P.op("act", lambda e, s=s, k=k, dst=dst: e.copy(out=dst[:, k, :], in_=s[:]), reads=[s], writes=[dst])
                    else:
                        P.op("pool", lambda e, s=s, k=k, dst=dst: e.tensor_copy(out=dst[:, k, :], in_=s[:]), reads=[s], writes=[dst])
                    n += 1
            gg = self.sb(st, "d_gg", [128, 128])
            ga1 = [self.sb(st, "d_ga1_%d" % r, [128, DM]) for r in range(2)]
            P.dma("sp", lambda e: e.dma_start(out=gg[:], in_=self.ggla[l:l + 1, :].partition_broadcast(128)), writes=[gg])
            for r in range(2):
                P.dma("sp", lambda e, r=r: e.dma_start(out=ga1[r][:], in_=self.AUX[r, 2:3, :].partition_broadcast(128)), writes=[ga1[r]])
            of = [self.sb(st, "d_of%d" % i, [128, 4, 128]) for i in range(2)]
            ob = [self.sb(st, "d_ob%d" % i, [128, 4, 128]) for i in range(2)]
            rg = [self.sb(st, "d_rg%d" % i, [128, 512], BF16) for i in range(2)]
            nao = [self.sb(st, "d_nao%d" % i, [128, 512], BF16) for i in range(2)]
            gt = [self.sb(st, "d_g%d" % i, [128, 2048], BF16) for i in range(2)]
            xt = [self.sb(st, "d_x%d" % i, [128, DM]) for i in range(2)]
            junk = self.sb(st, "d_junk", [128, 128], BF16)
            ss = [self.sb(st, "d_ss%d" % i, [128, 4, 4]) for i in range(2)]
            onb2 = [self.sb(st, "d_onb%d" % i, [128, 512], BF16) for i in range(2)]
            onT2 = [self.sb(st, "d_onT%d" % i, [128, 4, 128], BF16) for i in range(2)]
            naT2 = [self.sb(st, "d_naT%d" % i, [128, 4, 128], BF16) for i in range(2)]
            m1_2 = [self.sb(st, "d_m1%d" % i, [128, DM]) for i in range(2)]
            m2_2 = [self.sb(st, "d_m2%d" % i, [128, DM]) for i in range(2)]
            mb2 = [self.sb(st, "d_mb%d" % i, [128, DM], BF16) for i in range(2)]
            mT2 = [self.sb(st, "d_mT%d" % i, [128, 8, 128], BF16) for i in range(2)]
            tpA = self.ps(st, "d_tpA", [128, 8, 128], BF16)
            tpB = self.ps(st, "d_tpB", [128, 8, 128], BF16)
            ya = self.ps(st, "d_ya", [128, DM])
            yb = self.ps(st, "d_yb", [128, DM])
            yy = self.ps(st, "d_yy", [128, DM])
            tiles = ([0, 1] if with_ctx else []) + list(range(2, c.nt))

            def d_tile(n_, t):
                b = n_ % 2
                r = 1 if t < 2 else 0
                rows = slice(t * 128, (t + 1) * 128)
                of_, ob_, rg_, nao_, g_, x_, ss_ = of[b], ob[b], rg[b], nao[b], gt[b], xt[b], ss[b]
                onb, onT, naT, m1, m2, mb, mT = onb2[b], onT2[b], naT2[b], m1_2[b], m2_2[b], mb2[b], mT2[b]
                P.dma("sp", lambda e, of_=of_, rows=rows: e.dma_start(out=of_[:].rearrange("p h d -> p (h d)"), in_=self.OF[rows, :]), writes=[of_])
                P.dma("sp", lambda e, ob_=ob_, rows=rows: e.dma_start(out=ob_[:].rearrange("p h d -> p (h d)"), in_=self.OB[rows, :]), writes=[ob_])
                P.dma("sp", lambda e, rg_=rg_, rows=rows: e.dma_start(out=rg_[:], in_=self.RG[rows, :]), writes=[rg_])
                P.dma("sp", lambda e, nao_=nao_, rows=rows: e.dma_start(out=nao_[:], in_=self.NAO[rows, :]), writes=[nao_])
                P.dma("sp", lambda e, g_=g_, rows=rows: e.dma_start(out=g_[:], in_=self.G[rows, :]), writes=[g_])
                P.dma("sp", lambda e, x_=x_, rows=rows, l=l: e.dma_start(out=x_[:], in_=(self.xin if l == 0 else self.X)[rows, :]), writes=[x_])
                P.op("dve", lambda e, of_=of_, ob_=ob_: e.tensor_tensor(out=of_[:], in0=of_[:], in1=ob_[:], op=ALU.add),
                     reads=[of_, ob_], writes=[of_])
                for h in range(4):
                    P.op("act", lambda e, of_=of_, ss_=ss_, h=h: e.activation(out=junk[:], in_=of_[:, h, :], func=AF.Square,
                                                                             accum_out=ss_[:, 0, h:h + 1]), reads=[of_], writes=[junk, ss_])
                P.op("dve", lambda e, ss_=ss_: e.tensor_scalar(out=ss_[:, 1, :], in0=ss_[:, 0, :], scalar1=1.0 / 128, scalar2=EPS,
                                                               op0=ALU.mult, op1=ALU.add), reads=[ss_], writes=[ss_])
                P.op("act", lambda e, ss_=ss_: e.activation(out=ss_[:, 2, :], in_=ss_[:, 1, :], func=AF.Sqrt), reads=[ss_], writes=[ss_])
                P.op("dve", lambda e, ss_=ss_: e.reciprocal(out=ss_[:, 3, :], in_=ss_[:, 2, :]), reads=[ss_], writes=[ss_])
                P.op("dve", lambda e, of_=of_, ss_=ss_: e.tensor_tensor(
                    out=of_[:], in0=of_[:], in1=ss_[:, 3, :].unsqueeze(2).to_broadcast([128, 4, 128]), op=ALU.mult),
                    reads=[of_, ss_], writes=[of_])
                P.op("pool", lambda e, of_=of_: e.tensor_tensor(
                    out=of_[:], in0=of_[:], in1=gg[:].unsqueeze(1).to_broadcast([128, 4, 128]), op=ALU.mult),
                    reads=[of_, gg], writes=[of_])
                P.op("dve", lambda e, of_=of_, rg_=rg_: e.tensor_tensor(out=onb[:], in0=of_[:].rearrange("p h d -> p (h d)"),
                                                                      in1=rg_[:], op=ALU.mult), reads=[of_, rg_], writes=[onb])
                for k in range(4):
                    P.op("pe", lambda e, k=k: e.transpose(out=tpA[:, k, :], in_=onb[:, k * 128:(k + 1) * 128], identity=self.identb[:]),
                         reads=[onb, self.identb], writes=[tpA])
                for k in range(4):
                    P.op("pe", lambda e, k=k, nao_=nao_: e.transpose(out=tpA[:, 4 + k, :], in_=nao_[:, k * 128:(k + 1) * 128],
                                                                    identity=self.identb[:]), reads=[nao_, self.identb], writes=[tpA])
                P.op("act", lambda e: e.copy(out=onT[:], in_=tpA[:, 0:4, :]), reads=[tpA], writes=[onT])
                P.op("act", lambda e: e.copy(out=naT[:], in_=tpA[:, 4:8, :]), reads=[tpA], writes=[naT])
                for (yp, xT, w) in ((ya, onT, wpg), (yb, naT, wpn)):
                    for cc in range(2):
                        for k in range(4):
                            P.op("pe", lambda e, yp=yp, xT=xT, w=w, cc=cc, k=k: e.matmul(
                                yp[:, cc * 512:(cc + 1) * 512], lhsT=xT[:, k, :], rhs=w[:, k, cc * 512:(cc + 1) * 512],
                                start=(k == 0), stop=(k == 3)), reads=[xT, w], writes=[yp])
                P.op("dve", lambda e, g_=g_: e.tensor_tensor(out=m1[:], in0=ya[:], in1=g_[:, 0:DM], op=ALU.mult),
                     reads=[ya, g_], writes=[m1])
                P.op("dve", lambda e, g_=g_: e.tensor_tensor(out=m2[:], in0=yb[:], in1=g_[:, DM:2 * DM], op=ALU.mult),
                     reads=[yb, g_], writes=[m2])
                P.op("pool", lambda e: e.tensor_tensor(out=mb[:], in0=m1[:], in1=m2[:], op=ALU.add), reads=[m1, m2], writes=[mb])
                for k in range(8):
                    P.op("pe", lambda e, k=k: e.transpose(out=tpB[:, k, :], in_=mb[:, k * 128:(k + 1) * 128], identity=self.identb[:]),
                         reads=[mb, self.identb], writes=[tpB])
                P.op("act", lambda e: e.copy(out=mT[:], in_=tpB[:]), reads=[tpB], writes=[mT])
                for cc in range(2):
                    for k in range(8):
                        P.op("pe", lambda e, cc=cc, k=k: e.matmul(yy[:, cc * 512:(cc + 1) * 512], lhsT=mT[:, k, :],
                                                                 rhs=wo[:, k, cc * 512:(cc + 1) * 512], start=(k == 0), stop=(k == 7)),
                             reads=[mT, wo], writes=[yy])
                P.op("dve", lambda e, r=r: e.tensor_tensor(out=m1[:], in0=yy[:], in1=ga1[r][:], op=ALU.mult),
                     reads=[yy, ga1[r]], writes=[m1])
                P.op("pool", lambda e, x_=x_: e.tensor_tensor(out=x_[:], in0=x_[:], in1=m1[:], op=ALU.add), reads=[x_, m1], writes=[x_])
                P.dma("pool", lambda e, x_=x_, rows=rows: e.dma_start(out=self.X[rows, :], in_=x_[:]), reads=[x_])

            for n_, t in enumerate(tiles):
                d_tile(n_, t)
            P.barrier()

    def phase_e(self, l, with_ctx, final):
        P, nc, c = self.P, self.nc, self.cfg
        NBUF = 18
        BS = 8
        NBATCH = 128 // BS
        with contextlib.ExitStack() as st:
            wq = self.sb(st, "e_wq", [128, 8, 2048], BF16)
            skb = self.sb(st, "e_skb", [128, 16, 128], BF16)
            with contextlib.ExitStack() as st2:
                stg = [self.sb(st2, "e_stg%d" % i, [128, 2048]) for i in range(2)]
                for k in range(8):
                    s = stg[k % 2]
                    P.dma("sp", lambda e, s=s, k=k: e.dma_start(out=s[:], in_=self.w_q[l][k * 128:(k + 1) * 128, :]), writes=[s])
                    if k % 2 == 0:
                        P.op("act", lambda e, s=s, k=k: e.copy(out=wq[:, k, :], in_=s[:]), reads=[s], writes=[wq])
                    else:
                        P.op("pool", lambda e, s=s, k=k: e.tensor_copy(out=wq[:, k, :], in_=s[:]), reads=[s], writes=[wq])
                s = stg[0]
                P.dma("sp", lambda e, s=s: e.dma_start(out=s[:].rearrange("p (g n) -> p g n", g=16), in_=self.skT[l].rearrange("g d n -> d g n")),
                      writes=[s])
                P.op("act", lambda e, s=s: e.copy(out=skb[:], in_=s[:].rearrange("p (g n) -> p g n", g=16)), reads=[s], writes=[skb])
                P.barrier()
            A2 = self.sb(st, "e_A2", [128, DM])
            B2 = self.sb(st, "e_B2", [128, DM])
            GA2 = [self.sb(st, "e_GA2_%d" % r, [128, DM]) for r in range(2)]
            for r in range(2):
                P.dma("sp", lambda e, r=r: e.dma_start(out=GA2[r][:], in_=self.AUX[r, 3:4, :].partition_broadcast(128)), writes=[GA2[r]])
            gfin = None
            if final:
                gfin = self.sb(st, "e_gfin", [128, DM])
                P.dma("sp", lambda e: e.dma_start(out=gfin[:], in_=self.gfin.rearrange("(o d) -> o d", o=1).partition_broadcast(128)),
                      writes=[gfin])
            xt = [self.sb(st, "e_x%d" % i, [128, DM]) for i in range(2)]
            ss = [self.sb(st, "e_ss%d" % i, [128, 8]) for i in range(2)]
            h2b = self.sb(st, "e_h2b", [128, DM], BF16)
            m32 = self.sb(st, "e_m32", [128, DM])
            junkb = self.sb(st, "e_junkb", [128, DM], BF16)
            junkd = self.sb(st, "e_junkd", [128, DM], BF16)
            h2T = self.sb(st, "e_h2T", [128, 8, 128], BF16)
            qpT = self.sb(st, "e_qpT", [128, 16, 128], BF16)
            sc = self.sb(st, "e_sc", [128, 16, 128])
            s8 = self.sb(st, "e_s8", [128, 16, 16])
            i8 = self.sb(st, "e_i8", [128, 16, 16], U32)
            i8f = self.sb(st, "e_i8f", [128, 16, 16])
            i0s = self.sb(st, "e_i0s", [128, 8, 16])
            cand = self.sb(st, "e_cand", [128, 8, 16, 16])
            pos = self.sb(st, "e_pos", [128, 8, 16], U32)
            aub = self.sb(st, "e_aub", [128, 2, 8, 16], U32)
            abf = self.sb(st, "e_abf", [128, 2, 8, 16])
            tbl = self.sb(st, "e_tbl", [128, 2, 8, 16])
            accd = self.sb(st, "e_accd", [128, 2, 8, 16])
            tmpd = [self.sb(st, "e_tmpd%d" % i, [128, 2, 8, 16]) for i in range(2)]
            c4 = self.sb(st, "e_c4", [128, 1], U32)
            c15 = self.sb(st, "e_c15", [128, 1], U32)
            P.op("pool", lambda e: e.memset(c4[:], 4), writes=[c4])
            P.op("pool", lambda e: e.memset(c15[:], 15), writes=[c15])
            posh = [T(None, "posh") for _ in range(8)]
            best = self.sb(st, "e_best", [128, 8, 16])
            eidf = self.sb(st, "e_eidf", [128, 128])
            eidx2 = [self.sb(st, "e_eidx%d" % i, [128, 128], I32) for i in range(2)]
            ex = self.sb(st, "e_ex", [128, 8, 16])
            sm = self.sb(st, "e_sm", [128, 8, 2])
            ggp = [self.sb(st, "e_g%d" % i, [128, 128]) for i in range(2)]
            actb = [self.sb(st, "e_act%d" % i, [128, BS]) for i in range(2)]
            actc = [[T(None, "actc") for _ in range(BS)] for _ in range(2)]
            dkc = [[T(None, "dkc") for _ in range(BS)] for _ in range(2)]
            scg = [T(None, "scg") for _ in range(16)]
            candh = [T(None, "candh") for _ in range(8)]
            besth = [T(None, "besth") for _ in range(8)]
            eidc = [T(None, "eidc") for _ in range(128)]
            cidxr = T(None, "cidxr")
            i8fr = T(None, "i8fr")
            ta = [self.sb(st, "e_ta%d" % i, [128, BS]) for i in range(2)]
            tb = [self.sb(st, "e_tb%d" % i, [128, BS]) for i in range(2)]
            wg = [self.sb(st, "e_wg%d" % i, [128, BS]) for i in range(2)]
            Dk = [self.sb(st, "e_Dk%d" % i, [128, BS, 128], BF16) for i in range(2)]
            ring = [self.sb(st, "e_ring%d" % i, [128, 2 * DM], BF16) for i in range(NBUF)]
            acc_ps = self.ps(st, "e_acc", [128, DM])
            h2p = [self.ps(st, "e_h2ps%d" % i, [128, DM]) for i in range(2)]
            tp_ps = self.ps(st, "e_tp", [128, 8, 128], BF16)
            qs_ps = self.ps(st, "e_qs", [128, 4, 128])
            tiles = ([0, 1] if with_ctx else []) + list(range(2, c.nt))
            state = {"cur_r": None, "nring": 0}

            def S1(n_):
                t = tiles[n_]
                b = n_ % 2
                r = 1 if t < 2 else 0
                rows = slice(t * 128, (t + 1) * 128)
                eidx = eidx2[b]
                gg = ggp[b]
                h2 = h2p[b]
                if state["cur_r"] != r:
                    for i_, dst in ((0, A2), (1, B2)):
                        P.dma("sp", lambda e, i_=i_, dst=dst, r=r: e.dma_start(out=dst[:], in_=self.AUX[r, i_:i_ + 1, :].partition_broadcast(128)),
                              writes=[dst])
                    state["cur_r"] = r
                x_, ss_ = xt[b], ss[b]
                P.dma("sp", lambda e: e.dma_start(out=x_[:], in_=self.X[rows, :]), writes=[x_])
                P.op("act", lambda e: e.activation(out=junkb[:], in_=x_[:], func=AF.Square, accum_out=ss_[:, 0:1]),
                     reads=[x_], writes=[junkb, ss_])
                P.op("dve", lambda e: e.tensor_scalar(out=ss_[:, 1:2], in0=ss_[:, 0:1], scalar1=1.0 / DM, scalar2=EPS,
                                                      op0=ALU.mult, op1=ALU.add), reads=[ss_], writes=[ss_])
                P.op("act", lambda e: e.activation(out=ss_[:, 2:3], in_=ss_[:, 1:2], func=AF.Sqrt), reads=[ss_], writes=[ss_])
                P.op("dve", lambda e: e.reciprocal(out=ss_[:, 3:4], in_=ss_[:, 2:3]), reads=[ss_], writes=[ss_])
                yield
                P.op("dve", lambda e: e.scalar_tensor_tensor(out=h2[:], in0=x_[:], scalar=ss_[:, 3:4], in1=A2[:],
                                                             op0=ALU.mult, op1=ALU.mult), reads=[x_, ss_, A2], writes=[h2])
                yield
                P.op("dve", lambda e: e.tensor_tensor(out=h2[:], in0=h2[:], in1=B2[:], op=ALU.add), reads=[h2, B2], writes=[h2])
                P.op("act", lambda e: e.copy(out=h2b[:], in_=h2[:]), reads=[h2], writes=[h2b])
                for k in range(8):
                    P.op("pe", lambda e, k=k: e.transpose(out=tp_ps[:, k, :], in_=h2b[:, k * 128:(k + 1) * 128], identity=self.identb[:]),
                         reads=[h2b, self.identb], writes=[tp_ps])
                P.op("act", lambda e: e.copy(out=h2T[:], in_=tp_ps[:]), reads=[tp_ps], writes=[h2T])
                yield
                for q4 in range(4):
                    for bi in range(4):
                        blk = q4 * 4 + bi
                        for k in range(8):
                            P.op("pe", lambda e, bi=bi, blk=blk, k=k: e.matmul(
                                qs_ps[:, bi, :], lhsT=wq[:, k, blk * 128:(blk + 1) * 128], rhs=h2T[:, k, :], start=(k == 0), stop=(k == 7)),
                                reads=[wq, h2T], writes=[qs_ps])
                    P.op("act", lambda e, q4=q4: e.copy(out=qpT[:, q4 * 4:(q4 + 1) * 4, :], in_=qs_ps[:]), reads=[qs_ps], writes=[qpT])
                    yield
                for q4 in range(4):
                    g0 = q4 * 4
                    for g4 in range(4):
                        g = g0 + g4
                        P.op("pe", lambda e, g=g, g4=g4: e.matmul(qs_ps[:, g4, :], lhsT=qpT[:, g, :], rhs=skb[:, g, :], start=True, stop=True),
                             reads=[qpT, skb], writes=[qs_ps])
                    P.op("act", lambda e, g0=g0: e.copy(out=sc[:, g0:g0 + 4, :], in_=qs_ps[:]), reads=[qs_ps], writes=scg[g0:g0 + 4])
                    yield
                for g2 in range(0, 16, 2):
                    gs = (g2, g2 + 1)
                    for g in gs:
                        P.op("dve", lambda e, g=g: e.max(out=s8[:, g, 0:8], in_=sc[:, g, :]), reads=[scg[g]], writes=[scg[g]])
                    for g in gs:
                        P.op("dve", lambda e, g=g: e.max_index(out=i8[:, g, 0:8], in_max=s8[:, g, 0:8], in_values=sc[:, g, :]),
                             reads=[scg[g]], writes=[scg[g]])
                    for g in gs:
                        P.op("dve", lambda e, g=g: e.match_replace(out=sc[:, g, :], in_to_replace=s8[:, g, 0:8], in_values=sc[:, g, :],
                                                                   imm_value=-1e30), reads=[scg[g]], writes=[scg[g]])
                    for g in gs:
                        P.op("dve", lambda e, g=g: e.max(out=s8[:, g, 8:16], in_=sc[:, g, :]), reads=[scg[g]], writes=[scg[g]])
                    for g in gs:
                        P.op("dve", lambda e, g=g: e.max_index(out=i8[:, g, 8:16], in_max=s8[:, g, 8:16], in_values=sc[:, g, :]),
                             reads=[scg[g]], writes=[scg[g]])
                    yield
                P.op("dve", lambda e: e.tensor_copy(out=i8f[:], in_=i8[:]), reads=scg, writes=[i8fr])
                s8v = s8[:].rearrange("p (h two) k -> p h two k", two=2)
                i8v = i8f[:].rearrange("p (h two) k -> p h two k", two=2)
                P.op("dve", lambda e: e.tensor_tensor(out=cand[:], in0=s8v[:, :, 0, :].unsqueeze(3).to_broadcast([128, 8, 16, 16]),
                                                      in1=s8v[:, :, 1, :].unsqueeze(2).to_broadcast([128, 8, 16, 16]), op=ALU.add),
                     reads=scg, writes=candh)
                yield
                P.op("dve", lambda e: e.tensor_scalar(out=tbl[:, 0, :, :], in0=i8v[:, :, 0, :], scalar1=128.0, scalar2=None, op0=ALU.mult),
                     reads=[i8fr], writes=[cidxr])
                P.op("dve", lambda e: e.tensor_copy(out=tbl[:, 1, :, :], in_=i8v[:, :, 1, :]), reads=[i8fr, cidxr], writes=[cidxr])
                yield
                cand3 = cand[:].rearrange("p h a b -> p h (a b)")
                for h2_ in range(0, 8, 2):
                    hs = (h2_, h2_ + 1)
                    for h in hs:
                        P.op("dve", lambda e, h=h: e.max(out=best[:, h, 0:8], in_=cand3[:, h, :]), reads=[candh[h]], writes=[besth[h]])
                    for h in hs:
                        P.op("dve", lambda e, h=h: e.max_index(out=pos[:, h, 0:8], in_max=best[:, h, 0:8], in_values=cand3[:, h, :]),
                             reads=[candh[h], besth[h]], writes=[posh[h]])
                    for h in hs:
                        P.op("dve", lambda e, h=h: e.match_replace(out=cand3[:, h, :], in_to_replace=best[:, h, 0:8], in_values=cand3[:, h, :],
                                                                   imm_value=-1e30), reads=[candh[h], besth[h], posh[h]], writes=[candh[h]])
                    for h in hs:
                        P.op("dve", lambda e, h=h: e.max(out=best[:, h, 8:16], in_=cand3[:, h, :]), reads=[candh[h]], writes=[besth[h]])
                    for h in hs:
                        P.op("dve", lambda e, h=h: e.max_index(out=pos[:, h, 8:16], in_max=best[:, h, 8:16], in_values=cand3[:, h, :]),
                             reads=[candh[h], besth[h]], writes=[posh[h]])
                    yield
                P.op("dve", lambda e: e.tensor_scalar(out=aub[:, 0, :, :], in0=pos[:], scalar1=c4[:, 0:1], scalar2=None, op0=ALU.logical_shift_right),
                     reads=posh + [c4], writes=[aub])
                P.op("dve", lambda e: e.tensor_scalar(out=aub[:, 1, :, :], in0=pos[:], scalar1=c15[:, 0:1], scalar2=None, op0=ALU.bitwise_and),
                     reads=posh + [c15, aub], writes=[aub])
                P.op("dve", lambda e: e.tensor_copy(out=abf[:], in_=aub[:]), reads=[aub], writes=[abf])
                yield
                for a_ in range(16):
                    dst = accd if a_ == 0 else tmpd[a_ % 2]
                    P.op("dve", lambda e, a_=a_, dst=dst: e.scalar_tensor_tensor(
                        out=dst[:], in0=abf[:], scalar=float(a_), in1=tbl[:, :, :, a_:a_ + 1].to_broadcast([128, 2, 8, 16]),
                        op0=ALU.is_equal, op1=ALU.mult), reads=[abf, cidxr], writes=[dst])
                    if a_ > 0:
                        P.op("dve", lambda e, dst=dst: e.tensor_tensor(out=accd[:], in0=accd[:], in1=dst[:], op=ALU.add),
                             reads=[accd, dst], writes=[accd])
                    if a_ % 4 == 3:
                        yield
                P.op("dve", lambda e: e.tensor_tensor(out=eidf[:].rearrange("p (h k) -> p h k", h=8), in0=accd[:, 0, :, :], in1=accd[:, 1, :, :],
                                                      op=ALU.add), reads=[accd], writes=[eidf])
                P.op("dve", lambda e: e.tensor_scalar(out=eidf[:], in0=eidf[:], scalar1=16383.0, scalar2=0.0, op0=ALU.min, op1=ALU.max),
                     reads=[eidf], writes=[eidf])
                P.op("dve", lambda e: e.tensor_copy(out=eidx[:], in_=eidf[:]), reads=[eidf], writes=[eidx])
                P.op("dve", lambda e: e.tensor_tensor(out=ex[:], in0=best[:], in1=best[:, :, 0:1].to_broadcast([128, 8, 16]), op=ALU.subtract),
                     reads=besth, writes=[ex])
                P.op("act", lambda e: e.activation(out=ex[:], in_=ex[:], func=AF.Exp), reads=[ex], writes=[ex])
                yield
                P.op("dve", lambda e: e.tensor_reduce(out=sm[:, :, 0], in_=ex[:], axis=AX.X, op=ALU.add), reads=[ex], writes=[sm])
                P.op("dve", lambda e: e.reciprocal(out=sm[:, :, 1], in_=sm[:, :, 0]), reads=[sm], writes=[sm])
                P.op("dve", lambda e: e.tensor_tensor(out=gg[:].rearrange("p (h k) -> p h k", h=8), in0=ex[:],
                                                      in1=sm[:, :, 1:2].to_broadcast([128, 8, 16]), op=ALU.mult), reads=[ex, sm], writes=[gg])

            def drain(gen, nmax=None):
                if gen is None:
                    return None
                k = 0
                while nmax is None or k < nmax:
                    try:
                        next(gen)
                    except StopIteration:
                        return None
                    k += 1
                return gen

            def batch_front(n_, k):
                eidx = eidx2[n_ % 2]
                h2 = h2p[n_ % 2]
                kp = k % 2
                bufs = []
                for jj in range(BS):
                    j = k * BS + jj
                    rb = ring[state["nring"] % NBUF]
                    state["nring"] += 1
                    bufs.append(rb)
                    P.dma("pool", lambda e, rb=rb, j=j: e.indirect_dma_start(
                        out=rb[:], out_offset=None, in_=self.UVB[:, :], in_offset=bass.IndirectOffsetOnAxis(ap=eidx[:, j:j + 1], axis=0)),
                        reads=[eidx], writes=[rb])
                for jj in range(BS):
                    rb = bufs[jj]
                    P.op("dve", lambda e, rb=rb, jj=jj: e.scalar_tensor_tensor(
                        out=junkd[:], in0=rb[:, 0:DM], scalar=1.0, in1=h2[:], op0=ALU.mult, op1=ALU.mult, accum_out=actb[kp][:, jj:jj + 1]),
                        reads=[rb, h2], writes=[actc[kp][jj]])
                a_, ta_, tb_ = actb[kp], ta[kp], tb[kp]
                P.op("dve", lambda e: e.tensor_tensor(out=ta_[:], in0=a_[:], in1=a_[:], op=ALU.mult), reads=actc[kp], writes=[ta_])
                P.op("dve", lambda e: e.tensor_scalar(out=ta_[:], in0=ta_[:], scalar1=0.044715, scalar2=1.0, op0=ALU.mult, op1=ALU.add),
                     reads=[ta_], writes=[ta_])
                P.op("dve", lambda e: e.tensor_tensor(out=ta_[:], in0=ta_[:], in1=a_[:], op=ALU.mult), reads=[ta_] + actc[kp], writes=[ta_])
                P.op("act", lambda e: e.activation(out=tb_[:], in_=ta_[:], func=AF.Sigmoid, scale=1.5957691216057308), reads=[ta_], writes=[tb_])
                return bufs

            def batch_back(n_, k, bufs):
                gg = ggp[n_ % 2]
                kp = k % 2
                a_, tb_, wg_, Dk_ = actb[kp], tb[kp], wg[kp], Dk[kp]
                P.op("dve", lambda e: e.tensor_tensor(out=tb_[:], in0=tb_[:], in1=a_[:], op=ALU.mult), reads=[tb_] + actc[kp], writes=[tb_])
                P.op("dve", lambda e: e.tensor_tensor(out=wg_[:], in0=tb_[:], in1=gg[:, k * BS:(k + 1) * BS], op=ALU.mult),
                     reads=[tb_, gg], writes=[wg_] + actc[kp])
                for jj in range(BS):
                    P.op("act", lambda e, jj=jj: e.activation(out=Dk_[:, jj, :], in_=self.identb[:], func=AF.Copy, scale=wg_[:, jj:jj + 1]),
                         reads=[self.identb, wg_], writes=[dkc[kp][jj]])
                for jj in range(BS):
                    j = k * BS + jj
                    rb = bufs[jj]
                    for hf in range(2):
                        P.op("pe", lambda e, rb=rb, jj=jj, j=j, hf=hf: e.matmul(
                            acc_ps[:, hf * 512:(hf + 1) * 512], lhsT=Dk_[:, jj, :], rhs=rb[:, DM + hf * 512:DM + (hf + 1) * 512],
                            start=(j == 0), stop=(j == 127)), reads=[dkc[kp][jj], rb], writes=[acc_ps])

            def S3(n_):
                t = tiles[n_]
                b = n_ % 2
                r = 1 if t < 2 else 0
                rows = slice(t * 128, (t + 1) * 128)
                x_, ss_ = xt[b], ss[b]
                P.op("dve", lambda e: e.tensor_tensor(out=m32[:], in0=acc_ps[:], in1=GA2[r][:], op=ALU.mult), reads=[acc_ps, GA2[r]], writes=[m32])
                P.op("dve", lambda e: e.tensor_tensor(out=x_[:], in0=x_[:], in1=m32[:], op=ALU.add), reads=[x_, m32], writes=[x_])
                if final and t >= 2:
                    P.op("act", lambda e: e.activation(out=junkb[:], in_=x_[:], func=AF.Square, accum_out=ss_[:, 4:5]),
                         reads=[x_], writes=[junkb, ss_])
                    P.op("dve", lambda e: e.tensor_scalar(out=ss_[:, 5:6], in0=ss_[:, 4:5], scalar1=1.0 / DM, scalar2=EPS,
                                                          op0=ALU.mult, op1=ALU.add), reads=[ss_], writes=[ss_])
                    P.op("act", lambda e: e.activation(out=ss_[:, 6:7], in_=ss_[:, 5:6], func=AF.Sqrt), reads=[ss_], writes=[ss_])
                    P.op("dve", lambda e: e.reciprocal(out=ss_[:, 7:8], in_=ss_[:, 6:7]), reads=[ss_], writes=[ss_])
                    P.op("dve", lambda e: e.scalar_tensor_tensor(out=x_[:], in0=x_[:], scalar=ss_[:, 7:8], in1=gfin[:],
                                                                 op0=ALU.mult, op1=ALU.mult), reads=[x_, ss_, gfin], writes=[x_])
                    P.dma("sp", lambda e: e.dma_start(out=self.Y[(t - 2) * 128:(t - 1) * 128, :], in_=x_[:]), reads=[x_])
                else:
                    P.dma("sp", lambda e: e.dma_start(out=self.X[rows, :], in_=x_[:]), reads=[x_])

            nT = len(tiles)
            drain(S1(0))
            for n_ in range(nT):
                gen = S1(n_ + 1) if n_ + 1 < nT else None
                prev = None
                for k in range(NBATCH):
                    bufs = batch_front(n_, k)
                    if prev is not None:
                        batch_back(n_, k - 1, prev)
                    prev = bufs
                    gen = drain(gen, 3)
                batch_back(n_, NBATCH - 1, prev)
                drain(gen)
                S3(n_)
            P.barrier()

    def build(self):
        c = self.cfg
        with contextlib.ExitStack() as st:
            self.consts(st)
            for l in c.layers:
                last = (l == c.depth - 1)
                with contextlib.ExitStack() as modst:
                    if "M" in c.phases:
                        self.phase_mod(l, modst)
                    if "A" in c.phases:
                        self.phase_a(l)
                if "B" in c.phases:
                    self.phase_b(l)
                if "C" in c.phases:
                    self.phase_c(l, not last)
                if "D" in c.phases:
                    self.phase_d(l, not last)
                if "E" in c.phases:
                    self.phase_e(l, not last, last and c.final)
            self.P.barrier()
            with self.nc.allow_non_contiguous_dma(reason='small strided layout DMAs'):
                self.P.build()
        return self.nc


def host_consts(n_lat):
    p = np.arange(64)
    f = p % 64
    i = f % 16
    freq = (10000.0 ** (-(i.astype(np.float32)) / 16.0)).astype(np.float32)
    rope = np.zeros((n_lat, 64, 2, 128), np.float32)
    j = np.arange(128)
    for m in range(n_lat):
        row = (2 * m + j // 64).astype(np.float32)
        col = (j % 64).astype(np.float32)
        pos = np.where((f < 32)[:, None], row[None, :], col[None, :]).astype(np.float32)
        ang = pos * freq[:, None]
        rope[m, :, 0, :] = np.cos(ang)
        rope[m, :, 1, :] = np.sin(ang)
    rotm = np.zeros((64, 64), np.float32)
    for m_ in range(64):
        fm = m_ % 64
        if (fm % 32) < 16:
            rotm[m_ + 16, m_] = -1.0
        else:
            rotm[m_ - 16, m_] = 1.0
    return rope, rotm


def host_bias(rpb, pats):
    L = rpb.shape[0]
    out = np.empty((L, len(pats), 128, 8, 5, 128), np.float32)
    for pi, (valid, roff, coff) in enumerate(pats):
        g = rpb[:, :, roff, coff]
        g = np.where(valid[None, None], g, np.float32(NEG))
        out[:, pi] = g.transpose(0, 3, 1, 2, 4)
    return out


_CACHE = {}


def make_inputs(inputs, cfg, cores):
    n_lat = cfg.n_lat
    rope, rotm = host_consts(n_lat)
    _, _, pats = na_patterns(n_lat)
    shared = dict(
        w_mod=inputs["w_mod"], b_mod=inputs["b_mod"], g_norm1=inputs["g_norm1"], w_in=inputs["w_in"],
        w_alpha_f=inputs["w_alpha_f"], b_alpha_f=inputs["b_alpha_f"], w_alpha_b=inputs["w_alpha_b"], b_alpha_b=inputs["b_alpha_b"],
        g_gla=inputs["g_gla"], biasT=host_bias(np.asarray(inputs["rpb"]), pats), w_proj_gla=inputs["w_proj_gla"],
        w_proj_na=inputs["w_proj_na"], w_out=inputs["w_out"], g_norm2=inputs["g_norm2"], w_query=inputs["w_query"],
        skT=np.ascontiguousarray(np.asarray(inputs["sub_keys"]).reshape(-1, 16, 128, 128).transpose(0, 1, 3, 2)),
        expert_u=inputs["expert_u"], expert_v=inputs["expert_v"], g_final=inputs["g_final"], rope=rope, rotm=rotm)
    shared = {k: np.ascontiguousarray(np.asarray(v, dtype=np.float32)) for k, v in shared.items()}
    maps = []
    for b in cores:
        xin = np.concatenate([np.asarray(inputs["ctx"][b]), np.asarray(inputs["x"][b][:n_lat * 128])], axis=0).astype(np.float32)
        cv = np.stack([np.asarray(inputs["c"][b]), np.asarray(inputs["c_ctx"])], axis=-1).astype(np.float32)
        cvec = np.ascontiguousarray(cv.reshape(8, 128, 2).transpose(1, 0, 2))
        m = dict(shared)
        m["xin"] = np.ascontiguousarray(xin)
        m["cvec"] = cvec
        maps.append(m)
    return maps


def kernel(**inputs):
    cfg = Cfg()
    if "nc" not in _CACHE:
        _CACHE["nc"] = Builder(cfg).build()
    nc = _CACHE["nc"]
    maps = make_inputs(inputs, cfg, list(range(8)))
    res = run_bass_kernel_spmd(nc, maps, core_ids=list(range(8)))
    out = np.stack([np.asarray(r["Y"]) for r in res.results], axis=0).astype(np.float32)
    return out
```
